# Optimizing a Trainium2 kernel written in Bass

```python
import jax, jax.numpy as jnp
from jax import lax
import numpy as np


D_MODEL = 1024
BATCH = 32
SEQ = 2048
DEPTH = 4

GLA_HEADS = 4
GLA_VW = D_MODEL // 2
GLA_DV = GLA_VW // GLA_HEADS
GLA_KW = GLA_VW // 2
GLA_DK = GLA_KW // GLA_HEADS
GLA_LORA = 16
GLA_TAU = 16.0
GLA_CHUNK = 64
RWKV_W = D_MODEL - GLA_VW
RWKV_HEAD = 64
RWKV_HEADS = RWKV_W // RWKV_HEAD
RWKV_W_LORA = 32
RWKV_A_LORA = 32
RWKV_G_LORA = 96
RWKV_LNX_EPS = 64e-5
ATT_HEADS = 8
ATT_HEAD = D_MODEL // ATT_HEADS
IDX_HEADS = 8
IDX_DIM = 64
TOPK_MAX = 256
Q_BLOCK = 128
ROPE_THETA = 10000.0
NEG = -1e30
PEER_HEADS = 8
PEER_DKEY = 128
PEER_NKEYS = 128
PEER_N_EXPERTS = PEER_NKEYS * PEER_NKEYS
PEER_TOPK = 16
PEER_TOKEN_BLOCK = 128
DN_ALPHA = (2 * DEPTH) ** 0.25
DN_BETA = (8 * DEPTH) ** -0.25
LN_EPS = 1e-5
N_EVEN = (DEPTH + 1) // 2
N_ODD = DEPTH // 2

GLA_SPLITS = (GLA_KW, GLA_KW, GLA_VW, GLA_VW, GLA_LORA)
RWKV_SPLITS = (RWKV_W, RWKV_W, RWKV_W, RWKV_W_LORA, RWKV_A_LORA, RWKV_G_LORA)
GLA_IN = sum(GLA_SPLITS)
RWKV_IN = sum(RWKV_SPLITS)
EVEN_IN = GLA_IN + RWKV_IN
ODD_SPLITS = (ATT_HEADS * ATT_HEAD, ATT_HEAD, ATT_HEAD, IDX_HEADS * IDX_DIM, IDX_DIM, IDX_HEADS)
ODD_IN = sum(ODD_SPLITS)

kernel_name = 'hybrid_gla_rwkv7_dsa_peer_deepnorm'

F32 = jnp.float32


def _split(t, sizes):
    cuts = [int(c) for c in np.cumsum(sizes)[:-1]]
    return jnp.split(t, cuts, axis=-1)


def layer_norm(x, g, b):
    xf = x.astype(F32)
    mu = xf.mean(-1, keepdims=True)
    var = jnp.square(xf - mu).mean(-1, keepdims=True)
    return ((xf - mu) * lax.rsqrt(var + LN_EPS) * g + b).astype(x.dtype)


def rope_tables(T, dim):
    inv = ROPE_THETA ** (-jnp.arange(0, dim, 2, dtype=F32) / dim)
    ang = jnp.arange(T, dtype=F32)[:, None] * inv[None, :]
    return jnp.cos(ang), jnp.sin(ang)


def apply_rope(x, cos, sin):
    x1, x2 = jnp.split(x, 2, axis=-1)
    return jnp.concatenate([x1 * cos - x2 * sin, x2 * cos + x1 * sin], -1).astype(x.dtype)


def gla_chunked(q, k, v, log_a):
    B, T, H, dk = q.shape
    dv = v.shape[-1]
    C = GLA_CHUNK
    n = T // C

    def chunks(t):
        return t.astype(F32).reshape(B, n, C, H, -1).transpose(0, 3, 1, 2, 4)

    q, k, v, la = chunks(q) * dk ** -0.5, chunks(k), chunks(v), chunks(log_a)
    b = jnp.cumsum(la, axis=3)
    b_last = b[:, :, :, -1:, :]
    q_in = q * jnp.exp(b)
    k_in = k * jnp.exp(-b)
    k_out = k * jnp.exp(b_last - b)
    causal = jnp.tril(jnp.ones((C, C), dtype=bool))
    A = jnp.where(causal, jnp.einsum('bhnid,bhnjd->bhnij', q_in, k_in), 0.0)
    o = jnp.einsum('bhnij,bhnje->bhnie', A, v)
    kv = jnp.einsum('bhnjd,bhnje->bhnde', k_out, v)
    dec = jnp.exp(b_last[:, :, :, 0, :])

    def step(S, inp):
        dec_n, kv_n = inp
        return dec_n[..., None] * S + kv_n, S

    S0 = jnp.zeros((B, H, dk, dv), F32)
    _, S_prev = lax.scan(step, S0, (jnp.moveaxis(dec, 2, 0), jnp.moveaxis(kv, 2, 0)))
    S_prev = jnp.moveaxis(S_prev, 0, 2)
    o = o + jnp.einsum('bhnid,bhnde->bhnie', q_in, S_prev)
    return o.transpose(0, 2, 3, 1, 4).reshape(B, T, H, dv)


def rwkv7_scan(r, w, k, v, a, b):
    B, T, H, N = r.shape

    def step(S, inp):
        r_t, w_t, k_t, v_t, a_t, b_t = inp
        sa = jnp.einsum('bhij,bhj->bhi', S, a_t)
        S = S * w_t[:, :, None, :] + sa[..., None] * b_t[:, :, None, :] + v_t[..., None] * k_t[:, :, None, :]
        return S, jnp.einsum('bhij,bhj->bhi', S, r_t)

    xs = tuple(jnp.moveaxis(t.astype(F32), 1, 0) for t in (r, w, k, v, a, b))
    _, y = lax.scan(step, jnp.zeros((B, H, N, N), F32), xs)
    return jnp.moveaxis(y, 0, 1)


def even_mixer(x, w_in, gla_a_w2, gla_a_b, gla_norm_g, rwkv_mu, rwkv_w0, rwkv_w2, rwkv_a0, rwkv_a2,
               rwkv_g2, rwkv_k_k, rwkv_k_a, rwkv_r_k, rwkv_lnx_g, rwkv_lnx_b, w_out):
    B, T, _ = x.shape

    def heads(t, h):
        return t.reshape(B, T, h, -1)

    p = x @ w_in
    gla_p, rw_p = p[..., :GLA_IN], p[..., GLA_IN:]
    gq, gk, gv, gg, gal = _split(gla_p, GLA_SPLITS)
    log_a = jax.nn.log_sigmoid((gal @ gla_a_w2 + gla_a_b).astype(F32)) / GLA_TAU
    o = gla_chunked(heads(gq, GLA_HEADS), heads(gk, GLA_HEADS), heads(gv, GLA_HEADS), heads(log_a, GLA_HEADS))
    o = o * lax.rsqrt(jnp.mean(jnp.square(o), -1, keepdims=True) + LN_EPS) * gla_norm_g
    gla_out = (o.reshape(B, T, GLA_VW) * jax.nn.silu(gg.astype(F32))).astype(x.dtype)
    prev = jnp.pad(rw_p, ((0, 0), (1, 0), (0, 0)))[:, :-1]
    rw_p = rw_p + (prev - rw_p) * rwkv_mu
    r, k, v, wl, al, gl = _split(rw_p, RWKV_SPLITS)
    w_log = -jax.nn.softplus(-(rwkv_w0 + jnp.tanh(wl) @ rwkv_w2).astype(F32)) - 0.5
    decay = jnp.exp(-jnp.exp(w_log))
    a = jax.nn.sigmoid((rwkv_a0 + al @ rwkv_a2).astype(F32))
    g = jax.nn.sigmoid(gl) @ rwkv_g2
    kk = heads((k * rwkv_k_k).astype(F32), RWKV_HEADS)
    kk = kk / jnp.maximum(jnp.sqrt(jnp.sum(jnp.square(kk), -1, keepdims=True)), 1e-12)
    k = k * (1.0 + (a - 1.0) * rwkv_k_a)
    a_h = heads(a, RWKV_HEADS)
    r_h, k_h, v_h = heads(r, RWKV_HEADS), heads(k, RWKV_HEADS), heads(v, RWKV_HEADS)
    y = rwkv7_scan(r_h, heads(decay, RWKV_HEADS), k_h, v_h, -kk, kk * a_h)
    mu = y.mean(-1, keepdims=True)
    var = jnp.square(y - mu).mean(-1, keepdims=True)
    y = (y - mu) * lax.rsqrt(var + RWKV_LNX_EPS) * rwkv_lnx_g.reshape(RWKV_HEADS, RWKV_HEAD) \
        + rwkv_lnx_b.reshape(RWKV_HEADS, RWKV_HEAD)
    bonus = jnp.sum((r_h * k_h * rwkv_r_k).astype(F32), -1, keepdims=True) * v_h.astype(F32)
    rwkv_out = ((y + bonus).reshape(B, T, RWKV_W) * g.astype(F32)).astype(x.dtype)
    return jnp.concatenate([gla_out, rwkv_out], -1) @ w_out


def dsa_mixer(x, w_in, w_out):
    B, T, _ = x.shape
    q, k, v, qi, ki, wi = _split(x @ w_in, ODD_SPLITS)
    q = q.reshape(B, T, ATT_HEADS, ATT_HEAD)
    qi = qi.reshape(B, T, IDX_HEADS, IDX_DIM)
    cos_a, sin_a = rope_tables(T, ATT_HEAD)
    cos_i, sin_i = rope_tables(T, IDX_DIM)
    q = apply_rope(q, cos_a[:, None], sin_a[:, None])
    k = apply_rope(k, cos_a, sin_a)
    qi = apply_rope(qi, cos_i[:, None], sin_i[:, None])
    ki = apply_rope(ki, cos_i, sin_i)
    wi = wi.astype(F32) * (IDX_HEADS ** -0.5 * IDX_DIM ** -0.5)
    n_sel = min(TOPK_MAX, T // 4)
    key_pos = jnp.arange(T)
    gather = jax.vmap(lambda tab, ids: tab[ids])

    def block(start):
        qb = lax.dynamic_slice_in_dim(q, start, Q_BLOCK, axis=1)
        qib = lax.dynamic_slice_in_dim(qi, start, Q_BLOCK, axis=1)
        wib = lax.dynamic_slice_in_dim(wi, start, Q_BLOCK, axis=1)
        q_pos = start + jnp.arange(Q_BLOCK)
        s = jax.nn.relu(jnp.einsum('bqhd,bsd->bqhs', qib, ki).astype(F32))
        score = jnp.einsum('bqh,bqhs->bqs', wib, s)
        score = jnp.where(key_pos[None, None, :] <= q_pos[None, :, None], score, NEG)
        _, sel = lax.top_k(score, n_sel)
        valid = sel <= q_pos[None, :, None]
        ks, vs = gather(k, sel), gather(v, sel)
        logit = jnp.einsum('bqhd,bqkd->bqhk', qb, ks).astype(F32) * ATT_HEAD ** -0.5
        logit = jnp.where(valid[:, :, None, :], logit, NEG)
        pr = jax.nn.softmax(logit, axis=-1).astype(vs.dtype)
        return jnp.einsum('bqhk,bqkd->bqhd', pr, vs)

    starts = jnp.arange(T // Q_BLOCK, dtype=jnp.int32) * Q_BLOCK
    o = lax.map(block, starts)
    o = jnp.moveaxis(o, 0, 1).reshape(B, T, ATT_HEADS * ATT_HEAD)
    return o @ w_out


def peer_ffn(x, w_q, sub_keys, exp_u, exp_v):
    B, T, D = x.shape
    q = (x @ w_q).reshape(B, T, PEER_HEADS, 2, PEER_DKEY // 2)
    s = jnp.einsum('bthcd,hcnd->bthcn', q, sub_keys).astype(F32)
    top_s, top_i = lax.top_k(s, PEER_TOPK)
    cand_s = (top_s[..., 0, :, None] + top_s[..., 1, None, :]).reshape(B, T, PEER_HEADS, -1)
    cand_i = (top_i[..., 0, :, None] * PEER_NKEYS + top_i[..., 1, None, :]).reshape(B, T, PEER_HEADS, -1)
    best_s, pos = lax.top_k(cand_s, PEER_TOPK)
    idx = jnp.take_along_axis(cand_i, pos, axis=-1)
    gate = jax.nn.softmax(best_s, axis=-1)
    nb = (B * T) // PEER_TOKEN_BLOCK
    hk = PEER_HEADS * PEER_TOPK
    xs = (x.reshape(nb, PEER_TOKEN_BLOCK, D), idx.reshape(nb, PEER_TOKEN_BLOCK, hk),
          gate.reshape(nb, PEER_TOKEN_BLOCK, hk))

    def block(args):
        xb, ib, gb = args
        h = jax.nn.gelu(jnp.einsum('md,mkd->mk', xb, exp_u[ib]).astype(F32), approximate=False)
        coef = (gb * h).astype(xb.dtype)
        return jnp.einsum('mk,mkd->md', coef, exp_v[ib])

    return lax.map(block, xs).reshape(B, T, D)


def setup_inputs(seed: int = 0) -> dict:
    key = jax.random.key(seed)
    ks = iter(jax.random.split(key, 32))

    def nrm(shape, scale):
        return jax.random.normal(next(ks), shape, F32) * scale

    E, O, L, D = N_EVEN, N_ODD, DEPTH, D_MODEL
    return {
        'x': nrm((BATCH, SEQ, D), 1.0),
        'even_w_in': nrm((E, D, EVEN_IN), D ** -0.5),
        'gla_a_w2': nrm((E, GLA_LORA, GLA_KW), GLA_LORA ** -0.5),
        'gla_a_b': nrm((E, GLA_KW), 0.1),
        'gla_norm_g': 1.0 + nrm((E, GLA_DV), 0.05),
        'rwkv_mu': jax.random.uniform(next(ks), (E, RWKV_IN), F32, 0.0, 1.0),
        'rwkv_w0': jax.random.uniform(next(ks), (E, RWKV_W), F32, -6.5, -1.5),
        'rwkv_w2': nrm((E, RWKV_W_LORA, RWKV_W), 0.1 * RWKV_W_LORA ** -0.5),
        'rwkv_a0': nrm((E, RWKV_W), 0.1),
        'rwkv_a2': nrm((E, RWKV_A_LORA, RWKV_W), 0.5 * RWKV_A_LORA ** -0.5),
        'rwkv_g2': nrm((E, RWKV_G_LORA, RWKV_W), 2.0 * RWKV_G_LORA ** -0.5),
        'rwkv_k_k': 0.85 + nrm((E, RWKV_W), 0.05),
        'rwkv_k_a': 1.0 + nrm((E, RWKV_W), 0.05),
        'rwkv_r_k': nrm((E, RWKV_HEADS, RWKV_HEAD), 0.1),
        'rwkv_lnx_g': 1.0 + nrm((E, RWKV_W), 0.05),
        'rwkv_lnx_b': nrm((E, RWKV_W), 0.02),
        'even_w_out': nrm((E, D, D), DN_BETA * D ** -0.5),
        'odd_w_in': nrm((O, D, ODD_IN), D ** -0.5),
        'odd_w_out': nrm((O, D, D), DN_BETA * D ** -0.5),
        'mix_ln_g': 1.0 + nrm((L, D), 0.05),
        'mix_ln_b': nrm((L, D), 0.02),
        'peer_w_q': nrm((L, D, PEER_HEADS * PEER_DKEY), D ** -0.5),
        'peer_sub_keys': nrm((L, PEER_HEADS, 2, PEER_NKEYS, PEER_DKEY // 2), (PEER_DKEY // 2) ** -0.5),
        'peer_u': nrm((L, PEER_N_EXPERTS, D), D ** -0.5),
        'peer_v': nrm((L, PEER_N_EXPERTS, D), DN_BETA * PEER_HEADS ** -0.5),
        'ffn_ln_g': 1.0 + nrm((L, D), 0.05),
        'ffn_ln_b': nrm((L, D), 0.02),
    }


def reference(x, even_w_in, gla_a_w2, gla_a_b, gla_norm_g, rwkv_mu, rwkv_w0, rwkv_w2, rwkv_a0, rwkv_a2,
              rwkv_g2, rwkv_k_k, rwkv_k_a, rwkv_r_k, rwkv_lnx_g, rwkv_lnx_b, even_w_out, odd_w_in,
              odd_w_out, mix_ln_g, mix_ln_b, peer_w_q, peer_sub_keys, peer_u, peer_v, ffn_ln_g, ffn_ln_b):
    for layer in range(DEPTH):
        i = layer // 2
        if layer % 2 == 0:
            mix = even_mixer(x, even_w_in[i], gla_a_w2[i], gla_a_b[i], gla_norm_g[i], rwkv_mu[i], rwkv_w0[i],
                             rwkv_w2[i], rwkv_a0[i], rwkv_a2[i], rwkv_g2[i], rwkv_k_k[i], rwkv_k_a[i],
                             rwkv_r_k[i], rwkv_lnx_g[i], rwkv_lnx_b[i], even_w_out[i])
        else:
            mix = dsa_mixer(x, odd_w_in[i], odd_w_out[i])
        x = layer_norm(DN_ALPHA * x + mix, mix_ln_g[layer], mix_ln_b[layer])
        ffn = peer_ffn(x, peer_w_q[layer], peer_sub_keys[layer], peer_u[layer], peer_v[layer])
        x = layer_norm(DN_ALPHA * x + ffn, ffn_ln_g[layer], ffn_ln_b[layer])
    return x
```

```python
import numpy as np
from contextlib import ExitStack
import concourse.bass as bass
import concourse.mybir as mybir
from concourse.bass_utils import run_bass_kernel_spmd
import ml_dtypes

F32 = mybir.dt.float32
BF16 = mybir.dt.bfloat16
U32 = mybir.dt.uint32
AF = mybir.ActivationFunctionType
ALU = mybir.AluOpType
AX = mybir.AxisListType

T = 2048
D = 1024
DEPTH = 4
ALPHA = float((2 * DEPTH) ** 0.25)
LN_EPS = 1e-5
NEG = -1e30
SEM_EPOCH = 60000


class Tok:
    __slots__ = ("w", "r", "t", "dsem", "dcnt", "name")

    def __init__(self, t=None, name=None):
        self.w = None
        self.r = []
        self.t = t
        self.dsem = None
        self.dcnt = 0
        self.name = name

    def __getitem__(self, k):
        return self.t[k]


class Ctx:
    def __init__(self, nc, es):
        self.nc = nc
        self.es = es
        self.es0 = es
        self.dpool = {}
        self.eng = {"pe": nc.tensor, "dve": nc.vector, "act": nc.scalar, "pool": nc.gpsimd, "sp": nc.sync}
        self.sem = {}
        self.cnt = {}
        for e in self.eng:
            self.sem[e] = es.enter_context(nc.semaphore("s_" + e))
            self.cnt[e] = 0
        self.waited = {e: {} for e in self.eng}
        self.pe_sems = {id(self.sem["pe"])}
        self.nsem = 0
        self.ninst = 0
        self.rr = 0
        self.uid = 0

    def sb(self, name, shape, dt=F32):
        self.uid += 1
        return Tok(self.es.enter_context(self.nc.sbuf_tensor("sb%d_%s" % (self.uid, name), list(shape), dt)), name)

    def ps(self, name, shape, dt=F32):
        self.uid += 1
        return Tok(self.es.enter_context(self.nc.psum_tensor("ps%d_%s" % (self.uid, name), list(shape), dt)), name)

    def tok(self, name=None):
        return Tok(None, name)

    def _dsem(self, tok):
        if tok.name not in self.dpool or self.dpool[tok.name][1] >= SEM_EPOCH:
            self.dpool[tok.name] = [self.es0.enter_context(self.nc.semaphore("d%d" % self.nsem)), 0]
            self.nsem += 1
        return self.dpool[tok.name]

    def barrier(self):
        for e in self.eng:
            for o in self.eng:
                if o != e and self.cnt[o] > 0:
                    self._wait(e, (self.sem[o], self.cnt[o]))
            for sem, cnt in self.dpool.values():
                if cnt > 0:
                    self._wait(e, (sem, cnt))

    def _wait(self, e, dep):
        if dep is None:
            return
        sem, v = dep
        k = id(sem)
        if False and e == "pe" and k in self.pe_sems:
            return
        if self.waited[e].get(k, 0) >= v:
            return
        self.waited[e][k] = v
        self.eng[e].wait_ge(sem, v)
        self.ninst += 1

    def _deps(self, e, reads, writes):
        for t in reads:
            self._wait(e, t.w)
        for t in writes:
            self._wait(e, t.w)
            for d in t.r:
                self._wait(e, d)

    @staticmethod
    def _compact(lst):
        best = {}
        for sem, v in lst:
            k = id(sem)
            if k not in best or best[k][1] < v:
                best[k] = (sem, v)
        return list(best.values())

    def _mark(self, me, reads, writes):
        for t in reads:
            t.r.append(me)
            if len(t.r) > 12:
                t.r = self._compact(t.r)
        for t in writes:
            t.w = me
            t.r = []

    def op(self, e, fn, reads=(), writes=()):
        self._deps(e, reads, writes)
        if self.cnt[e] >= SEM_EPOCH:
            self.sem[e] = self.es0.enter_context(self.nc.semaphore("s_%s_%d" % (e, self.nsem)))
            self.nsem += 1
            self.cnt[e] = 0
            if e == "pe":
                self.pe_sems.add(id(self.sem[e]))
        ins = fn(self.eng[e])
        self.cnt[e] += 1
        ins.then_inc(self.sem[e], 1)
        self.ninst += 1
        self._mark((self.sem[e], self.cnt[e]), reads, writes)
        return ins

    def dma(self, q, out_ap, in_ap, reads=(), writes=(), wtok=None, **kw):
        self._deps(q, reads, writes)
        tk = wtok or (writes[0] if writes else reads[0])
        ent = self._dsem(tk)
        ins = self.eng[q].dma_start(out=out_ap, in_=in_ap, **kw)
        ent[1] += 16
        ins.then_inc(ent[0], 16)
        self.ninst += 1
        self._mark((ent[0], ent[1]), reads, writes)
        return ins

    def gather(self, out_ap, in_ap, idx_ap, reads=(), writes=()):
        q = "pool"
        self._deps(q, reads, writes)
        tk = writes[0]
        ent = self._dsem(tk)
        ins = self.nc.gpsimd.indirect_dma_start(
            out=out_ap, out_offset=None, in_=in_ap,
            in_offset=bass.IndirectOffsetOnAxis(ap=idx_ap, axis=0))
        ent[1] += 16
        ins.then_inc(ent[0], 16)
        self.ninst += 1
        self._mark((ent[0], ent[1]), reads, writes)
        return ins

    def finish(self, toks, e="sp"):
        for t in toks:
            self._wait(e, t.w)
            for d in t.r:
                self._wait(e, d)

    def qsel(self):
        self.rr += 1
        return ("sp", "act")[self.rr % 2]


class Shared:
    pass


def load_w_bf16(c, S, dst, src, K, N, pofs=0, rows=128):
    for k in range(K):
        for n0 in range(0, N, 1024):
            n1 = min(N, n0 + 1024)
            S.wsi += 1
            st = S.wstage[S.wsi % 2]
            c.dma(c.qsel(), st[pofs:pofs + rows, 0:n1 - n0], src[k * rows:(k + 1) * rows, n0:n1], writes=[st])
            e = ("dve", "pool")[S.wsi % 2]
            c.op(e, lambda en, st=st, k=k, n0=n0, n1=n1: en.tensor_copy(dst[pofs:pofs + rows, k, n0:n1], st[pofs:pofs + rows, 0:n1 - n0]),
                 reads=[st], writes=[dst])


def bcast_row(c, dst, src_row, n):
    c.dma(c.qsel(), dst[:, 0:n], src_row.partition_broadcast(128), writes=[dst])


def layer_norm(c, S, out_ap, out_tok, z, g_bc, b_bc):
    st, mv, sd = S.ln_st, S.ln_mv, S.ln_sd
    c.op("dve", lambda e: e.bn_stats(st[:, 0, :], z[:, 0:512]), reads=[z], writes=[st])
    c.op("dve", lambda e: e.bn_stats(st[:, 1, :], z[:, 512:1024]), reads=[z], writes=[st])
    c.op("dve", lambda e: e.bn_aggr(mv[:], st[:]), reads=[st], writes=[mv])
    c.op("dve", lambda e: e.tensor_scalar(sd[:, 0:1], mv[:, 1:2], LN_EPS, None, op0=ALU.add), reads=[mv], writes=[sd])
    c.op("act", lambda e: e.activation(sd[:, 1:2], sd[:, 0:1], AF.Sqrt), reads=[sd], writes=[sd])
    c.op("dve", lambda e: e.reciprocal(sd[:, 2:3], sd[:, 1:2]), reads=[sd], writes=[sd])
    c.op("dve", lambda e: e.tensor_scalar(z[:], z[:], mv[:, 0:1], sd[:, 2:3], op0=ALU.subtract, op1=ALU.mult),
         reads=[z, mv, sd], writes=[z])
    c.op("pool", lambda e: e.tensor_tensor(z[:], z[:], g_bc[:], ALU.mult), reads=[z, g_bc], writes=[z])
    c.op("pool", lambda e: e.tensor_tensor(out_ap, z[:], b_bc[:], ALU.add), reads=[z, b_bc], writes=[out_tok])


NG = 12
GS = 4
PULLN = 1
PULLD = 1


def uv_prep(c, S, A, layer):
    with ExitStack() as esP:
        c.es = esP
        uf = [c.sb("p_uf%d" % j, [128, 1024]) for j in range(2)]
        vf = [c.sb("p_vf%d" % j, [128, 1024]) for j in range(2)]
        uvb = [c.sb("p_uvb%d" % j, [128, 2048], BF16) for j in range(2)]
        for r in range(128):
            U, V, B = uf[r % 2], vf[r % 2], uvb[r % 2]
            c.dma("sp", U[:], A["peer_u"][layer, r * 128:(r + 1) * 128, :], writes=[U])
            c.dma("act", V[:], A["peer_v"][layer, r * 128:(r + 1) * 128, :], writes=[V])
            c.op("dve", lambda e, U=U, B=B: e.tensor_copy(B[:, 0:1024], U[:]), reads=[U], writes=[B])
            c.op("pool", lambda e, V=V, B=B: e.tensor_copy(B[:, 1024:2048], V[:]), reads=[V], writes=[B])
            c.dma("sp", S.UV_s[r * 128:(r + 1) * 128, :], B[:], reads=[B], writes=[S.uv_tok], wtok=B)
        c.barrier()


def tail_alloc(c, S):
    S.wout = c.sb("wout", [128, 8, 1024], BF16)
    S.wq = c.sb("wq", [128, 8, 1024], BF16)
    S.keysT = c.sb("keysT", [128, 8, 128], BF16)
    S.lng = [c.sb("lng%d" % i, [128, 1024]) for i in range(4)]
    S.iota16 = c.sb("iota16", [128, 16])
    S.oT = c.sb("oT_sb", [128, 8, 128], BF16)
    S.xt = c.sb("xt", [128, 1024])
    S.z = c.sb("z", [128, 1024])
    S.xm2 = [c.sb("xm%d" % i, [128, 1024]) for i in range(2)]
    S.xmb2 = [c.sb("xmb%d" % i, [128, 1024], BF16) for i in range(2)]
    S.idxu2 = [c.sb("idxu%d" % i, [128, 128], U32) for i in range(2)]
    S.gate2 = [c.sb("gate%d" % i, [128, 8, 16]) for i in range(2)]
    S.xm, S.xmb = S.xm2[0], S.xmb2[0]
    S.xmT = c.sb("xmT", [128, 8, 128], BF16)
    S.qT = c.sb("qT", [128, 8, 128], BF16)
    S.ssc = c.sb("ssc", [128, 128])
    S.tops = c.sb("tops", [128, 16, 16])
    S.topi = c.sb("topi", [128, 16, 16], U32)
    S.topf = c.sb("topf", [128, 16, 16])
    S.cands = c.sb("cands", [128, 8, 256])
    S.candw = c.sb("candw", [128, 256])
    S.best = c.sb("best", [128, 8, 16])
    S.pos = c.sb("pos", [128, 8, 16], U32)
    S.pab = c.sb("pab", [128, 2, 128], U32)
    S.pabf = c.sb("pabf", [128, 2, 128])
    S.eq = [c.sb("eq%d" % i, [128, 128, 16]) for i in range(2)]
    S.idx2 = c.sb("idx2", [128, 2, 128])
    S.idxf = c.sb("idxf", [128, 128])
    S.idxu, S.gate = S.idxu2[0], S.gate2[0]
    S.gsum = c.sb("gsum", [128, 8])
    S.hh4 = [c.sb("hh%d" % i, [128, GS]) for i in range(4)]
    S.coef4 = [c.sb("coef%d" % i, [128, GS]) for i in range(4)]
    S.junk2 = [c.sb("junk%d" % i, [128, 1024], BF16) for i in range(2)]
    S.acc = c.sb("acc", [128, 1024])
    S.G = [c.sb("G%d" % i, [128, 2048], BF16) for i in range(NG)]
    S.dg = [c.sb("dg%d" % i, [128, 128], BF16) for i in range(4)]
    S.ot = c.sb("ot", [128, 1024])
    S.psA = c.ps("psA", [128, 1024])
    S.psT = c.ps("psT", [128, 1024])
    S.psS = c.ps("psS", [128, 8, 128])
    S.psAcc = c.ps("psAcc", [128, 1024])


def tail_weights(c, S, A, layer):
    wo = A["even_w_out"][layer // 2] if layer % 2 == 0 else A["odd_w_out"][layer // 2]
    load_w_bf16(c, S, S.wout, wo, 8, 1024)
    load_w_bf16(c, S, S.wq, A["peer_w_q"][layer], 8, 1024)
    bcast_row(c, S.lng[0], A["mix_ln_g"][layer:layer + 1, :], 1024)
    bcast_row(c, S.lng[1], A["mix_ln_b"][layer:layer + 1, :], 1024)
    bcast_row(c, S.lng[2], A["ffn_ln_g"][layer:layer + 1, :], 1024)
    bcast_row(c, S.lng[3], A["ffn_ln_b"][layer:layer + 1, :], 1024)
    c.dma("sp", S.iota16[:], A["iota16"], writes=[S.iota16])
    sk = A["peer_sub_keys"][layer]
    for h in range(8):
        st = S.wstage[h % 2]
        for cc in range(2):
            c.dma(c.qsel(), st[:, cc * 64:(cc + 1) * 64], sk[h, cc], writes=[st])
        c.op("dve", lambda e, st=st: e.tensor_copy(S.xmb[:, 0:128], st[:, 0:128]), reads=[st], writes=[S.xmb])
        c.op("pe", lambda e: e.matmul(S.psT[:, 0:128], S.xmb[:, 0:128], S.identb[:], start=True, stop=True), reads=[S.xmb, S.identb], writes=[S.psT])
        c.op("act", lambda e, h=h: e.copy(S.keysT[:, h, :], S.psT[:, 0:128]), reads=[S.psT], writes=[S.keysT])


def tail_front(c, S, A, layer, x_src, oT_src, par):
    xm, xmb, idxu, gate = S.xm2[par], S.xmb2[par], S.idxu2[par], S.gate2[par]
    c.dma("sp", S.xt[:], x_src, writes=[S.xt], reads=[S.xs_tok])
    yield
    c.dma("act", S.oT[:].rearrange("p k t -> p (k t)"), oT_src, writes=[S.oT], reads=[S.oT_tok])
    yield
    for half in range(2):
        for k in range(8):
            c.op("pe", lambda e, k=k, half=half: e.matmul(S.psA[:, half * 512:(half + 1) * 512], S.oT[:, k, :],
                                                            S.wout[:, k, half * 512:(half + 1) * 512],
                                                            start=(k == 0), stop=(k == 7)),
                 reads=[S.oT, S.wout], writes=[S.psA])
            yield
    c.op("dve", lambda e: e.scalar_tensor_tensor(S.z[:], S.xt[:], ALPHA, S.psA[:], op0=ALU.mult, op1=ALU.add),
         reads=[S.xt, S.psA], writes=[S.z])
    yield
    layer_norm(c, S, xm[:], xm, S.z, S.lng[0], S.lng[1])
    yield
    c.op("act", lambda e: e.copy(xmb[:], xm[:]), reads=[xm], writes=[xmb])
    yield
    for k in range(8):
        c.op("pe", lambda e, k=k: e.matmul(S.psT[:, k * 128:(k + 1) * 128], xmb[:, k * 128:(k + 1) * 128], S.identb[:], start=True, stop=True),
             reads=[xmb, S.identb], writes=[S.psT])
        yield
    c.op("act", lambda e: e.copy(S.xmT[:].rearrange("p k t -> p (k t)"), S.psT[:]), reads=[S.psT], writes=[S.xmT])
    yield
    for h in range(8):
        for k in range(8):
            c.op("pe", lambda e, k=k, h=h: e.matmul(S.psA[:, h * 128:(h + 1) * 128], S.wq[:, k, h * 128:(h + 1) * 128],
                                                      S.xmT[:, k, :], start=(k == 0), stop=(k == 7)),
                 reads=[S.wq, S.xmT], writes=[S.psA])
            yield
    c.op("act", lambda e: e.copy(S.qT[:].rearrange("p k t -> p (k t)"), S.psA[:]), reads=[S.psA], writes=[S.qT])
    yield
    for hf in range(2):
        for j in range(8):
            hc = hf * 8 + j
            h, cc = hc // 2, hc % 2
            c.op("pe", lambda e, h=h, cc=cc, j=j: e.matmul(S.psS[:, j, :], S.qT[cc * 64:(cc + 1) * 64, h, :],
                                                            S.keysT[cc * 64:(cc + 1) * 64, h, :], start=True, stop=True),
                 reads=[S.qT, S.keysT], writes=[S.psS])
            yield
        for j in range(8):
            hc = hf * 8 + j
            c.op("dve", lambda e, hc=hc, j=j: e.max(S.tops[:, hc, 0:8], S.psS[:, j, :]), reads=[S.psS], writes=[S.tops])
            yield
            c.op("dve", lambda e, hc=hc, j=j: e.max_index(S.topi[:, hc, 0:8], S.tops[:, hc, 0:8], S.psS[:, j, :]),
                 reads=[S.psS, S.tops], writes=[S.topi])
            yield
            c.op("dve", lambda e, hc=hc, j=j: e.match_replace(S.ssc[:], S.tops[:, hc, 0:8], S.psS[:, j, :], NEG),
                 reads=[S.psS, S.tops], writes=[S.ssc])
            yield
            c.op("dve", lambda e, hc=hc: e.max(S.tops[:, hc, 8:16], S.ssc[:]), reads=[S.ssc], writes=[S.tops])
            yield
            c.op("dve", lambda e, hc=hc: e.max_index(S.topi[:, hc, 8:16], S.tops[:, hc, 8:16], S.ssc[:]),
                 reads=[S.ssc, S.tops], writes=[S.topi])
            yield
    c.op("dve", lambda e: e.tensor_copy(S.topf[:], S.topi[:]), reads=[S.topi], writes=[S.topf])
    yield
    tops4 = S.tops[:].rearrange("p (h c) k -> p h c k", c=2)
    topf4 = S.topf[:].rearrange("p (h c) k -> p h c k", c=2)
    c.op("dve", lambda e: e.tensor_scalar(topf4[:, :, 0, :], topf4[:, :, 0, :], 128.0, None, op0=ALU.mult),
         reads=[S.topf], writes=[S.topf])
    yield
    cs4 = S.cands[:].rearrange("p h (a b) -> p h a b", b=16)
    for h in range(8):
        c.op("pool", lambda e, h=h: e.tensor_tensor(cs4[:, h], tops4[:, h, 0, :].unsqueeze(2).to_broadcast([128, 16, 16]),
                                                     tops4[:, h, 1, :].unsqueeze(1).to_broadcast([128, 16, 16]), ALU.add),
             reads=[S.tops], writes=[S.cands])
        yield
    for h in range(8):
        c.op("dve", lambda e, h=h: e.max(S.best[:, h, 0:8], S.cands[:, h, :]), reads=[S.cands], writes=[S.best])
        yield
        c.op("dve", lambda e, h=h: e.max_index(S.pos[:, h, 0:8], S.best[:, h, 0:8], S.cands[:, h, :]), reads=[S.cands, S.best], writes=[S.pos])
        yield
        c.op("dve", lambda e, h=h: e.match_replace(S.candw[:], S.best[:, h, 0:8], S.cands[:, h, :], NEG),
             reads=[S.cands, S.best], writes=[S.candw])
        yield
        c.op("dve", lambda e, h=h: e.max(S.best[:, h, 8:16], S.candw[:]), reads=[S.candw], writes=[S.best])
        yield
        c.op("dve", lambda e, h=h: e.max_index(S.pos[:, h, 8:16], S.best[:, h, 8:16], S.candw[:]), reads=[S.candw, S.best], writes=[S.pos])
        yield
    posf = S.pos[:].rearrange("p h k -> p (h k)")
    c.op("dve", lambda e: e.tensor_scalar(S.pab[:, 0, :], posf, 4, None, op0=ALU.logical_shift_right), reads=[S.pos], writes=[S.pab])
    yield
    c.op("dve", lambda e: e.tensor_scalar(S.pab[:, 1, :], posf, 15, None, op0=ALU.bitwise_and), reads=[S.pos], writes=[S.pab])
    yield
    c.op("dve", lambda e: e.tensor_copy(S.pabf[:], S.pab[:]), reads=[S.pab], writes=[S.pabf])
    yield
    for ab in range(2):
        eq = S.eq[ab]
        c.op("dve", lambda e, ab=ab, eq=eq: e.tensor_tensor(eq[:], S.pabf[:, ab, :].unsqueeze(2).to_broadcast([128, 128, 16]),
                                                         S.iota16[:].unsqueeze(1).to_broadcast([128, 128, 16]), ALU.is_equal),
             reads=[S.pabf, S.iota16], writes=[eq])
        yield
        c.op("pool", lambda e, ab=ab, eq=eq: e.tensor_tensor(eq[:].rearrange("p (h k) a -> p h k a", h=8), eq[:].rearrange("p (h k) a -> p h k a", h=8),
                                                          topf4[:, :, ab, :].unsqueeze(2).to_broadcast([128, 8, 16, 16]), ALU.mult),
             reads=[S.topf, eq], writes=[eq])
        yield
        c.op("dve", lambda e, ab=ab, eq=eq: e.tensor_reduce(S.idx2[:, ab, :], eq[:], AX.X, ALU.add), reads=[eq], writes=[S.idx2])
        yield
    c.op("dve", lambda e: e.tensor_tensor(S.idxf[:], S.idx2[:, 0, :], S.idx2[:, 1, :], ALU.add), reads=[S.idx2], writes=[S.idxf])
    yield
    c.op("dve", lambda e: e.tensor_scalar(S.idxf[:], S.idxf[:], 16383.0, 0.0, op0=ALU.min, op1=ALU.max),
         reads=[S.idxf], writes=[S.idxf])
    yield
    c.op("dve", lambda e: e.tensor_copy(idxu[:], S.idxf[:]), reads=[S.idxf], writes=[idxu])
    yield
    c.op("dve", lambda e: e.tensor_tensor(gate[:], S.best[:], S.best[:, :, 0:1].to_broadcast([128, 8, 16]), ALU.subtract),
         reads=[S.best], writes=[gate])
    yield
    c.op("act", lambda e: e.activation(gate[:], gate[:], AF.Exp), reads=[gate], writes=[gate])
    yield
    c.op("dve", lambda e: e.tensor_reduce(S.gsum[:], gate[:], AX.X, ALU.add), reads=[gate], writes=[S.gsum])
    yield
    c.op("dve", lambda e: e.reciprocal(S.gsum[:], S.gsum[:]), reads=[S.gsum], writes=[S.gsum])
    yield
    c.op("dve", lambda e: e.tensor_tensor(gate[:], gate[:], S.gsum[:].unsqueeze(2).to_broadcast([128, 8, 16]), ALU.mult),
         reads=[gate, S.gsum], writes=[gate])
    yield
def tail_issue(c, S, par, k):
    G = S.G[(S.gi + k) % NG]
    c.gather(G[:], S.UV_s, S.idxu2[par][:, k:k + 1], reads=[S.idxu2[par], S.uv_tok], writes=[G])


def tail_back(c, S, par, out_dst, out_tok, fgen, nxt_par, prefetched):
    xm, xmb, idxu, gate = S.xm2[par], S.xmb2[par], S.idxu2[par], S.gate2[par]
    gate2 = gate[:].rearrange("p h k -> p (h k)")
    LOOK = NG - GS
    if not prefetched:
        for k in range(LOOK):
            tail_issue(c, S, par, k)
    issued = LOOK

    def pull(n):
        for _ in range(n):
            if next(fgen, "done") == "done":
                return
    for g0 in range(0, 128, GS):
        HH, CF = S.hh4[(g0 // GS) % 4], S.coef4[(g0 // GS) % 4]
        for k in range(g0, g0 + GS):
            G = S.G[(S.gi + k) % NG]
            JK = S.junk2[k % 2]
            c.op("dve", lambda e, G=G, k=k, JK=JK, HH=HH: e.scalar_tensor_tensor(JK[:], G[:, 0:1024], 1.0, xmb[:], op0=ALU.mult, op1=ALU.mult,
                                                                              accum_out=HH[:, k - g0:k - g0 + 1]),
                 reads=[G, xmb], writes=[JK, HH])
            pull(PULLD)
        c.op("act", lambda e, HH=HH, CF=CF: e.activation(CF[:], HH[:], AF.Gelu), reads=[HH], writes=[CF])
        c.op("dve", lambda e, g0=g0, CF=CF: e.tensor_tensor(CF[:], CF[:], gate2[:, g0:g0 + GS], ALU.mult),
             reads=[CF, gate], writes=[CF])
        for k in range(g0, g0 + GS):
            G = S.G[(S.gi + k) % NG]
            DG = S.dg[k % 4]
            c.op("act", lambda e, DG=DG, k=k, CF=CF: e.activation(DG[:], S.identb[:], AF.Copy, scale=CF[:, k - g0:k - g0 + 1]), reads=[S.identb, CF], writes=[DG])
            for half in range(2):
                c.op("pe", lambda e, DG=DG, G=G, half=half, k=k: e.matmul(S.psAcc[:, half * 512:(half + 1) * 512], DG[:],
                                                                          G[:, 1024 + half * 512:1536 + half * 512], start=(k == 0), stop=(k == 127)),
                     reads=[DG, G], writes=[S.psAcc])
            if issued < 128:
                tail_issue(c, S, par, issued)
                issued += 1
            pull(PULLN)
    pull(100000)
    S.gi += 128
    if nxt_par is not None:
        for k in range(LOOK):
            tail_issue(c, S, nxt_par, k)
    c.op("dve", lambda e: e.scalar_tensor_tensor(S.acc[:], xm[:], ALPHA, S.psAcc[:], op0=ALU.mult, op1=ALU.add),
         reads=[xm, S.psAcc], writes=[S.acc])
    layer_norm(c, S, S.ot[:], S.ot, S.acc, S.lng[2], S.lng[3])
    c.dma("sp", out_dst, S.ot[:], reads=[S.ot], writes=[out_tok], wtok=out_tok)


WSPEC = {
    "even_w_in": [2, 1024, 3248], "gla_a_w2": [2, 16, 256], "gla_a_b": [2, 256], "gla_norm_g": [2, 128],
    "rwkv_mu": [2, 1696], "rwkv_w0": [2, 512], "rwkv_w2": [2, 32, 512], "rwkv_a0": [2, 512],
    "rwkv_a2": [2, 32, 512], "rwkv_g2": [2, 96, 512], "rwkv_k_k": [2, 512], "rwkv_k_a": [2, 512],
    "rwkv_r_k": [2, 8, 64], "rwkv_lnx_g": [2, 512], "rwkv_lnx_b": [2, 512], "even_w_out": [2, 1024, 1024],
    "odd_w_in": [2, 1024, 1864], "odd_w_out": [2, 1024, 1024], "mix_ln_g": [4, 1024], "mix_ln_b": [4, 1024],
    "peer_w_q": [4, 1024, 1024], "peer_sub_keys": [4, 8, 2, 128, 64], "peer_u": [4, 16384, 1024],
    "peer_v": [4, 16384, 1024], "ffn_ln_g": [4, 1024], "ffn_ln_b": [4, 1024],
}


def consts():
    cs = {}
    cs["identb"] = np.eye(128, dtype=np.float32).astype(ml_dtypes.bfloat16)
    cs["identf"] = np.eye(128, dtype=np.float32)
    cs["iota16"] = np.tile(np.arange(16, dtype=np.float32)[None, :], (128, 1))
    cs.update(rope_consts())
    cs.update(even_consts())
    return cs


def shared_alloc(c, S, A):
    S.wstage = [c.sb("wst%d" % i, [128, 1024]) for i in range(2)]
    S.wsi = 0
    S.identb = c.sb("identb", [128, 128], BF16)
    S.identf = c.sb("identf", [128, 128])
    S.ln_st = c.sb("ln_st", [128, 2, 6])
    S.ln_mv = c.sb("ln_mv", [128, 2])
    S.ln_sd = c.sb("ln_sd", [128, 4])
    c.dma("sp", S.identb[:], A["identb"], writes=[S.identb])
    c.dma("sp", S.identf[:], A["identf"], writes=[S.identf])
    S.gi = 0


def build(NB=4, layers=(0, 1, 2, 3), mode="full"):
    nc = bass.Bass("TRN2", target_bir_lowering=False)
    NT = NB * T
    ntiles = NT // 128
    A = {}
    A["x"] = nc.dram_tensor("x", [NT, D], F32, kind="ExternalInput").ap()
    for k, shp in WSPEC.items():
        A[k] = nc.dram_tensor(k, shp, F32, kind="ExternalInput").ap()
    for k, v in consts().items():
        A[k] = nc.dram_tensor(k, list(v.shape), BF16 if v.dtype == ml_dtypes.bfloat16 else F32, kind="ExternalInput").ap()
    y = nc.dram_tensor("y", [NT, D], F32, kind="ExternalOutput").ap()
    if mode == "mixer":
        oT = nc.dram_tensor("oT_out", [ntiles, 128, 1024], BF16, kind="ExternalOutput").ap()
    elif mode == "tail":
        oT = nc.dram_tensor("oT_in", [ntiles, 128, 1024], BF16, kind="ExternalInput").ap()
    else:
        oT = nc.dram_tensor("oT_s", [ntiles, 128, 1024], BF16, kind="Internal").ap()
    xs = nc.dram_tensor("xs_s", [NT, D], F32, kind="Internal").ap()
    PR_s = nc.dram_tensor("PR_s", [NB, 14, 128, T], BF16, kind="Internal").ap()
    V_s = nc.dram_tensor("V_s", [NB, 128, 16 * 128], BF16, kind="Internal").ap()
    WI_s = nc.dram_tensor("WI_s", [NB, 128, 16 * 8], F32, kind="Internal").ap()
    UV_s = nc.dram_tensor("UV_s", [16384, 2048], BF16, kind="Internal").ap()
    EF_s = nc.dram_tensor("EF_s", [NB, 19, 128, T], F32, kind="Internal").ap()
    GV_s = nc.dram_tensor("GV_s", [NB, 16, 128, 512], BF16, kind="Internal").ap()
    GG_s = nc.dram_tensor("GG_s", [NB, 16, 128, 512], F32, kind="Internal").ap()
    es = ExitStack()
    with es:
        c = Ctx(nc, es)
        S = Shared()
        shared_alloc(c, S, A)
        S.xs_tok = c.tok("xs_tok")
        S.oT_tok = c.tok("oT_tok")
        S.y_tok = c.tok("y_tok")
        S.pr_tok = c.tok("pr_tok")
        S.PR_s, S.V_s, S.WI_s = PR_s, V_s, WI_s
        S.EF_s, S.GV_s, S.GG_s = EF_s, GV_s, GG_s
        S.UV_s = UV_s
        S.uv_tok = c.tok("uv_tok")
        for li, layer in enumerate(layers):
            x_in = A["x"] if li == 0 else xs
            last = (li == len(layers) - 1)
            if mode != "tail":
                with ExitStack() as es2:
                    c.es = es2
                    if layer % 2 == 0:
                        even_stage(c, S, A, layer, x_in, oT, NB)
                    else:
                        odd_stage(c, S, A, layer, x_in, oT, NB)
                    c.barrier()
            if mode == "mixer":
                continue
            uv_prep(c, S, A, layer)
            with ExitStack() as es2:
                c.es = es2
                tail_alloc(c, S)
                tail_weights(c, S, A, layer)
                dst = y if last else xs

                def mkfront(g):
                    return tail_front(c, S, A, layer, x_in[g * 128:(g + 1) * 128, :], oT[g], g % 2)

                for _ in mkfront(0):
                    pass
                for g in range(ntiles):
                    fgen = mkfront(g + 1) if g + 1 < ntiles else iter(())
                    tail_back(c, S, g % 2, dst[g * 128:(g + 1) * 128, :], S.y_tok if last else S.xs_tok, fgen,
                              (g + 1) % 2 if g + 1 < ntiles else None, g > 0)
                c.barrier()
            c.es = es
        c.finish([S.y_tok, S.xs_tok, S.oT_tok], "sp")
        c.barrier()
        print("ninst", c.ninst, "nsem", c.nsem)
    return nc


def rope_consts():
    def tabs(dim, reps):
        inv = (10000.0 ** (-np.arange(0, dim, 2, dtype=np.float32) / dim)).astype(np.float32)
        ang = np.arange(T, dtype=np.float32)[None, :] * inv[:, None]
        co, si = np.cos(ang).astype(np.float32), np.sin(ang).astype(np.float32)
        co = np.concatenate([co, co], 0)
        si = np.concatenate([si, si], 0)
        return np.ascontiguousarray(np.tile(co, (reps, 1))), np.ascontiguousarray(np.tile(si, (reps, 1)))
    cA, sA = tabs(128, 1)
    cI, sI = tabs(64, 2)
    tri = np.where(np.arange(128)[None, :] <= np.arange(128)[:, None], 0.0, NEG).astype(np.float32)
    return {"ropetab": np.ascontiguousarray(np.stack([cA, sA, cI, sI], 0)), "trineg": tri,
            "onesb": np.ones((128, 128), np.float32).astype(ml_dtypes.bfloat16)}


def odd_stage(c, S, A, layer, x_in, oT, NB):
    nc = c.nc
    i = layer // 2
    PR = S.PR_s
    VS = S.V_s
    WS = S.WI_s
    with ExitStack() as esA:
        c.es = esA
        win = c.sb("o_win", [128, 8, 1864], BF16)
        wrot = c.sb("o_wrot", [128, 8, 1792], BF16)
        wki2 = c.sb("o_wki2", [128, 8, 128], BF16)
        load_w_bf16(c, S, win, A["odd_w_in"][i], 8, 1864)
        for k in range(8):
            for (s0, d0, n, half) in ((0, 0, 1152, 64), (1280, 1152, 512, 32)):
                src = win[:, k, s0:s0 + n].rearrange("p (h two j) -> p h two j", two=2, j=half)
                dst = wrot[:, k, d0:d0 + n].rearrange("p (h two j) -> p h two j", two=2, j=half)
                c.op("dve", lambda e, src=src, dst=dst: e.tensor_scalar(dst[:, :, 0, :], src[:, :, 1, :], -1.0, None, op0=ALU.mult),
                     reads=[win], writes=[wrot])
                c.op("pool", lambda e, src=src, dst=dst: e.tensor_copy(dst[:, :, 1, :], src[:, :, 0, :]), reads=[win], writes=[wrot])
            for hlf in range(2):
                c.op("pool", lambda e, k=k, hlf=hlf: e.tensor_copy(wki2[:, k, hlf * 64:(hlf + 1) * 64], win[:, k, 1792:1856]),
                     reads=[win], writes=[wki2])
                c.op("dve", lambda e, k=k, hlf=hlf: e.tensor_scalar(wrot[:, k, 1664 + hlf * 64:1696 + hlf * 64], win[:, k, 1824:1856], -1.0, None, op0=ALU.mult),
                     reads=[win], writes=[wrot])
                c.op("pool", lambda e, k=k, hlf=hlf: e.tensor_copy(wrot[:, k, 1696 + hlf * 64:1728 + hlf * 64], win[:, k, 1792:1824]),
                     reads=[win], writes=[wrot])
        groups = []
        for h in range(8):
            groups.append((win, h * 128, wrot, h * 128, 0))
        groups.append((win, 1024, wrot, 1024, 0))
        for g in range(4):
            groups.append((win, 1280 + g * 128, wrot, 1152 + g * 128, 1))
        groups.append((wki2, 0, wrot, 1664, 1))
        xt = [c.sb("o_xt%d" % j, [128, 1024]) for j in range(2)]
        xb = [c.sb("o_xb%d" % j, [128, 1024], BF16) for j in range(2)]
        xTb = c.sb("o_xTb", [128, 8, 512], BF16)
        tabs = c.sb("o_tabs", [128, 4, 512])
        t1 = [c.sb("o_t1%d" % j, [128, 512]) for j in range(2)]
        t2 = [c.sb("o_t2%d" % j, [128, 512]) for j in range(2)]
        stg = [c.sb("o_stg%d" % j, [128, 512], BF16) for j in range(3)]
        vt = [c.sb("o_vt%d" % j, [128, 128], BF16) for j in range(2)]
        wt = [c.sb("o_wt%d" % j, [128, 8]) for j in range(2)]
        psQ = [c.ps("o_psQ%d" % j, [128, 512]) for j in range(2)]
        psR = [c.ps("o_psR%d" % j, [128, 512]) for j in range(2)]
        psT = c.ps("o_psT", [128, 1024], BF16)
        psV = c.ps("o_psV", [128, 512])
        cnt = 0
        for b in range(NB):
            for tb in range(4):
                c.dma("sp", tabs[:], A["ropetab"][:, :, tb * 512:(tb + 1) * 512].rearrange("f p t -> p f t"), writes=[tabs])
                for tt in range(4):
                    g = b * 16 + tb * 4 + tt
                    X, XB = xt[tt % 2], xb[tt % 2]
                    c.dma(c.qsel(), X[:], x_in[g * 128:(g + 1) * 128, :], writes=[X], reads=[S.xs_tok])
                    c.op("act", lambda e, X=X, XB=XB: e.copy(XB[:], X[:]), reads=[X], writes=[XB])
                    for k in range(8):
                        c.op("pe", lambda e, k=k, XB=XB: e.transpose(psT[:, k * 128:(k + 1) * 128], XB[:, k * 128:(k + 1) * 128], S.identb[:]),
                             reads=[XB, S.identb], writes=[psT])
                    c.op("dve", lambda e, tt=tt: e.tensor_copy(xTb[:, :, tt * 128:(tt + 1) * 128], psT[:].rearrange("p (k t) -> p k t", t=128)),
                         reads=[psT], writes=[xTb])
                for tt in range(4):
                    g = tb * 4 + tt
                    for k in range(8):
                        c.op("pe", lambda e, k=k, tt=tt: e.matmul(psV[:, 0:128], xTb[:, k, tt * 128:(tt + 1) * 128], win[:, k, 1152:1280],
                                                                    start=(k == 0), stop=(k == 7)), reads=[xTb, win], writes=[psV])
                    for k in range(8):
                        c.op("pe", lambda e, k=k, tt=tt: e.matmul(psV[:, 128:136], xTb[:, k, tt * 128:(tt + 1) * 128], win[:, k, 1856:1864],
                                                                    start=(k == 0), stop=(k == 7)), reads=[xTb, win], writes=[psV])
                    V, W = vt[tt % 2], wt[tt % 2]
                    c.op("act", lambda e, V=V: e.copy(V[:], psV[:, 0:128]), reads=[psV], writes=[V])
                    c.op("act", lambda e, W=W: e.copy(W[:], psV[:, 128:136]), reads=[psV], writes=[W])
                    c.dma("sp", VS[b, :, g * 128:(g + 1) * 128], V[:], reads=[V], writes=[S.pr_tok], wtok=V)
                    c.dma("sp", WS[b, :, g * 8:(g + 1) * 8], W[:], reads=[W], writes=[S.pr_tok], wtok=W)
                for gi, (w0, c0, w1, c1, tab) in enumerate(groups):
                    pq, pr = psQ[cnt % 2], psR[cnt % 2]
                    a1, a2, sg = t1[cnt % 2], t2[cnt % 2], stg[cnt % 3]
                    cnt += 1
                    for k in range(8):
                        c.op("pe", lambda e, k=k, w0=w0, c0=c0, pq=pq: e.matmul(pq[:], w0[:, k, c0:c0 + 128], xTb[:, k, :], start=(k == 0), stop=(k == 7)),
                             reads=[w0, xTb], writes=[pq])
                    for k in range(8):
                        c.op("pe", lambda e, k=k, w1=w1, c1=c1, pr=pr: e.matmul(pr[:], w1[:, k, c1:c1 + 128], xTb[:, k, :], start=(k == 0), stop=(k == 7)),
                             reads=[w1, xTb], writes=[pr])
                    c.op("dve", lambda e, pq=pq, a1=a1, tab=tab: e.tensor_tensor(a1[:], pq[:], tabs[:, 2 * tab, :], ALU.mult), reads=[pq, tabs], writes=[a1])
                    c.op("dve", lambda e, pr=pr, a2=a2, tab=tab: e.tensor_tensor(a2[:], pr[:], tabs[:, 2 * tab + 1, :], ALU.mult), reads=[pr, tabs], writes=[a2])
                    c.op("pool", lambda e, a1=a1, a2=a2, sg=sg: e.tensor_tensor(sg[:], a1[:], a2[:], ALU.add), reads=[a1, a2], writes=[sg])
                    c.dma(c.qsel(), PR[b, gi, :, tb * 512:(tb + 1) * 512], sg[:], reads=[sg], writes=[S.pr_tok], wtok=sg)
        c.barrier()
    with ExitStack() as esB:
        c.es = esB
        qT = c.sb("o_qT", [128, 8, T], BF16)
        kT = c.sb("o_kT", [128, T], BF16)
        qiT = c.sb("o_qiT", [128, 4, T], BF16)
        kiT = c.sb("o_kiT", [128, T], BF16)
        vtk = c.sb("o_vtk", [128, 16, 128], BF16)
        wi = c.sb("o_wi", [128, 16, 8])
        sc = c.sb("o_sc", [128, T])
        work = c.sb("o_work", [128, T])
        tmpr = [c.sb("o_tmpr%d" % j, [128, 1024]) for j in range(2)]
        m8 = c.sb("o_m8", [128, 8])
        thr0 = c.sb("o_thr0", [128, 1])
        maskf = c.sb("o_maskf", [128, T], BF16)
        maskT = c.sb("o_maskT", [128, 16, 128], BF16)
        E = [c.sb("o_E%d" % j, [128, 1024], BF16) for j in range(2)]
        PT = [c.sb("o_PT%d" % j, [128, 1024], BF16) for j in range(2)]
        rden = c.sb("o_rden", [128, 1024])
        oTt = [c.sb("o_oTt%d" % j, [128, 1024], BF16) for j in range(2)]
        trineg = c.sb("o_tri", [128, 128])
        onesb = c.sb("o_ones", [128, 128], BF16)
        psI = c.ps("o_psI", [128, 1024])
        psL = c.ps("o_psL", [128, 1024])
        psO = c.ps("o_psO", [128, 1024])
        psD = c.ps("o_psD", [128, 1024])
        psDb = psD[:].bitcast(BF16)
        c.dma("sp", trineg[:], A["trineg"], writes=[trineg])
        c.dma("sp", onesb[:], A["onesb"], writes=[onesb])
        c.op("pool", lambda e: e.memset(thr0[:], -1e29), writes=[thr0])
        SCALE = float(128 ** -0.5)
        for b in range(NB):
            for h in range(8):
                c.dma(c.qsel(), qT[:, h, :], PR[b, h], writes=[qT], reads=[S.pr_tok])
            c.dma(c.qsel(), kT[:], PR[b, 8], writes=[kT], reads=[S.pr_tok])
            for g in range(4):
                c.dma(c.qsel(), qiT[:, g, :], PR[b, 9 + g], writes=[qiT], reads=[S.pr_tok])
            c.dma(c.qsel(), kiT[:], PR[b, 13], writes=[kiT], reads=[S.pr_tok])
            c.dma(c.qsel(), vtk[:].rearrange("p g d -> p (g d)"), VS[b], writes=[vtk], reads=[S.pr_tok])
            c.dma(c.qsel(), wi[:].rearrange("p g d -> p (g d)"), WS[b], writes=[wi], reads=[S.pr_tok])
            for qt in range(16):
                Sk = (qt + 1) * 128
                q0 = qt * 128
                for h in range(8):
                    hh, g = h % 2, h // 2
                    for blk in range((Sk + 1023) // 1024):
                        k0 = blk * 1024
                        n = min(1024, Sk - k0)
                        for sub in range((n + 511) // 512):
                            s0 = k0 + sub * 512
                            m = min(512, Sk - s0)
                            c.op("pe", lambda e, hh=hh, g=g, s0=s0, m=m, sub=sub: e.matmul(
                                psI[:, sub * 512:sub * 512 + m], qiT[hh * 64:(hh + 1) * 64, g, q0:q0 + 128],
                                kiT[hh * 64:(hh + 1) * 64, s0:s0 + m], start=True, stop=True),
                                reads=[qiT, kiT], writes=[psI])
                        if h == 0:
                            c.op("dve", lambda e, k0=k0, n=n, h=h: e.tensor_scalar(sc[:, k0:k0 + n], psI[:, 0:n], 0.0, wi[:, qt, h:h + 1], op0=ALU.max, op1=ALU.mult),
                                 reads=[psI, wi], writes=[sc])
                        else:
                            tr = tmpr[(h + blk) % 2]
                            c.op("dve", lambda e, k0=k0, n=n, h=h, tr=tr: e.tensor_scalar(tr[:, 0:n], psI[:, 0:n], 0.0, wi[:, qt, h:h + 1], op0=ALU.max, op1=ALU.mult),
                                 reads=[psI, wi], writes=[tr])
                            c.op("pool", lambda e, k0=k0, n=n, tr=tr: e.tensor_tensor(sc[:, k0:k0 + n], sc[:, k0:k0 + n], tr[:, 0:n], ALU.add),
                                 reads=[tr, sc], writes=[sc])
                c.op("pool", lambda e: e.tensor_tensor(sc[:, q0:q0 + 128], sc[:, q0:q0 + 128], trineg[:], ALU.add), reads=[sc, trineg], writes=[sc])
                if Sk <= 256:
                    thr_ap, thr_tok = thr0[:, 0:1], thr0
                else:
                    c.op("dve", lambda e: e.max(m8[:], sc[:, 0:Sk]), reads=[sc], writes=[m8])
                    c.op("dve", lambda e: e.match_replace(work[:, 0:Sk], m8[:], sc[:, 0:Sk], NEG), reads=[sc, m8], writes=[work])
                    for r in range(1, 32):
                        c.op("dve", lambda e: e.max(m8[:], work[:, 0:Sk]), reads=[work], writes=[m8])
                        if r < 31:
                            c.op("dve", lambda e: e.match_replace(work[:, 0:Sk], m8[:], work[:, 0:Sk], NEG), reads=[work, m8], writes=[work])
                    thr_ap, thr_tok = m8[:, 7:8], m8
                c.op("dve", lambda e, thr_ap=thr_ap: e.tensor_scalar(maskf[:, 0:Sk], sc[:, 0:Sk], thr_ap, None, op0=ALU.is_ge),
                     reads=[sc, thr_tok], writes=[maskf])
                for cb in range((qt + 8) // 8):
                    c0 = cb * 8
                    nch = min(8, qt + 1 - c0)
                    for j in range(nch):
                        c.op("pe", lambda e, j=j, c0=c0: e.transpose(psDb[:, j * 128:(j + 1) * 128], maskf[:, (c0 + j) * 128:(c0 + j + 1) * 128], S.identb[:]),
                             reads=[maskf, S.identb], writes=[psD])
                    c.op("act", lambda e, c0=c0, nch=nch: e.copy(maskT[:, c0:c0 + nch, :].rearrange("p c t -> p (c t)"), psDb[:, 0:nch * 128]),
                         reads=[psD], writes=[maskT])
                for ch in range(qt + 1):
                    Ej, Pj = E[ch % 2], PT[ch % 2]
                    for hf in range(2):
                        c.op("pe", lambda e, ch=ch, hf=hf: e.matmul(psL[:, hf * 512:(hf + 1) * 512], kT[:, ch * 128:(ch + 1) * 128],
                                                                      qT[:, hf * 4:(hf + 1) * 4, q0:q0 + 128], start=True, stop=True),
                             reads=[kT, qT], writes=[psL])
                    c.op("act", lambda e, Ej=Ej: e.activation(Ej[:], psL[:], AF.Exp, scale=SCALE), reads=[psL], writes=[Ej])
                    c.op("pool", lambda e, Ej=Ej, Pj=Pj, ch=ch: e.tensor_tensor(Pj[:].rearrange("p (h t) -> p h t", t=128), Ej[:].rearrange("p (h t) -> p h t", t=128),
                                                                               maskT[:, ch, :].unsqueeze(1).to_broadcast([128, 8, 128]), ALU.mult),
                         reads=[Ej, maskT], writes=[Pj])
                    for hf in range(2):
                        c.op("pe", lambda e, ch=ch, hf=hf, Pj=Pj: e.matmul(psO[:, hf * 512:(hf + 1) * 512], vtk[:, ch, :], Pj[:, hf * 512:(hf + 1) * 512],
                                                                             start=(ch == 0), stop=(ch == qt)), reads=[vtk, Pj], writes=[psO])
                        c.op("pe", lambda e, ch=ch, hf=hf, Pj=Pj: e.matmul(psD[:, hf * 512:(hf + 1) * 512], onesb[:], Pj[:, hf * 512:(hf + 1) * 512],
                                                                             start=(ch == 0), stop=(ch == qt)), reads=[onesb, Pj], writes=[psD])
                c.op("dve", lambda e: e.reciprocal(rden[:], psD[:]), reads=[psD], writes=[rden])
                O = oTt[qt % 2]
                c.op("dve", lambda e, O=O: e.tensor_tensor(O[:], psO[:], rden[:], ALU.mult), reads=[psO, rden], writes=[O])
                c.dma("sp", oT[b * 16 + qt], O[:], reads=[O], writes=[S.oT_tok], wtok=O)
        c.barrier()


def even_consts():
    j = np.arange(128)[:, None]
    i_ = np.arange(128)[None, :]
    bc = ((j // 64 == i_ // 64) & (j <= i_)).astype(np.float32)
    bo = (j // 64 == i_ // 64).astype(np.float32)
    ah = np.zeros((128, 255), np.float32)
    ah[:, 127] = 1.0
    return {"bcmask": bc, "blockones": bo, "blockonesb": bo.astype(ml_dtypes.bfloat16), "ahwin": ah.astype(ml_dtypes.bfloat16)}


EV_GROUPS = ([(c0, 128, False, 0) for c0 in (0, 128, 256, 384)] + [(1536, 16, False, 0)] +
             [(1552 + p * 128, 128, True, p * 128) for p in range(4)] +
             [(2064 + p * 128, 128, True, 512 + p * 128) for p in range(4)] +
             [(2576 + p * 128, 128, True, 1024 + p * 128) for p in range(4)] +
             [(3088, 64, True, 1536), (3152, 96, True, 1600)])


def even_phaseA(c, S, A, layer, x_in, NB):
    i = layer // 2
    EF, GV, GG = S.EF_s, S.GV_s, S.GG_s
    with ExitStack() as esA:
        c.es = esA
        win = c.sb("e_win", [128, 8, 3248], BF16)
        load_w_bf16(c, S, win, A["even_w_in"][i], 8, 3248)
        mu = c.sb("e_mu", [128, 19])
        for gi, (c0, n, sh, rc) in enumerate(EV_GROUPS):
            if sh:
                c.dma(c.qsel(), mu[0:n, gi:gi + 1], A["rwkv_mu"][i:i + 1, rc:rc + n].rearrange("o n -> n o"), writes=[mu])
        xt = [c.sb("e_xt%d" % j, [128, 1024]) for j in range(2)]
        xb = [c.sb("e_xb%d" % j, [128, 1024], BF16) for j in range(2)]
        xT = c.sb("e_xT", [128, 8, 513], BF16)
        t1 = [c.sb("e_t1%d" % j, [128, 512]) for j in range(2)]
        t2 = [c.sb("e_t2%d" % j, [128, 512]) for j in range(2)]
        stg = [c.sb("e_stg%d" % j, [128, 512]) for j in range(3)]
        vst = [c.sb("e_vst%d" % j, [128, 512], BF16) for j in range(2)]
        gst = [c.sb("e_gst%d" % j, [128, 512]) for j in range(2)]
        psQ = [c.ps("e_psQ%d" % j, [128, 512]) for j in range(2)]
        psR = [c.ps("e_psR%d" % j, [128, 512]) for j in range(2)]
        psT = c.ps("e_psT", [128, 1024], BF16)
        psV = c.ps("e_psV", [128, 1024])
        cnt = 0
        for b in range(NB):
            c.op("pool", lambda e: e.memset(xT[:, :, 0:1], 0.0), writes=[xT])
            for tb in range(4):
                if tb > 0:
                    c.op("pool", lambda e: e.tensor_copy(xT[:, :, 0:1], xT[:, :, 512:513]), reads=[xT], writes=[xT])
                for tt in range(4):
                    g = b * 16 + tb * 4 + tt
                    X, XB = xt[tt % 2], xb[tt % 2]
                    c.dma(c.qsel(), X[:], x_in[g * 128:(g + 1) * 128, :], writes=[X], reads=[S.xs_tok])
                    c.op("act", lambda e, X=X, XB=XB: e.copy(XB[:], X[:]), reads=[X], writes=[XB])
                    for k in range(8):
                        c.op("pe", lambda e, k=k, XB=XB: e.transpose(psT[:, k * 128:(k + 1) * 128], XB[:, k * 128:(k + 1) * 128], S.identb[:]),
                             reads=[XB, S.identb], writes=[psT])
                    c.op("dve", lambda e, tt=tt: e.tensor_copy(xT[:, :, 1 + tt * 128:1 + (tt + 1) * 128], psT[:].rearrange("p (k t) -> p k t", t=128)),
                         reads=[psT], writes=[xT])
                for tt in range(4):
                    g = tb * 4 + tt
                    for hf, c0 in enumerate((512, 1024)):
                        for k in range(8):
                            c.op("pe", lambda e, k=k, tt=tt, hf=hf, c0=c0: e.matmul(psV[:, hf * 512:(hf + 1) * 512], xT[:, k, 1 + tt * 128:1 + (tt + 1) * 128],
                                                                                      win[:, k, c0:c0 + 512], start=(k == 0), stop=(k == 7)),
                                 reads=[xT, win], writes=[psV])
                    V, G = vst[tt % 2], gst[tt % 2]
                    c.op("dve", lambda e, V=V: e.tensor_copy(V[:], psV[:, 0:512]), reads=[psV], writes=[V])
                    c.op("act", lambda e, G=G: e.activation(G[:], psV[:, 512:1024], AF.Silu), reads=[psV], writes=[G])
                    c.dma("sp", GV[b, g], V[:], reads=[V], writes=[S.pr_tok], wtok=V)
                    c.dma("sp", GG[b, g], G[:], reads=[G], writes=[S.pr_tok], wtok=G)
                for gi, (c0, n, sh, rc) in enumerate(EV_GROUPS):
                    pq, pr = psQ[cnt % 2], psR[cnt % 2]
                    a1, a2, sg = t1[cnt % 2], t2[cnt % 2], stg[cnt % 3]
                    cnt += 1
                    for k in range(8):
                        c.op("pe", lambda e, k=k, c0=c0, n=n, pq=pq: e.matmul(pq[0:n, :], win[:, k, c0:c0 + n], xT[:, k, 1:513], start=(k == 0), stop=(k == 7)),
                             reads=[win, xT], writes=[pq])
                    if not sh:
                        c.op("act", lambda e, pq=pq, sg=sg, n=n: e.copy(sg[0:n, :], pq[0:n, :]), reads=[pq], writes=[sg])
                    else:
                        for k in range(8):
                            c.op("pe", lambda e, k=k, c0=c0, n=n, pr=pr: e.matmul(pr[0:n, :], win[:, k, c0:c0 + n], xT[:, k, 0:512], start=(k == 0), stop=(k == 7)),
                                 reads=[win, xT], writes=[pr])
                        c.op("act", lambda e, pq=pq, a1=a1, n=n: e.copy(a1[0:n, :], pq[0:n, :]), reads=[pq], writes=[a1])
                        c.op("dve", lambda e, pr=pr, a1=a1, a2=a2, n=n: e.tensor_tensor(a2[0:n, :], pr[0:n, :], a1[0:n, :], ALU.subtract), reads=[pr, a1], writes=[a2])
                        c.op("dve", lambda e, a1=a1, a2=a2, sg=sg, n=n, gi=gi: e.scalar_tensor_tensor(sg[0:n, :], a2[0:n, :], mu[0:n, gi:gi + 1], a1[0:n, :], op0=ALU.mult, op1=ALU.add),
                             reads=[a1, a2, mu], writes=[sg])
                    c.dma(c.qsel(), EF[b, gi, 0:n, tb * 512:(tb + 1) * 512], sg[0:n, :], reads=[sg], writes=[S.pr_tok], wtok=sg)
        c.barrier()


def gla_phase(c, S, A, layer, oT, NB):
    i = layer // 2
    EF, GV, GG = S.EF_s, S.GV_s, S.GG_s
    with ExitStack() as esB:
        c.es = esB
        aw2 = c.sb("g_aw2", [16, 256])
        nab = c.sb("g_nab", [128, 2])
        ngb = c.sb("g_ngb", [128, 128])
        msk = c.sb("g_msk", [128, T])
        bcm = c.sb("g_bcm", [128, 128])
        qf = c.sb("g_qf", [128, T])
        kf = c.sb("g_kf", [128, T])
        gal = c.sb("g_gal", [16, T])
        cum = c.sb("g_cum", [128, T])
        ex = c.sb("g_ex", [128, T])
        dec = c.sb("g_dec", [128, 32])
        qz = c.sb("g_qz", [128, 16, 2, 128], BF16)
        kin = c.sb("g_kin", [128, T], BF16)
        kout = c.sb("g_kout", [128, T], BF16)
        ktok = [c.sb("g_ktok%d" % j, [128, 128], BF16) for j in range(2)]
        vt = [c.sb("g_vt%d" % j, [128, 512], BF16) for j in range(2)]
        gg = [c.sb("g_gg%d" % j, [128, 512]) for j in range(2)]
        At = [c.sb("g_At%d" % j, [128, 128], BF16) for j in range(2)]
        Sf = [c.sb("g_Sf%d" % j, [128, 128]) for j in range(2)]
        Sb = [c.sb("g_Sb%d" % j, [128, 128], BF16) for j in range(3)]
        osball = c.sb("g_osb", [128, 16, 512])
        ss = c.sb("g_ss", [128, 8])
        junk = c.sb("g_junk", [128, 128])
        gob = c.sb("g_gob", [128, 512], BF16)
        oTt = [c.sb("g_oTt%d" % j, [128, 512], BF16) for j in range(2)]
        psZ = c.ps("g_psZ", [128, 512])
        psA = c.ps("g_psA", [128, 128])
        psK = c.ps("g_psK", [128, 128], BF16)
        psKV = c.ps("g_psKV", [128, 256])
        psO = [c.ps("g_psO%d" % j, [128, 128]) for j in range(2)]
        psTt = c.ps("g_psT", [128, 512], BF16)
        c.dma("sp", aw2[:], A["gla_a_w2"][i], writes=[aw2])
        for p in range(2):
            c.dma("sp", nab[:, p:p + 1], A["gla_a_b"][i:i + 1, p * 128:(p + 1) * 128].rearrange("o n -> n o"), writes=[nab])
        c.op("dve", lambda e: e.tensor_scalar(nab[:], nab[:], -1.0, None, op0=ALU.mult), reads=[nab], writes=[nab])
        bcast_row(c, ngb, A["gla_norm_g"][i:i + 1, :], 128)
        c.dma("sp", bcm[:], A["bcmask"], writes=[bcm])
        c.op("pool", lambda e: e.memset(msk[:], 1.0), writes=[msk])
        c.op("pool", lambda e: e.memset(msk[:].rearrange("p (n c) -> p n c", c=64)[:, :, 0:1], 0.0), writes=[msk])
        c.op("pool", lambda e: e.memset(qz[:].rearrange("p a b c -> p (a b c)"), 0.0), writes=[qz])
        sbi = 0
        for b in range(NB):
            c.dma(c.qsel(), gal[:], EF[b, 4, 0:16, :], writes=[gal], reads=[S.pr_tok])
            for p in range(2):
                c.dma(c.qsel(), qf[:], EF[b, p], writes=[qf], reads=[S.pr_tok])
                c.dma(c.qsel(), kf[:], EF[b, 2 + p], writes=[kf], reads=[S.pr_tok])
                for tb in range(4):
                    c.op("pe", lambda e, tb=tb, p=p: e.matmul(psZ[:], aw2[:, p * 128:(p + 1) * 128], gal[:, tb * 512:(tb + 1) * 512], start=True, stop=True),
                         reads=[aw2, gal], writes=[psZ])
                    c.op("act", lambda e, tb=tb, p=p: e.activation(ex[:, tb * 512:(tb + 1) * 512], psZ[:], AF.Exp, scale=-1.0, bias=nab[:, p:p + 1]),
                         reads=[psZ, nab], writes=[ex])
                c.op("act", lambda e: e.activation(ex[:], ex[:], AF.Ln, bias=1.0), reads=[ex], writes=[ex])
                c.op("dve", lambda e: e.tensor_scalar(ex[:], ex[:], -1.0 / 16.0, None, op0=ALU.mult), reads=[ex], writes=[ex])
                c.op("dve", lambda e: e.tensor_tensor_scan(cum[:], msk[:], ex[:], 0.0, ALU.mult, ALU.add), reads=[msk, ex], writes=[cum])
                cum3 = cum[:].rearrange("p (n c) -> p n c", c=64)
                c.op("act", lambda e: e.activation(dec[:], cum3[:, :, 63], AF.Exp), reads=[cum], writes=[dec])
                c.op("act", lambda e: e.activation(ex[:], cum[:], AF.Exp), reads=[cum], writes=[ex])
                for par in range(2):
                    src_e = ex[:].rearrange("p (t two c) -> p t two c", two=2, c=64)[:, :, par, :]
                    src_q = qf[:].rearrange("p (t two c) -> p t two c", two=2, c=64)[:, :, par, :]
                    c.op("dve", lambda e, par=par, src_e=src_e, src_q=src_q: e.scalar_tensor_tensor(
                        qz[:, :, par, par * 64:(par + 1) * 64], src_q, 0.125, src_e, op0=ALU.mult, op1=ALU.mult),
                        reads=[qf, ex], writes=[qz])
                c.op("act", lambda e: e.activation(ex[:], cum[:], AF.Exp, scale=-1.0), reads=[cum], writes=[ex])
                c.op("dve", lambda e: e.tensor_tensor(kin[:], kf[:], ex[:], ALU.mult), reads=[kf, ex], writes=[kin])
                c.op("dve", lambda e: e.tensor_tensor(ex[:].rearrange("p (n c) -> p n c", c=64), cum3[:, :, 63:64].to_broadcast([128, 32, 64]), cum3, ALU.subtract),
                     reads=[cum], writes=[ex])
                c.op("act", lambda e: e.activation(ex[:], ex[:], AF.Exp), reads=[ex], writes=[ex])
                c.op("dve", lambda e: e.tensor_tensor(kout[:], kf[:], ex[:], ALU.mult), reads=[kf, ex], writes=[kout])
                Sc = Sf[0]
                c.op("pool", lambda e, Sc=Sc: e.memset(Sc[:], 0.0), writes=[Sc])
                S0b = Sb[sbi % 3]; sbi += 1
                c.op("pool", lambda e, S0b=S0b: e.memset(S0b[:], 0.0), writes=[S0b])
                for tt in range(16):
                    V, Gg = vt[tt % 2], gg[tt % 2]
                    c.dma(c.qsel(), V[:], GV[b, tt], writes=[V], reads=[S.pr_tok])
                    if p == 1:
                        c.dma(c.qsel(), Gg[:], GG[b, tt], writes=[Gg], reads=[S.pr_tok])
                    t0 = tt * 128
                    KT = ktok[tt % 2]
                    c.op("pe", lambda e, t0=t0: e.transpose(psK[:], kout[:, t0:t0 + 128], S.identb[:]), reads=[kout, S.identb], writes=[psK])
                    c.op("act", lambda e, KT=KT: e.copy(KT[:], psK[:]), reads=[psK], writes=[KT])
                    Sbs = [S0b]
                    Scur = Sc
                    for ch in range(2):
                        n = tt * 2 + ch
                        c.op("pe", lambda e, ch=ch, KT=KT, V=V, p=p: e.matmul(psKV[:], KT[ch * 64:(ch + 1) * 64, :], V[ch * 64:(ch + 1) * 64, p * 256:(p + 1) * 256],
                                                                               start=True, stop=True), reads=[KT, V], writes=[psKV])
                        Snew = Sf[(tt * 2 + ch + 1) % 2]
                        for hh in range(2):
                            c.op("dve", lambda e, hh=hh, n=n, Scur=Scur, Snew=Snew: e.scalar_tensor_tensor(
                                Snew[hh * 64:(hh + 1) * 64, :], Scur[hh * 64:(hh + 1) * 64, :], dec[hh * 64:(hh + 1) * 64, n:n + 1],
                                psKV[hh * 64:(hh + 1) * 64, hh * 128:(hh + 1) * 128], op0=ALU.mult, op1=ALU.add),
                                reads=[Scur, dec, psKV], writes=[Snew])
                        Sn_b = Sb[sbi % 3]; sbi += 1
                        c.op("act", lambda e, Snew=Snew, Sn_b=Sn_b: e.copy(Sn_b[:], Snew[:]), reads=[Snew], writes=[Sn_b])
                        Sbs.append(Sn_b)
                        Scur = Snew
                    Sc = Scur
                    for hh in range(2):
                        h = p * 2 + hh
                        pl, ph = hh * 64, (hh + 1) * 64
                        AT = At[hh]
                        PO = psO[hh]
                        for par in range(2):
                            c.op("pe", lambda e, pl=pl, ph=ph, t0=t0, par=par, tt=tt: e.matmul(psA[:], kin[pl:ph, t0:t0 + 128], qz[pl:ph, tt, par, :],
                                                                                               start=(par == 0), stop=(par == 1)),
                                 reads=[kin, qz], writes=[psA])
                        c.op("dve", lambda e, AT=AT: e.tensor_tensor(AT[:], psA[:], bcm[:], ALU.mult), reads=[psA, bcm], writes=[AT])
                        c.op("pe", lambda e, AT=AT, V=V, h=h, PO=PO: e.matmul(PO[:], AT[:], V[:, h * 128:(h + 1) * 128], start=True, stop=False),
                             reads=[AT, V], writes=[PO])
                        for ch in range(2):
                            c.op("pe", lambda e, ch=ch, pl=pl, ph=ph, PO=PO, sbv=Sbs[ch]: e.matmul(PO[:], qz[pl:ph, tt, ch, :], sbv[pl:ph, :], start=False, stop=(ch == 1)),
                                 reads=[qz, Sbs[ch]], writes=[PO])
                        c.op("act", lambda e, PO=PO, h=h, tt=tt: e.copy(osball[:, tt, h * 128:(h + 1) * 128], PO[:]), reads=[PO], writes=[osball])
                    S0b = Sbs[2]
                    if p == 1:
                        for h in range(4):
                            c.op("act", lambda e, h=h, tt=tt: e.activation(junk[:], osball[:, tt, h * 128:(h + 1) * 128], AF.Square, accum_out=ss[:, h:h + 1]),
                                 reads=[osball], writes=[junk, ss])
                        c.op("dve", lambda e: e.tensor_scalar(ss[:, 0:4], ss[:, 0:4], 1.0 / 128.0, LN_EPS, op0=ALU.mult, op1=ALU.add), reads=[ss], writes=[ss])
                        c.op("act", lambda e: e.activation(ss[:, 0:4], ss[:, 0:4], AF.Sqrt), reads=[ss], writes=[ss])
                        c.op("dve", lambda e: e.reciprocal(ss[:, 4:8], ss[:, 0:4]), reads=[ss], writes=[ss])
                        for h in range(4):
                            c.op("dve", lambda e, h=h, tt=tt: e.scalar_tensor_tensor(osball[:, tt, h * 128:(h + 1) * 128], osball[:, tt, h * 128:(h + 1) * 128], ss[:, 4 + h:5 + h], ngb[:],
                                                                                      op0=ALU.mult, op1=ALU.mult), reads=[osball, ss, ngb], writes=[osball])
                        c.op("pool", lambda e, tt=tt, Gg=Gg: e.tensor_tensor(gob[:], osball[:, tt, :], Gg[:], ALU.mult), reads=[osball, Gg], writes=[gob])
                        for h in range(4):
                            c.op("pe", lambda e, h=h: e.transpose(psTt[:, h * 128:(h + 1) * 128], gob[:, h * 128:(h + 1) * 128], S.identb[:]),
                                 reads=[gob, S.identb], writes=[psTt])
                        O = oTt[tt % 2]
                        c.op("act", lambda e, O=O: e.copy(O[:], psTt[:]), reads=[psTt], writes=[O])
                        c.dma("sp", oT[b * 16 + tt][:, 0:512], O[:], reads=[O], writes=[S.oT_tok], wtok=O)
        c.barrier()


def even_stage(c, S, A, layer, x_in, oT, NB):
    even_phaseA(c, S, A, layer, x_in, NB)
    gla_phase(c, S, A, layer, oT, NB)
    rwkv_phase(c, S, A, layer, oT, NB)


def rwkv_phase(c, S, A, layer, oT, NB):
    i = layer // 2
    EF = S.EF_s
    NBH = NB * 4
    NF = NBH * 64
    PW = max(NF, 512)
    chunks = [(c0, min(NF, c0 + 512)) for c0 in range(0, NF, 512)]
    DEC_SCALE = -float(np.exp(-0.5))
    with ExitStack() as esB:
        c.es = esB
        HG = NBH // 2
        HW = HG * 64
        PWH = max(HW, 512)
        Zq = [[c.sb("r_Z%d_%d" % (q, j), [128, HW]) for j in range(2)] for q in range(2)]
        tA = [c.sb("r_tA%d" % q, [128, HW], BF16) for q in range(2)]
        tB = [c.sb("r_tB%d" % q, [128, HW]) for q in range(2)]
        tP = [c.sb("r_tP%d" % q, [128, HW]) for q in range(2)]
        tD = [c.sb("r_tD%d" % q, [128, HW], BF16) for q in range(2)]
        vs = [c.sb("r_vs%d" % q, [128, HW]) for q in range(2)]
        tCn = [[c.sb("r_tCn%d_%d" % (q, j), [128, HW]) for j in range(2)] for q in range(2)]
        WOP, AOP, BOP, KOP, ROP = [c.sb("r_op%d" % j, [128, 128, NBH]) for j in range(5)]
        vtok = c.sb("r_vtok", [128, NBH, 128], BF16)
        bonus = c.sb("r_bonus", [128, NBH, 128])
        gT = c.sb("r_gT", [128, NBH, 128])
        ysb = c.sb("r_ysb", [128, NBH * 2, 64])
        ysq = c.sb("r_ysq", [128, NBH * 2, 64])
        yst = c.sb("r_yst", [128, NBH * 2, 2])
        w2b = c.sb("r_w2b", [32, 512], BF16)
        a2b = c.sb("r_a2b", [64, 512], BF16)
        g2b = c.sb("r_g2b", [96, 512], BF16)
        pp = c.sb("r_pp", [128, 4, 8])
        bof = c.sb("r_bof", [128, 128])
        bob = c.sb("r_bob", [128, 128], BF16)
        ahw = c.sb("r_ahw", [128, 255], BF16)
        rT = [c.sb("r_rT%d" % j, [128, 128]) for j in range(2)]
        kTt = [c.sb("r_kT%d" % j, [128, 128]) for j in range(2)]
        vT = [c.sb("r_vT%d" % j, [128, 128]) for j in range(2)]
        wlal = c.sb("r_wlal", [64, 128])
        glt = c.sb("r_glt", [96, 128])
        twb = c.sb("r_twb", [64, 128], BF16)
        sglb = c.sb("r_sglb", [96, 128], BF16)
        e1 = c.sb("r_e1", [128, 128])
        e2 = c.sb("r_e2", [128, 128])
        e3 = c.sb("r_e3", [128, 128])
        e4 = c.sb("r_e4", [128, 128])
        asb = c.sb("r_asb", [128, 128])
        yo = [c.sb("r_yo%d" % j, [128, 128]) for j in range(2)]
        yob = [c.sb("r_yob%d" % j, [128, 128], BF16) for j in range(2)]
        psSAq = [c.ps("r_psSA%d" % q, [128, PWH]) for q in range(2)]
        psVBq = [c.ps("r_psVB%d" % q, [128, PWH]) for q in range(2)]
        psYq = [[c.ps("r_psY%d_%d" % (q, j), [128, HW]) for j in range(2)] for q in range(2)]
        psSA, psVB = psSAq[0], psVBq[0]
        for (dst, nm, rows, pofs) in ((w2b, "rwkv_w2", 32, 0), (a2b, "rwkv_a2", 32, 32), (g2b, "rwkv_g2", 96, 0)):
            st = S.wstage[0]
            c.dma("sp", st[pofs:pofs + rows, 0:512], A[nm][i], writes=[st])
            c.op("dve", lambda e, dst=dst, st=st, rows=rows, pofs=pofs: e.tensor_copy(dst[pofs:pofs + rows, :], st[pofs:pofs + rows, 0:512]), reads=[st], writes=[dst])
        for j, nm in enumerate(("rwkv_w0", "rwkv_a0", "rwkv_k_k", "rwkv_k_a", None, "rwkv_r_k", "rwkv_lnx_g", "rwkv_lnx_b")):
            if nm is None:
                continue
            src = A[nm][i:i + 1] if nm != "rwkv_r_k" else A[nm][i:i + 1].rearrange("o h d -> o (h d)")
            for hp in range(4):
                c.dma(c.qsel(), pp[:, hp, j:j + 1], src[:, hp * 128:(hp + 1) * 128].rearrange("o n -> n o"), writes=[pp])
        c.op("dve", lambda e: e.tensor_scalar(pp[:, :, 4], pp[:, :, 3], -1.0, 1.0, op0=ALU.mult, op1=ALU.add), reads=[pp], writes=[pp])
        c.dma("sp", bof[:], A["blockones"], writes=[bof])
        c.dma("sp", bob[:], A["blockonesb"], writes=[bob])
        c.dma("sp", ahw[:], A["ahwin"], writes=[ahw])
        for q in range(2):
            c.op("pool", lambda e, q=q: e.memset(Zq[q][0][:], 0.0), writes=[Zq[q][0]])
        step = 0
        for tb in range(16):
            t0 = tb * 128
            for b in range(NB):
                c.dma(c.qsel(), wlal[:], EF[b, 17, 0:64, t0:t0 + 128], writes=[wlal], reads=[S.pr_tok])
                c.dma(c.qsel(), glt[:], EF[b, 18, 0:96, t0:t0 + 128], writes=[glt], reads=[S.pr_tok])
                c.op("act", lambda e: e.activation(twb[0:32, :], wlal[0:32, :], AF.Tanh), reads=[wlal], writes=[twb])
                c.op("act", lambda e: e.copy(twb[32:64, :], wlal[32:64, :]), reads=[wlal], writes=[twb])
                c.op("act", lambda e: e.activation(sglb[:], glt[:], AF.Sigmoid), reads=[glt], writes=[sglb])
                for hp in range(4):
                    bh = b * 4 + hp
                    R_, K_, V_ = rT[bh % 2], kTt[bh % 2], vT[bh % 2]
                    c.dma(c.qsel(), R_[:], EF[b, 5 + hp, :, t0:t0 + 128], writes=[R_], reads=[S.pr_tok])
                    c.dma(c.qsel(), K_[:], EF[b, 9 + hp, :, t0:t0 + 128], writes=[K_], reads=[S.pr_tok])
                    c.dma(c.qsel(), V_[:], EF[b, 13 + hp, :, t0:t0 + 128], writes=[V_], reads=[S.pr_tok])
                    cs = slice(hp * 128, (hp + 1) * 128)
                    c.op("pe", lambda e, cs=cs: e.matmul(psSA[:, 0:128], w2b[0:32, cs], twb[0:32, :], start=True, stop=True), reads=[w2b, twb], writes=[psSA])
                    c.op("act", lambda e, hp=hp: e.activation(e1[:], psSA[:, 0:128], AF.Sigmoid, bias=pp[:, hp, 0:1]), reads=[psSA, pp], writes=[e1])
                    c.op("act", lambda e, bh=bh: e.activation(WOP[:, :, bh], e1[:], AF.Exp, scale=DEC_SCALE), reads=[e1], writes=[WOP])
                    c.op("pe", lambda e, cs=cs: e.matmul(psSA[:, 128:256], a2b[32:64, cs], twb[32:64, :], start=True, stop=True), reads=[a2b, twb], writes=[psSA])
                    c.op("act", lambda e, hp=hp: e.activation(asb[:], psSA[:, 128:256], AF.Sigmoid, bias=pp[:, hp, 1:2]), reads=[psSA, pp], writes=[asb])
                    c.op("pe", lambda e, cs=cs: e.matmul(psVB[:, 0:128], g2b[0:96, cs], sglb[0:96, :], start=True, stop=True), reads=[g2b, sglb], writes=[psVB])
                    c.op("act", lambda e, bh=bh: e.copy(gT[:, bh, :], psVB[:, 0:128]), reads=[psVB], writes=[gT])
                    c.op("dve", lambda e, K_=K_, hp=hp: e.tensor_scalar(e2[:], K_[:], pp[:, hp, 2:3], None, op0=ALU.mult), reads=[K_, pp], writes=[e2])
                    c.op("pool", lambda e: e.tensor_tensor(e3[:], e2[:], e2[:], ALU.mult), reads=[e2], writes=[e3])
                    c.op("pe", lambda e: e.matmul(psVB[:, 128:256], bof[:], e3[:], start=True, stop=True), reads=[bof, e3], writes=[psVB])
                    c.op("act", lambda e: e.activation(e3[:], psVB[:, 128:256], AF.Sqrt), reads=[psVB], writes=[e3])
                    c.op("dve", lambda e: e.tensor_scalar(e3[:], e3[:], 1e-12, None, op0=ALU.max), reads=[e3], writes=[e3])
                    c.op("dve", lambda e: e.reciprocal(e3[:], e3[:]), reads=[e3], writes=[e3])
                    c.op("dve", lambda e: e.tensor_tensor(e2[:], e2[:], e3[:], ALU.mult), reads=[e2, e3], writes=[e2])
                    c.op("dve", lambda e, bh=bh: e.tensor_scalar(AOP[:, :, bh], e2[:], -1.0, None, op0=ALU.mult), reads=[e2], writes=[AOP])
                    c.op("dve", lambda e, bh=bh: e.tensor_tensor(BOP[:, :, bh], e2[:], asb[:], ALU.mult), reads=[e2, asb], writes=[BOP])
                    c.op("dve", lambda e, hp=hp: e.tensor_scalar(e4[:], asb[:], pp[:, hp, 3:4], pp[:, hp, 4:5], op0=ALU.mult, op1=ALU.add), reads=[asb, pp], writes=[e4])
                    c.op("dve", lambda e, K_=K_: e.tensor_tensor(e4[:], e4[:], K_[:], ALU.mult), reads=[e4, K_], writes=[e4])
                    c.op("pool", lambda e, bh=bh: e.tensor_copy(KOP[:, :, bh], e4[:]), reads=[e4], writes=[KOP])
                    c.op("pool", lambda e, bh=bh, R_=R_: e.tensor_copy(ROP[:, :, bh], R_[:]), reads=[R_], writes=[ROP])
                    c.op("dve", lambda e, R_=R_, hp=hp: e.scalar_tensor_tensor(e1[:], R_[:], pp[:, hp, 5:6], e4[:], op0=ALU.mult, op1=ALU.mult), reads=[R_, pp, e4], writes=[e1])
                    c.op("pe", lambda e: e.matmul(psVB[:, 256:384], bof[:], e1[:], start=True, stop=True), reads=[bof, e1], writes=[psVB])
                    c.op("dve", lambda e, bh=bh, V_=V_: e.tensor_tensor(bonus[:, bh, :], psVB[:, 256:384], V_[:], ALU.mult), reads=[psVB, V_], writes=[bonus])
                    c.op("pe", lambda e, V_=V_: e.transpose(psSA[:, 256:384], V_[:], S.identf[:]), reads=[V_, S.identf], writes=[psSA])
                    c.op("act", lambda e, bh=bh: e.copy(vtok[:, bh, :], psSA[:, 256:384]), reads=[psSA], writes=[vtok])
            vt4 = vtok[:].rearrange("p g (h i) -> p g h i", h=2)

            def v3(tk):
                return tk[:, 0:HW].rearrange("p (g i) -> p g i", i=64)

            def bc(op_, tl, q):
                return op_[:, tl, q * HG:(q + 1) * HG].unsqueeze(2).to_broadcast([128, HG, 64])

            def lookahead_pe(tl):
                for q in range(2):
                    for hh in range(2):
                        c.op("pe", lambda e, q=q, hh=hh, tl=tl: e.matmul(psVBq[q][hh * 64:(hh + 1) * 64, 0:HW], S.identb[:, tl:tl + 1].to_broadcast([128, 64]),
                                                                         vt4[:, q * HG:(q + 1) * HG, hh, :], start=True, stop=True),
                             reads=[S.identb, vtok], writes=[psVBq[q]])
                    c.op("act", lambda e, q=q: e.copy(vs[q][:], psVBq[q][:, 0:HW]), reads=[psVBq[q]], writes=[vs[q]])

            def lookahead_pool(tl):
                for q in range(2):
                    TC = tCn[q][tl % 2]
                    c.op("pool", lambda e, tl=tl, TC=TC, q=q: e.tensor_tensor(v3(TC), v3(vs[q]), bc(KOP, tl, q), ALU.mult), reads=[vs[q], KOP], writes=[TC])

            def emit_tmpA(tl, par):
                for q in range(2):
                    Zi = Zq[q][par]
                    c.op("dve", lambda e, tl=tl, q=q, Zi=Zi: e.tensor_tensor(v3(tA[q]), v3(Zi), bc(AOP, tl, q), ALU.mult), reads=[Zi, AOP], writes=[tA[q]])

            lookahead_pe(0)
            lookahead_pool(0)
            emit_tmpA(0, step % 2)
            for tl in range(128):
                par = step % 2
                step += 1
                for q in range(2):
                    c.op("pe", lambda e, q=q: e.matmul(psSAq[q][:, 0:HW], bob[:], tA[q][:, 0:HW], start=True, stop=True), reads=[bob, tA[q]], writes=[psSAq[q]])
                if tl < 127:
                    lookahead_pe(tl + 1)
                for q in range(2):
                    Zi, TC = Zq[q][par], tCn[q][tl % 2]
                    c.op("pool", lambda e, tl=tl, q=q, Zi=Zi: e.tensor_tensor(v3(tP[q]), v3(Zi), bc(WOP, tl, q), ALU.mult), reads=[Zi, WOP], writes=[tP[q]])
                    c.op("pool", lambda e, q=q, TC=TC: e.tensor_tensor(tP[q][:], tP[q][:], TC[:], ALU.add), reads=[tP[q], TC], writes=[tP[q]])
                if tl < 127:
                    lookahead_pool(tl + 1)
                for q in range(2):
                    Zo = Zq[q][1 - par]
                    c.op("dve", lambda e, tl=tl, q=q: e.tensor_tensor(v3(tB[q]), v3(psSAq[q]), bc(BOP, tl, q), ALU.mult), reads=[psSAq[q], BOP], writes=[tB[q]])
                    c.op("dve", lambda e, q=q, Zo=Zo: e.tensor_tensor(Zo[:], tP[q][:], tB[q][:], ALU.add), reads=[tP[q], tB[q]], writes=[Zo])
                if tl < 127:
                    emit_tmpA(tl + 1, 1 - par)
                for q in range(2):
                    Zo = Zq[q][1 - par]
                    c.op("dve", lambda e, tl=tl, q=q, Zo=Zo: e.tensor_tensor(v3(tD[q]), v3(Zo), bc(ROP, tl, q), ALU.mult), reads=[Zo, ROP], writes=[tD[q]])
                    for hh in range(2):
                        c.op("pe", lambda e, q=q, hh=hh, tl=tl: e.matmul(psYq[q][hh][:, 0:HW], ahw[hh * 64:(hh + 1) * 64, 127 - tl:255 - tl],
                                                                         tD[q][hh * 64:(hh + 1) * 64, 0:HW], start=(tl == 0), stop=(tl == 127)),
                             reads=[ahw, tD[q]], writes=[psYq[q][hh]])
            ys4 = ysb[:].rearrange("p (g h) i -> p g h i", h=2)
            for q in range(2):
                for hh in range(2):
                    c.op("act", lambda e, hh=hh, q=q: e.copy(ys4[:, q * HG:(q + 1) * HG, hh, :], psYq[q][hh][:, 0:HW].rearrange("p (g i) -> p g i", i=64)),
                         reads=[psYq[q][hh]], writes=[ysb])
            G2 = NBH * 2
            c.op("dve", lambda e: e.tensor_reduce(yst[:, :, 0], ysb[:], AX.X, ALU.add), reads=[ysb], writes=[yst])
            c.op("dve", lambda e: e.tensor_scalar(yst[:, :, 0], yst[:, :, 0], 1.0 / 64.0, None, op0=ALU.mult), reads=[yst], writes=[yst])
            c.op("dve", lambda e: e.tensor_tensor(ysb[:], ysb[:], yst[:, :, 0:1].to_broadcast([128, G2, 64]), ALU.subtract), reads=[ysb, yst], writes=[ysb])
            c.op("pool", lambda e: e.tensor_tensor(ysq[:], ysb[:], ysb[:], ALU.mult), reads=[ysb], writes=[ysq])
            c.op("dve", lambda e: e.tensor_reduce(yst[:, :, 1], ysq[:], AX.X, ALU.add), reads=[ysq], writes=[yst])
            c.op("dve", lambda e: e.tensor_scalar(yst[:, :, 1], yst[:, :, 1], 1.0 / 64.0, 64e-5, op0=ALU.mult, op1=ALU.add), reads=[yst], writes=[yst])
            c.op("act", lambda e: e.activation(yst[:, :, 1], yst[:, :, 1], AF.Sqrt), reads=[yst], writes=[yst])
            c.op("dve", lambda e: e.reciprocal(yst[:, :, 1], yst[:, :, 1]), reads=[yst], writes=[yst])
            c.op("dve", lambda e: e.tensor_tensor(ysb[:], ysb[:], yst[:, :, 1:2].to_broadcast([128, G2, 64]), ALU.mult), reads=[ysb, yst], writes=[ysb])
            for b in range(NB):
                for hp in range(4):
                    bh = b * 4 + hp
                    Y, YB = yo[bh % 2], yob[bh % 2]
                    c.op("pe", lambda e, bh=bh: e.transpose(psSA[:, 0:128], ysb[:, 2 * bh:2 * bh + 2, :].rearrange("p h i -> p (h i)"), S.identf[:]),
                         reads=[ysb, S.identf], writes=[psSA])
                    c.op("dve", lambda e, Y=Y, hp=hp: e.tensor_scalar(Y[:], psSA[:, 0:128], pp[:, hp, 6:7], pp[:, hp, 7:8], op0=ALU.mult, op1=ALU.add), reads=[psSA, pp], writes=[Y])
                    c.op("pool", lambda e, Y=Y, bh=bh: e.tensor_tensor(Y[:], Y[:], bonus[:, bh, :], ALU.add), reads=[Y, bonus], writes=[Y])
                    c.op("pool", lambda e, Y=Y, YB=YB, bh=bh: e.tensor_tensor(YB[:], Y[:], gT[:, bh, :], ALU.mult), reads=[Y, gT], writes=[YB])
                    c.dma(c.qsel(), oT[b * 16 + tb][:, (4 + hp) * 128:(5 + hp) * 128], YB[:], reads=[YB], writes=[S.oT_tok], wtok=YB)
        c.barrier()


_NC_CACHE = {}


def kernel(**inputs):
    n = 8
    NB = 32 // n
    if "nc" not in _NC_CACHE:
        _NC_CACHE["nc"] = build(NB=NB, layers=(0, 1, 2, 3), mode="full")
    nc = _NC_CACHE["nc"]
    x = np.ascontiguousarray(inputs["x"], dtype=np.float32)
    cs = consts()
    in_maps = []
    for ci in range(n):
        m = {"x": x[ci * NB:(ci + 1) * NB].reshape(NB * T, D)}
        for k in WSPEC:
            m[k] = np.ascontiguousarray(inputs[k], dtype=np.float32)
        m.update(cs)
        in_maps.append(m)
    res = run_bass_kernel_spmd(nc, in_maps, core_ids=list(range(n)))
    out = np.stack([r["y"].reshape(NB, T, D) for r in res.results], 0).reshape(32, T, D)
    return out.astype(np.float32)
```

```python
import numpy as np
from contextlib import ExitStack
import concourse.bass as bass
import concourse.mybir as mybir
from concourse.bass_utils import run_bass_kernel_spmd
import ml_dtypes

F32 = mybir.dt.float32
BF16 = mybir.dt.bfloat16
U32 = mybir.dt.uint32
AF = mybir.ActivationFunctionType
ALU = mybir.AluOpType
AX = mybir.AxisListType

T = 2048
D = 1024
DEPTH = 4
ALPHA = float((2 * DEPTH) ** 0.25)
LN_EPS = 1e-5
NEG = -1e30
SEM_EPOCH = 60000


class Tok:
    __slots__ = ("w", "r", "t", "dsem", "dcnt", "name")

    def __init__(self, t=None, name=None):
        self.w = None
        self.r = []
        self.t = t
        self.dsem = None
        self.dcnt = 0
        self.name = name

    def __getitem__(self, k):
        return self.t[k]


class Ctx:
    def __init__(self, nc, es):
        self.nc = nc
        self.es = es
        self.es0 = es
        self.dpool = {}
        self.eng = {"pe": nc.tensor, "dve": nc.vector, "act": nc.scalar, "pool": nc.gpsimd, "sp": nc.sync}
        self.sem = {}
        self.cnt = {}
        for e in self.eng:
            self.sem[e] = es.enter_context(nc.semaphore("s_" + e))
            self.cnt[e] = 0
        self.waited = {e: {} for e in self.eng}
        self.pe_sems = {id(self.sem["pe"])}
        self.nsem = 0
        self.ninst = 0
        self.rr = 0
        self.uid = 0

    def sb(self, name, shape, dt=F32):
        self.uid += 1
        return Tok(self.es.enter_context(self.nc.sbuf_tensor("sb%d_%s" % (self.uid, name), list(shape), dt)), name)

    def ps(self, name, shape, dt=F32):
        self.uid += 1
        return Tok(self.es.enter_context(self.nc.psum_tensor("ps%d_%s" % (self.uid, name), list(shape), dt)), name)

    def tok(self, name=None):
        return Tok(None, name)

    def _dsem(self, tok):
        if tok.name not in self.dpool or self.dpool[tok.name][1] >= SEM_EPOCH:
            self.dpool[tok.name] = [self.es0.enter_context(self.nc.semaphore("d%d" % self.nsem)), 0]
            self.nsem += 1
        return self.dpool[tok.name]

    def barrier(self):
        for e in self.eng:
            for o in self.eng:
                if o != e and self.cnt[o] > 0:
                    self._wait(e, (self.sem[o], self.cnt[o]))
            for sem, cnt in self.dpool.values():
                if cnt > 0:
                    self._wait(e, (sem, cnt))

    def _wait(self, e, dep):
        if dep is None:
            return
        sem, v = dep
        k = id(sem)
        if False and e == "pe" and k in self.pe_sems:
            return
        if self.waited[e].get(k, 0) >= v:
            return
        self.waited[e][k] = v
        self.eng[e].wait_ge(sem, v)
        self.ninst += 1

    def _deps(self, e, reads, writes):
        for t in reads:
            self._wait(e, t.w)
        for t in writes:
            self._wait(e, t.w)
            for d in t.r:
                self._wait(e, d)

    @staticmethod
    def _compact(lst):
        best = {}
        for sem, v in lst:
            k = id(sem)
            if k not in best or best[k][1] < v:
                best[k] = (sem, v)
        return list(best.values())

    def _mark(self, me, reads, writes):
        for t in reads:
            t.r.append(me)
            if len(t.r) > 12:
                t.r = self._compact(t.r)
        for t in writes:
            t.w = me
            t.r = []

    def op(self, e, fn, reads=(), writes=()):
        self._deps(e, reads, writes)
        if self.cnt[e] >= SEM_EPOCH:
            self.sem[e] = self.es0.enter_context(self.nc.semaphore("s_%s_%d" % (e, self.nsem)))
            self.nsem += 1
            self.cnt[e] = 0
            if e == "pe":
                self.pe_sems.add(id(self.sem[e]))
        ins = fn(self.eng[e])
        self.cnt[e] += 1
        ins.then_inc(self.sem[e], 1)
        self.ninst += 1
        self._mark((self.sem[e], self.cnt[e]), reads, writes)
        return ins

    def dma(self, q, out_ap, in_ap, reads=(), writes=(), wtok=None, **kw):
        self._deps(q, reads, writes)
        tk = wtok or (writes[0] if writes else reads[0])
        ent = self._dsem(tk)
        ins = self.eng[q].dma_start(out=out_ap, in_=in_ap, **kw)
        ent[1] += 16
        ins.then_inc(ent[0], 16)
        self.ninst += 1
        self._mark((ent[0], ent[1]), reads, writes)
        return ins

    def gather(self, out_ap, in_ap, idx_ap, reads=(), writes=()):
        q = "pool"
        self._deps(q, reads, writes)
        tk = writes[0]
        ent = self._dsem(tk)
        ins = self.nc.gpsimd.indirect_dma_start(
            out=out_ap, out_offset=None, in_=in_ap,
            in_offset=bass.IndirectOffsetOnAxis(ap=idx_ap, axis=0))
        ent[1] += 16
        ins.then_inc(ent[0], 16)
        self.ninst += 1
        self._mark((ent[0], ent[1]), reads, writes)
        return ins

    def finish(self, toks, e="sp"):
        for t in toks:
            self._wait(e, t.w)
            for d in t.r:
                self._wait(e, d)

    def qsel(self):
        self.rr += 1
        return ("sp", "act")[self.rr % 2]


class Shared:
    pass


def load_w_bf16(c, S, dst, src, K, N, pofs=0, rows=128):
    for k in range(K):
        for n0 in range(0, N, 1024):
            n1 = min(N, n0 + 1024)
            S.wsi += 1
            st = S.wstage[S.wsi % 2]
            c.dma(c.qsel(), st[pofs:pofs + rows, 0:n1 - n0], src[k * rows:(k + 1) * rows, n0:n1], writes=[st])
            e = ("dve", "pool")[S.wsi % 2]
            c.op(e, lambda en, st=st, k=k, n0=n0, n1=n1: en.tensor_copy(dst[pofs:pofs + rows, k, n0:n1], st[pofs:pofs + rows, 0:n1 - n0]),
                 reads=[st], writes=[dst])


def bcast_row(c, dst, src_row, n):
    c.dma(c.qsel(), dst[:, 0:n], src_row.partition_broadcast(128), writes=[dst])


def layer_norm(c, S, out_ap, out_tok, z, g_bc, b_bc):
    st, mv, sd = S.ln_st, S.ln_mv, S.ln_sd
    c.op("dve", lambda e: e.bn_stats(st[:, 0, :], z[:, 0:512]), reads=[z], writes=[st])
    c.op("dve", lambda e: e.bn_stats(st[:, 1, :], z[:, 512:1024]), reads=[z], writes=[st])
    c.op("dve", lambda e: e.bn_aggr(mv[:], st[:]), reads=[st], writes=[mv])
    c.op("dve", lambda e: e.tensor_scalar(sd[:, 0:1], mv[:, 1:2], LN_EPS, None, op0=ALU.add), reads=[mv], writes=[sd])
    c.op("act", lambda e: e.activation(sd[:, 1:2], sd[:, 0:1], AF.Sqrt), reads=[sd], writes=[sd])
    c.op("dve", lambda e: e.reciprocal(sd[:, 2:3], sd[:, 1:2]), reads=[sd], writes=[sd])
    c.op("dve", lambda e: e.tensor_scalar(z[:], z[:], mv[:, 0:1], sd[:, 2:3], op0=ALU.subtract, op1=ALU.mult),
         reads=[z, mv, sd], writes=[z])
    c.op("pool", lambda e: e.tensor_tensor(z[:], z[:], g_bc[:], ALU.mult), reads=[z, g_bc], writes=[z])
    c.op("pool", lambda e: e.tensor_tensor(out_ap, z[:], b_bc[:], ALU.add), reads=[z, b_bc], writes=[out_tok])


NG = 12
GS = 4
PULLN = 1
PULLD = 1


def uv_prep(c, S, A, layer):
    with ExitStack() as esP:
        c.es = esP
        uf = [c.sb("p_uf%d" % j, [128, 1024]) for j in range(2)]
        vf = [c.sb("p_vf%d" % j, [128, 1024]) for j in range(2)]
        uvb = [c.sb("p_uvb%d" % j, [128, 2048], BF16) for j in range(2)]
        for r in range(128):
            U, V, B = uf[r % 2], vf[r % 2], uvb[r % 2]
            c.dma("sp", U[:], A["peer_u"][layer, r * 128:(r + 1) * 128, :], writes=[U])
            c.dma("act", V[:], A["peer_v"][layer, r * 128:(r + 1) * 128, :], writes=[V])
            c.op("dve", lambda e, U=U, B=B: e.tensor_copy(B[:, 0:1024], U[:]), reads=[U], writes=[B])
            c.op("pool", lambda e, V=V, B=B: e.tensor_copy(B[:, 1024:2048], V[:]), reads=[V], writes=[B])
            c.dma("sp", S.UV_s[r * 128:(r + 1) * 128, :], B[:], reads=[B], writes=[S.uv_tok], wtok=B)
        c.barrier()


def tail_alloc(c, S):
    S.wout = c.sb("wout", [128, 8, 1024], BF16)
    S.wq = c.sb("wq", [128, 8, 1024], BF16)
    S.keysT = c.sb("keysT", [128, 8, 128], BF16)
    S.lng = [c.sb("lng%d" % i, [128, 1024]) for i in range(4)]
    S.iota16 = c.sb("iota16", [128, 16])
    S.oT = c.sb("oT_sb", [128, 8, 128], BF16)
    S.xt = c.sb("xt", [128, 1024])
    S.z = c.sb("z", [128, 1024])
    S.xm2 = [c.sb("xm%d" % i, [128, 1024]) for i in range(2)]
    S.xmb2 = [c.sb("xmb%d" % i, [128, 1024], BF16) for i in range(2)]
    S.idxu2 = [c.sb("idxu%d" % i, [128, 128], U32) for i in range(2)]
    S.gate2 = [c.sb("gate%d" % i, [128, 8, 16]) for i in range(2)]
    S.xm, S.xmb = S.xm2[0], S.xmb2[0]
    S.xmT = c.sb("xmT", [128, 8, 128], BF16)
    S.qT = c.sb("qT", [128, 8, 128], BF16)
    S.ssc = c.sb("ssc", [128, 128])
    S.tops = c.sb("tops", [128, 16, 16])
    S.topi = c.sb("topi", [128, 16, 16], U32)
    S.topf = c.sb("topf", [128, 16, 16])
    S.cands = c.sb("cands", [128, 8, 256])
    S.candw = c.sb("candw", [128, 256])
    S.best = c.sb("best", [128, 8, 16])
    S.pos = c.sb("pos", [128, 8, 16], U32)
    S.pab = c.sb("pab", [128, 2, 128], U32)
    S.pabf = c.sb("pabf", [128, 2, 128])
    S.eq = [c.sb("eq%d" % i, [128, 128, 16]) for i in range(2)]
    S.idx2 = c.sb("idx2", [128, 2, 128])
    S.idxf = c.sb("idxf", [128, 128])
    S.idxu, S.gate = S.idxu2[0], S.gate2[0]
    S.gsum = c.sb("gsum", [128, 8])
    S.hh4 = [c.sb("hh%d" % i, [128, GS]) for i in range(4)]
    S.coef4 = [c.sb("coef%d" % i, [128, GS]) for i in range(4)]
    S.junk2 = [c.sb("junk%d" % i, [128, 1024], BF16) for i in range(2)]
    S.acc = c.sb("acc", [128, 1024])
    S.G = [c.sb("G%d" % i, [128, 2048], BF16) for i in range(NG)]
    S.dg = [c.sb("dg%d" % i, [128, 128], BF16) for i in range(4)]
    S.ot = c.sb("ot", [128, 1024])
    S.psA = c.ps("psA", [128, 1024])
    S.psT = c.ps("psT", [128, 1024])
    S.psS = c.ps("psS", [128, 8, 128])
    S.psAcc = c.ps("psAcc", [128, 1024])


def tail_weights(c, S, A, layer):
    wo = A["even_w_out"][layer // 2] if layer % 2 == 0 else A["odd_w_out"][layer // 2]
    load_w_bf16(c, S, S.wout, wo, 8, 1024)
    load_w_bf16(c, S, S.wq, A["peer_w_q"][layer], 8, 1024)
    bcast_row(c, S.lng[0], A["mix_ln_g"][layer:layer + 1, :], 1024)
    bcast_row(c, S.lng[1], A["mix_ln_b"][layer:layer + 1, :], 1024)
    bcast_row(c, S.lng[2], A["ffn_ln_g"][layer:layer + 1, :], 1024)
    bcast_row(c, S.lng[3], A["ffn_ln_b"][layer:layer + 1, :], 1024)
    c.dma("sp", S.iota16[:], A["iota16"], writes=[S.iota16])
    sk = A["peer_sub_keys"][layer]
    for h in range(8):
        st = S.wstage[h % 2]
        for cc in range(2):
            c.dma(c.qsel(), st[:, cc * 64:(cc + 1) * 64], sk[h, cc], writes=[st])
        c.op("dve", lambda e, st=st: e.tensor_copy(S.xmb[:, 0:128], st[:, 0:128]), reads=[st], writes=[S.xmb])
        c.op("pe", lambda e: e.matmul(S.psT[:, 0:128], S.xmb[:, 0:128], S.identb[:], start=True, stop=True), reads=[S.xmb, S.identb], writes=[S.psT])
        c.op("act", lambda e, h=h: e.copy(S.keysT[:, h, :], S.psT[:, 0:128]), reads=[S.psT], writes=[S.keysT])


def tail_front(c, S, A, layer, x_src, oT_src, par):
    xm, xmb, idxu, gate = S.xm2[par], S.xmb2[par], S.idxu2[par], S.gate2[par]
    c.dma("sp", S.xt[:], x_src, writes=[S.xt], reads=[S.xs_tok])
    yield
    c.dma("act", S.oT[:].rearrange("p k t -> p (k t)"), oT_src, writes=[S.oT], reads=[S.oT_tok])
    yield
    for half in range(2):
        for k in range(8):
            c.op("pe", lambda e, k=k, half=half: e.matmul(S.psA[:, half * 512:(half + 1) * 512], S.oT[:, k, :],
                                                            S.wout[:, k, half * 512:(half + 1) * 512],
                                                            start=(k == 0), stop=(k == 7)),
                 reads=[S.oT, S.wout], writes=[S.psA])
            yield
    c.op("dve", lambda e: e.scalar_tensor_tensor(S.z[:], S.xt[:], ALPHA, S.psA[:], op0=ALU.mult, op1=ALU.add),
         reads=[S.xt, S.psA], writes=[S.z])
    yield
    layer_norm(c, S, xm[:], xm, S.z, S.lng[0], S.lng[1])
    yield
    c.op("act", lambda e: e.copy(xmb[:], xm[:]), reads=[xm], writes=[xmb])
    yield
    for k in range(8):
        c.op("pe", lambda e, k=k: e.matmul(S.psT[:, k * 128:(k + 1) * 128], xmb[:, k * 128:(k + 1) * 128], S.identb[:], start=True, stop=True),
             reads=[xmb, S.identb], writes=[S.psT])
        yield
    c.op("act", lambda e: e.copy(S.xmT[:].rearrange("p k t -> p (k t)"), S.psT[:]), reads=[S.psT], writes=[S.xmT])
    yield
    for h in range(8):
        for k in range(8):
            c.op("pe", lambda e, k=k, h=h: e.matmul(S.psA[:, h * 128:(h + 1) * 128], S.wq[:, k, h * 128:(h + 1) * 128],
                                                      S.xmT[:, k, :], start=(k == 0), stop=(k == 7)),
                 reads=[S.wq, S.xmT], writes=[S.psA])
            yield
    c.op("act", lambda e: e.copy(S.qT[:].rearrange("p k t -> p (k t)"), S.psA[:]), reads=[S.psA], writes=[S.qT])
    yield
    for hf in range(2):
        for j in range(8):
            hc = hf * 8 + j
            h, cc = hc // 2, hc % 2
            c.op("pe", lambda e, h=h, cc=cc, j=j: e.matmul(S.psS[:, j, :], S.qT[cc * 64:(cc + 1) * 64, h, :],
                                                            S.keysT[cc * 64:(cc + 1) * 64, h, :], start=True, stop=True),
                 reads=[S.qT, S.keysT], writes=[S.psS])
            yield
        for j in range(8):
            hc = hf * 8 + j
            c.op("dve", lambda e, hc=hc, j=j: e.max(S.tops[:, hc, 0:8], S.psS[:, j, :]), reads=[S.psS], writes=[S.tops])
            yield
            c.op("dve", lambda e, hc=hc, j=j: e.max_index(S.topi[:, hc, 0:8], S.tops[:, hc, 0:8], S.psS[:, j, :]),
                 reads=[S.psS, S.tops], writes=[S.topi])
            yield
            c.op("dve", lambda e, hc=hc, j=j: e.match_replace(S.ssc[:], S.tops[:, hc, 0:8], S.psS[:, j, :], NEG),
                 reads=[S.psS, S.tops], writes=[S.ssc])
            yield
            c.op("dve", lambda e, hc=hc: e.max(S.tops[:, hc, 8:16], S.ssc[:]), reads=[S.ssc], writes=[S.tops])
            yield
            c.op("dve", lambda e, hc=hc: e.max_index(S.topi[:, hc, 8:16], S.tops[:, hc, 8:16], S.ssc[:]),
                 reads=[S.ssc, S.tops], writes=[S.topi])
            yield
    c.op("dve", lambda e: e.tensor_copy(S.topf[:], S.topi[:]), reads=[S.topi], writes=[S.topf])
    yield
    tops4 = S.tops[:].rearrange("p (h c) k -> p h c k", c=2)
    topf4 = S.topf[:].rearrange("p (h c) k -> p h c k", c=2)
    c.op("dve", lambda e: e.tensor_scalar(topf4[:, :, 0, :], topf4[:, :, 0, :], 128.0, None, op0=ALU.mult),
         reads=[S.topf], writes=[S.topf])
    yield
    cs4 = S.cands[:].rearrange("p h (a b) -> p h a b", b=16)
    for h in range(8):
        c.op("pool", lambda e, h=h: e.tensor_tensor(cs4[:, h], tops4[:, h, 0, :].unsqueeze(2).to_broadcast([128, 16, 16]),
                                                     tops4[:, h, 1, :].unsqueeze(1).to_broadcast([128, 16, 16]), ALU.add),
             reads=[S.tops], writes=[S.cands])
        yield
    for h in range(8):
        c.op("dve", lambda e, h=h: e.max(S.best[:, h, 0:8], S.cands[:, h, :]), reads=[S.cands], writes=[S.best])
        yield
        c.op("dve", lambda e, h=h: e.max_index(S.pos[:, h, 0:8], S.best[:, h, 0:8], S.cands[:, h, :]), reads=[S.cands, S.best], writes=[S.pos])
        yield
        c.op("dve", lambda e, h=h: e.match_replace(S.candw[:], S.best[:, h, 0:8], S.cands[:, h, :], NEG),
             reads=[S.cands, S.best], writes=[S.candw])
        yield
        c.op("dve", lambda e, h=h: e.max(S.best[:, h, 8:16], S.candw[:]), reads=[S.candw], writes=[S.best])
        yield
        c.op("dve", lambda e, h=h: e.max_index(S.pos[:, h, 8:16], S.best[:, h, 8:16], S.candw[:]), reads=[S.candw, S.best], writes=[S.pos])
        yield
    posf = S.pos[:].rearrange("p h k -> p (h k)")
    c.op("dve", lambda e: e.tensor_scalar(S.pab[:, 0, :], posf, 4, None, op0=ALU.logical_shift_right), reads=[S.pos], writes=[S.pab])
    yield
    c.op("dve", lambda e: e.tensor_scalar(S.pab[:, 1, :], posf, 15, None, op0=ALU.bitwise_and), reads=[S.pos], writes=[S.pab])
    yield
    c.op("dve", lambda e: e.tensor_copy(S.pabf[:], S.pab[:]), reads=[S.pab], writes=[S.pabf])
    yield
    for ab in range(2):
        eq = S.eq[ab]
        c.op("dve", lambda e, ab=ab, eq=eq: e.tensor_tensor(eq[:], S.pabf[:, ab, :].unsqueeze(2).to_broadcast([128, 128, 16]),
                                                         S.iota16[:].unsqueeze(1).to_broadcast([128, 128, 16]), ALU.is_equal),
             reads=[S.pabf, S.iota16], writes=[eq])
        yield
        c.op("pool", lambda e, ab=ab, eq=eq: e.tensor_tensor(eq[:].rearrange("p (h k) a -> p h k a", h=8), eq[:].rearrange("p (h k) a -> p h k a", h=8),
                                                          topf4[:, :, ab, :].unsqueeze(2).to_broadcast([128, 8, 16, 16]), ALU.mult),
             reads=[S.topf, eq], writes=[eq])
        yield
        c.op("dve", lambda e, ab=ab, eq=eq: e.tensor_reduce(S.idx2[:, ab, :], eq[:], AX.X, ALU.add), reads=[eq], writes=[S.idx2])
        yield
    c.op("dve", lambda e: e.tensor_tensor(S.idxf[:], S.idx2[:, 0, :], S.idx2[:, 1, :], ALU.add), reads=[S.idx2], writes=[S.idxf])
    yield
    c.op("dve", lambda e: e.tensor_scalar(S.idxf[:], S.idxf[:], 16383.0, 0.0, op0=ALU.min, op1=ALU.max),
         reads=[S.idxf], writes=[S.idxf])
    yield
    c.op("dve", lambda e: e.tensor_copy(idxu[:], S.idxf[:]), reads=[S.idxf], writes=[idxu])
    yield
    c.op("dve", lambda e: e.tensor_tensor(gate[:], S.best[:], S.best[:, :, 0:1].to_broadcast([128, 8, 16]), ALU.subtract),
         reads=[S.best], writes=[gate])
    yield
    c.op("act", lambda e: e.activation(gate[:], gate[:], AF.Exp), reads=[gate], writes=[gate])
    yield
    c.op("dve", lambda e: e.tensor_reduce(S.gsum[:], gate[:], AX.X, ALU.add), reads=[gate], writes=[S.gsum])
    yield
    c.op("dve", lambda e: e.reciprocal(S.gsum[:], S.gsum[:]), reads=[S.gsum], writes=[S.gsum])
    yield
    c.op("dve", lambda e: e.tensor_tensor(gate[:], gate[:], S.gsum[:].unsqueeze(2).to_broadcast([128, 8, 16]), ALU.mult),
         reads=[gate, S.gsum], writes=[gate])
    yield
def tail_issue(c, S, par, k):
    G = S.G[(S.gi + k) % NG]
    c.gather(G[:], S.UV_s, S.idxu2[par][:, k:k + 1], reads=[S.idxu2[par], S.uv_tok], writes=[G])


def tail_back(c, S, par, out_dst, out_tok, fgen, nxt_par, prefetched):
    xm, xmb, idxu, gate = S.xm2[par], S.xmb2[par], S.idxu2[par], S.gate2[par]
    gate2 = gate[:].rearrange("p h k -> p (h k)")
    LOOK = NG - GS
    if not prefetched:
        for k in range(LOOK):
            tail_issue(c, S, par, k)
    issued = LOOK

    def pull(n):
        for _ in range(n):
            if next(fgen, "done") == "done":
                return
    for g0 in range(0, 128, GS):
        HH, CF = S.hh4[(g0 // GS) % 4], S.coef4[(g0 // GS) % 4]
        for k in range(g0, g0 + GS):
            G = S.G[(S.gi + k) % NG]
            JK = S.junk2[k % 2]
            c.op("dve", lambda e, G=G, k=k, JK=JK, HH=HH: e.scalar_tensor_tensor(JK[:], G[:, 0:1024], 1.0, xmb[:], op0=ALU.mult, op1=ALU.mult,
                                                                              accum_out=HH[:, k - g0:k - g0 + 1]),
                 reads=[G, xmb], writes=[JK, HH])
            pull(PULLD)
        c.op("act", lambda e, HH=HH, CF=CF: e.activation(CF[:], HH[:], AF.Gelu), reads=[HH], writes=[CF])
        c.op("dve", lambda e, g0=g0, CF=CF: e.tensor_tensor(CF[:], CF[:], gate2[:, g0:g0 + GS], ALU.mult),
             reads=[CF, gate], writes=[CF])
        for k in range(g0, g0 + GS):
            G = S.G[(S.gi + k) % NG]
            DG = S.dg[k % 4]
            c.op("act", lambda e, DG=DG, k=k, CF=CF: e.activation(DG[:], S.identb[:], AF.Copy, scale=CF[:, k - g0:k - g0 + 1]), reads=[S.identb, CF], writes=[DG])
            for half in range(2):
                c.op("pe", lambda e, DG=DG, G=G, half=half, k=k: e.matmul(S.psAcc[:, half * 512:(half + 1) * 512], DG[:],
                                                                          G[:, 1024 + half * 512:1536 + half * 512], start=(k == 0), stop=(k == 127)),
                     reads=[DG, G], writes=[S.psAcc])
            if issued < 128:
                tail_issue(c, S, par, issued)
                issued += 1
            pull(PULLN)
    pull(100000)
    S.gi += 128
    if nxt_par is not None:
        for k in range(LOOK):
            tail_issue(c, S, nxt_par, k)
    c.op("dve", lambda e: e.scalar_tensor_tensor(S.acc[:], xm[:], ALPHA, S.psAcc[:], op0=ALU.mult, op1=ALU.add),
         reads=[xm, S.psAcc], writes=[S.acc])
    layer_norm(c, S, S.ot[:], S.ot, S.acc, S.lng[2], S.lng[3])
    c.dma("sp", out_dst, S.ot[:], reads=[S.ot], writes=[out_tok], wtok=out_tok)


WSPEC = {
    "even_w_in": [2, 1024, 3248], "gla_a_w2": [2, 16, 256], "gla_a_b": [2, 256], "gla_norm_g": [2, 128],
    "rwkv_mu": [2, 1696], "rwkv_w0": [2, 512], "rwkv_w2": [2, 32, 512], "rwkv_a0": [2, 512],
    "rwkv_a2": [2, 32, 512], "rwkv_g2": [2, 96, 512], "rwkv_k_k": [2, 512], "rwkv_k_a": [2, 512],
    "rwkv_r_k": [2, 8, 64], "rwkv_lnx_g": [2, 512], "rwkv_lnx_b": [2, 512], "even_w_out": [2, 1024, 1024],
    "odd_w_in": [2, 1024, 1864], "odd_w_out": [2, 1024, 1024], "mix_ln_g": [4, 1024], "mix_ln_b": [4, 1024],
    "peer_w_q": [4, 1024, 1024], "peer_sub_keys": [4, 8, 2, 128, 64], "peer_u": [4, 16384, 1024],
    "peer_v": [4, 16384, 1024], "ffn_ln_g": [4, 1024], "ffn_ln_b": [4, 1024],
}


def consts():
    cs = {}
    cs["identb"] = np.eye(128, dtype=np.float32).astype(ml_dtypes.bfloat16)
    cs["identf"] = np.eye(128, dtype=np.float32)
    cs["iota16"] = np.tile(np.arange(16, dtype=np.float32)[None, :], (128, 1))
    cs.update(rope_consts())
    cs.update(even_consts())
    return cs


def shared_alloc(c, S, A):
    S.wstage = [c.sb("wst%d" % i, [128, 1024]) for i in range(2)]
    S.wsi = 0
    S.identb = c.sb("identb", [128, 128], BF16)
    S.identf = c.sb("identf", [128, 128])
    S.ln_st = c.sb("ln_st", [128, 2, 6])
    S.ln_mv = c.sb("ln_mv", [128, 2])
    S.ln_sd = c.sb("ln_sd", [128, 4])
    c.dma("sp", S.identb[:], A["identb"], writes=[S.identb])
    c.dma("sp", S.identf[:], A["identf"], writes=[S.identf])
    S.gi = 0


def build(NB=4, layers=(0, 1, 2, 3), mode="full"):
    nc = bass.Bass("TRN2", target_bir_lowering=False)
    NT = NB * T
    ntiles = NT // 128
    A = {}
    A["x"] = nc.dram_tensor("x", [NT, D], F32, kind="ExternalInput").ap()
    for k, shp in WSPEC.items():
        A[k] = nc.dram_tensor(k, shp, F32, kind="ExternalInput").ap()
    for k, v in consts().items():
        A[k] = nc.dram_tensor(k, list(v.shape), BF16 if v.dtype == ml_dtypes.bfloat16 else F32, kind="ExternalInput").ap()
    y = nc.dram_tensor("y", [NT, D], F32, kind="ExternalOutput").ap()
    if mode == "mixer":
        oT = nc.dram_tensor("oT_out", [ntiles, 128, 1024], BF16, kind="ExternalOutput").ap()
    elif mode == "tail":
        oT = nc.dram_tensor("oT_in", [ntiles, 128, 1024], BF16, kind="ExternalInput").ap()
    else:
        oT = nc.dram_tensor("oT_s", [ntiles, 128, 1024], BF16, kind="Internal").ap()
    xs = nc.dram_tensor("xs_s", [NT, D], F32, kind="Internal").ap()
    PR_s = nc.dram_tensor("PR_s", [NB, 14, 128, T], BF16, kind="Internal").ap()
    V_s = nc.dram_tensor("V_s", [NB, 128, 16 * 128], BF16, kind="Internal").ap()
    WI_s = nc.dram_tensor("WI_s", [NB, 128, 16 * 8], F32, kind="Internal").ap()
    UV_s = nc.dram_tensor("UV_s", [16384, 2048], BF16, kind="Internal").ap()
    EF_s = nc.dram_tensor("EF_s", [NB, 19, 128, T], F32, kind="Internal").ap()
    GV_s = nc.dram_tensor("GV_s", [NB, 16, 128, 512], BF16, kind="Internal").ap()
    GG_s = nc.dram_tensor("GG_s", [NB, 16, 128, 512], F32, kind="Internal").ap()
    es = ExitStack()
    with es:
        c = Ctx(nc, es)
        S = Shared()
        shared_alloc(c, S, A)
        S.xs_tok = c.tok("xs_tok")
        S.oT_tok = c.tok("oT_tok")
        S.y_tok = c.tok("y_tok")
        S.pr_tok = c.tok("pr_tok")
        S.PR_s, S.V_s, S.WI_s = PR_s, V_s, WI_s
        S.EF_s, S.GV_s, S.GG_s = EF_s, GV_s, GG_s
        S.UV_s = UV_s
        S.uv_tok = c.tok("uv_tok")
        for li, layer in enumerate(layers):
            x_in = A["x"] if li == 0 else xs
            last = (li == len(layers) - 1)
            if mode != "tail":
                with ExitStack() as es2:
                    c.es = es2
                    if layer % 2 == 0:
                        even_stage(c, S, A, layer, x_in, oT, NB)
                    else:
                        odd_stage(c, S, A, layer, x_in, oT, NB)
                    c.barrier()
            if mode == "mixer":
                continue
            uv_prep(c, S, A, layer)
            with ExitStack() as es2:
                c.es = es2
                tail_alloc(c, S)
                tail_weights(c, S, A, layer)
                dst = y if last else xs

                def mkfront(g):
                    return tail_front(c, S, A, layer, x_in[g * 128:(g + 1) * 128, :], oT[g], g % 2)

                for _ in mkfront(0):
                    pass
                for g in range(ntiles):
                    fgen = mkfront(g + 1) if g + 1 < ntiles else iter(())
                    tail_back(c, S, g % 2, dst[g * 128:(g + 1) * 128, :], S.y_tok if last else S.xs_tok, fgen,
                              (g + 1) % 2 if g + 1 < ntiles else None, g > 0)
                c.barrier()
            c.es = es
        c.finish([S.y_tok, S.xs_tok, S.oT_tok], "sp")
        c.barrier()
        print("ninst", c.ninst, "nsem", c.nsem)
    return nc


def rope_consts():
    def tabs(dim, reps):
        inv = (10000.0 ** (-np.arange(0, dim, 2, dtype=np.float32) / dim)).astype(np.float32)
        ang = np.arange(T, dtype=np.float32)[None, :] * inv[:, None]
        co, si = np.cos(ang).astype(np.float32), np.sin(ang).astype(np.float32)
        co = np.concatenate([co, co], 0)
        si = np.concatenate([si, si], 0)
        return np.ascontiguousarray(np.tile(co, (reps, 1))), np.ascontiguousarray(np.tile(si, (reps, 1)))
    cA, sA = tabs(128, 1)
    cI, sI = tabs(64, 2)
    tri = np.where(np.arange(128)[None, :] <= np.arange(128)[:, None], 0.0, NEG).astype(np.float32)
    return {"ropetab": np.ascontiguousarray(np.stack([cA, sA, cI, sI], 0)), "trineg": tri,
            "onesb": np.ones((128, 128), np.float32).astype(ml_dtypes.bfloat16)}


def odd_stage(c, S, A, layer, x_in, oT, NB):
    nc = c.nc
    i = layer // 2
    PR = S.PR_s
    VS = S.V_s
    WS = S.WI_s
    with ExitStack() as esA:
        c.es = esA
        win = c.sb("o_win", [128, 8, 1864], BF16)
        wrot = c.sb("o_wrot", [128, 8, 1792], BF16)
        wki2 = c.sb("o_wki2", [128, 8, 128], BF16)
        load_w_bf16(c, S, win, A["odd_w_in"][i], 8, 1864)
        for k in range(8):
            for (s0, d0, n, half) in ((0, 0, 1152, 64), (1280, 1152, 512, 32)):
                src = win[:, k, s0:s0 + n].rearrange("p (h two j) -> p h two j", two=2, j=half)
                dst = wrot[:, k, d0:d0 + n].rearrange("p (h two j) -> p h two j", two=2, j=half)
                c.op("dve", lambda e, src=src, dst=dst: e.tensor_scalar(dst[:, :, 0, :], src[:, :, 1, :], -1.0, None, op0=ALU.mult),
                     reads=[win], writes=[wrot])
                c.op("pool", lambda e, src=src, dst=dst: e.tensor_copy(dst[:, :, 1, :], src[:, :, 0, :]), reads=[win], writes=[wrot])
            for hlf in range(2):
                c.op("pool", lambda e, k=k, hlf=hlf: e.tensor_copy(wki2[:, k, hlf * 64:(hlf + 1) * 64], win[:, k, 1792:1856]),
                     reads=[win], writes=[wki2])
                c.op("dve", lambda e, k=k, hlf=hlf: e.tensor_scalar(wrot[:, k, 1664 + hlf * 64:1696 + hlf * 64], win[:, k, 1824:1856], -1.0, None, op0=ALU.mult),
                     reads=[win], writes=[wrot])
                c.op("pool", lambda e, k=k, hlf=hlf: e.tensor_copy(wrot[:, k, 1696 + hlf * 64:1728 + hlf * 64], win[:, k, 1792:1824]),
                     reads=[win], writes=[wrot])
        groups = []
        for h in range(8):
            groups.append((win, h * 128, wrot, h * 128, 0))
        groups.append((win, 1024, wrot, 1024, 0))
        for g in range(4):
            groups.append((win, 1280 + g * 128, wrot, 1152 + g * 128, 1))
        groups.append((wki2, 0, wrot, 1664, 1))
        xt = [c.sb("o_xt%d" % j, [128, 1024]) for j in range(2)]
        xb = [c.sb("o_xb%d" % j, [128, 1024], BF16) for j in range(2)]
        xTb = c.sb("o_xTb", [128, 8, 512], BF16)
        tabs = c.sb("o_tabs", [128, 4, 512])
        t1 = [c.sb("o_t1%d" % j, [128, 512]) for j in range(2)]
        t2 = [c.sb("o_t2%d" % j, [128, 512]) for j in range(2)]
        stg = [c.sb("o_stg%d" % j, [128, 512], BF16) for j in range(3)]
        vt = [c.sb("o_vt%d" % j, [128, 128], BF16) for j in range(2)]
        wt = [c.sb("o_wt%d" % j, [128, 8]) for j in range(2)]
        psQ = [c.ps("o_psQ%d" % j, [128, 512]) for j in range(2)]
        psR = [c.ps("o_psR%d" % j, [128, 512]) for j in range(2)]
        psT = c.ps("o_psT", [128, 1024], BF16)
        psV = c.ps("o_psV", [128, 512])
        cnt = 0
        for b in range(NB):
            for tb in range(4):
                c.dma("sp", tabs[:], A["ropetab"][:, :, tb * 512:(tb + 1) * 512].rearrange("f p t -> p f t"), writes=[tabs])
                for tt in range(4):
                    g = b * 16 + tb * 4 + tt
                    X, XB = xt[tt % 2], xb[tt % 2]
                    c.dma(c.qsel(), X[:], x_in[g * 128:(g + 1) * 128, :], writes=[X], reads=[S.xs_tok])
                    c.op("act", lambda e, X=X, XB=XB: e.copy(XB[:], X[:]), reads=[X], writes=[XB])
                    for k in range(8):
                        c.op("pe", lambda e, k=k, XB=XB: e.transpose(psT[:, k * 128:(k + 1) * 128], XB[:, k * 128:(k + 1) * 128], S.identb[:]),
                             reads=[XB, S.identb], writes=[psT])
                    c.op("dve", lambda e, tt=tt: e.tensor_copy(xTb[:, :, tt * 128:(tt + 1) * 128], psT[:].rearrange("p (k t) -> p k t", t=128)),
                         reads=[psT], writes=[xTb])
                for tt in range(4):
                    g = tb * 4 + tt
                    for k in range(8):
                        c.op("pe", lambda e, k=k, tt=tt: e.matmul(psV[:, 0:128], xTb[:, k, tt * 128:(tt + 1) * 128], win[:, k, 1152:1280],
                                                                    start=(k == 0), stop=(k == 7)), reads=[xTb, win], writes=[psV])
                    for k in range(8):
                        c.op("pe", lambda e, k=k, tt=tt: e.matmul(psV[:, 128:136], xTb[:, k, tt * 128:(tt + 1) * 128], win[:, k, 1856:1864],
                                                                    start=(k == 0), stop=(k == 7)), reads=[xTb, win], writes=[psV])
                    V, W = vt[tt % 2], wt[tt % 2]
                    c.op("act", lambda e, V=V: e.copy(V[:], psV[:, 0:128]), reads=[psV], writes=[V])
                    c.op("act", lambda e, W=W: e.copy(W[:], psV[:, 128:136]), reads=[psV], writes=[W])
                    c.dma("sp", VS[b, :, g * 128:(g + 1) * 128], V[:], reads=[V], writes=[S.pr_tok], wtok=V)
                    c.dma("sp", WS[b, :, g * 8:(g + 1) * 8], W[:], reads=[W], writes=[S.pr_tok], wtok=W)
                for gi, (w0, c0, w1, c1, tab) in enumerate(groups):
                    pq, pr = psQ[cnt % 2], psR[cnt % 2]
                    a1, a2, sg = t1[cnt % 2], t2[cnt % 2], stg[cnt % 3]
                    cnt += 1
                    for k in range(8):
                        c.op("pe", lambda e, k=k, w0=w0, c0=c0, pq=pq: e.matmul(pq[:], w0[:, k, c0:c0 + 128], xTb[:, k, :], start=(k == 0), stop=(k == 7)),
                             reads=[w0, xTb], writes=[pq])
                    for k in range(8):
                        c.op("pe", lambda e, k=k, w1=w1, c1=c1, pr=pr: e.matmul(pr[:], w1[:, k, c1:c1 + 128], xTb[:, k, :], start=(k == 0), stop=(k == 7)),
                             reads=[w1, xTb], writes=[pr])
                    c.op("dve", lambda e, pq=pq, a1=a1, tab=tab: e.tensor_tensor(a1[:], pq[:], tabs[:, 2 * tab, :], ALU.mult), reads=[pq, tabs], writes=[a1])
                    c.op("dve", lambda e, pr=pr, a2=a2, tab=tab: e.tensor_tensor(a2[:], pr[:], tabs[:, 2 * tab + 1, :], ALU.mult), reads=[pr, tabs], writes=[a2])
                    c.op("pool", lambda e, a1=a1, a2=a2, sg=sg: e.tensor_tensor(sg[:], a1[:], a2[:], ALU.add), reads=[a1, a2], writes=[sg])
                    c.dma(c.qsel(), PR[b, gi, :, tb * 512:(tb + 1) * 512], sg[:], reads=[sg], writes=[S.pr_tok], wtok=sg)
        c.barrier()
    with ExitStack() as esB:
        c.es = esB
        qT = c.sb("o_qT", [128, 8, T], BF16)
        kT = c.sb("o_kT", [128, T], BF16)
        qiT = c.sb("o_qiT", [128, 4, T], BF16)
        kiT = c.sb("o_kiT", [128, T], BF16)
        vtk = c.sb("o_vtk", [128, 16, 128], BF16)
        wi = c.sb("o_wi", [128, 16, 8])
        scP = [c.sb("o_sc%d" % j, [128, T]) for j in range(2)]
        workP = [c.sb("o_work%d" % j, [128, T]) for j in range(2)]
        tmpr = [c.sb("o_tmpr%d" % j, [128, 1024]) for j in range(2)]
        m8P = [c.sb("o_m8%d" % j, [128, 8]) for j in range(2)]
        thr0 = c.sb("o_thr0", [128, 1])
        maskfP = [c.sb("o_maskf%d" % j, [128, T], BF16) for j in range(2)]
        maskTP = [c.sb("o_maskT%d" % j, [128, 16, 128], BF16) for j in range(2)]
        E = [c.sb("o_E%d" % j, [128, 1024], BF16) for j in range(2)]
        PT = [c.sb("o_PT%d" % j, [128, 1024], BF16) for j in range(2)]
        rden = c.sb("o_rden", [128, 1024])
        oTt = [c.sb("o_oTt%d" % j, [128, 1024], BF16) for j in range(2)]
        trineg = c.sb("o_tri", [128, 128])
        onesb = c.sb("o_ones", [128, 128], BF16)
        psI = c.ps("o_psI", [128, 1024])
        psL = c.ps("o_psL", [128, 1024])
        psO = c.ps("o_psO", [128, 1024])
        psD = c.ps("o_psD", [128, 1024])
        psDb = psD[:].bitcast(BF16)
        c.dma("sp", trineg[:], A["trineg"], writes=[trineg])
        c.dma("sp", onesb[:], A["onesb"], writes=[onesb])
        c.op("pool", lambda e: e.memset(thr0[:], -1e29), writes=[thr0])
        SCALE = float(128 ** -0.5)
        for b in range(NB):
            for h in range(8):
                c.dma(c.qsel(), qT[:, h, :], PR[b, h], writes=[qT], reads=[S.pr_tok])
            c.dma(c.qsel(), kT[:], PR[b, 8], writes=[kT], reads=[S.pr_tok])
            for g in range(4):
                c.dma(c.qsel(), qiT[:, g, :], PR[b, 9 + g], writes=[qiT], reads=[S.pr_tok])
            c.dma(c.qsel(), kiT[:], PR[b, 13], writes=[kiT], reads=[S.pr_tok])
            c.dma(c.qsel(), vtk[:].rearrange("p g d -> p (g d)"), VS[b], writes=[vtk], reads=[S.pr_tok])
            c.dma(c.qsel(), wi[:].rearrange("p g d -> p (g d)"), WS[b], writes=[wi], reads=[S.pr_tok])
            def sA(qt):
                Sk = (qt + 1) * 128
                q0 = qt * 128
                sc, work, m8, maskf, maskT = scP[qt % 2], workP[qt % 2], m8P[qt % 2], maskfP[qt % 2], maskTP[qt % 2]
                for h in range(8):
                    hh, g = h % 2, h // 2
                    for blk in range((Sk + 1023) // 1024):
                        k0 = blk * 1024
                        n = min(1024, Sk - k0)
                        for sub in range((n + 511) // 512):
                            s0 = k0 + sub * 512
                            m = min(512, Sk - s0)
                            c.op("pe", lambda e, hh=hh, g=g, s0=s0, m=m, sub=sub: e.matmul(
                                psI[:, sub * 512:sub * 512 + m], qiT[hh * 64:(hh + 1) * 64, g, q0:q0 + 128],
                                kiT[hh * 64:(hh + 1) * 64, s0:s0 + m], start=True, stop=True),
                                reads=[qiT, kiT], writes=[psI])
                        if h == 0:
                            c.op("dve", lambda e, k0=k0, n=n, h=h: e.tensor_scalar(sc[:, k0:k0 + n], psI[:, 0:n], 0.0, wi[:, qt, h:h + 1], op0=ALU.max, op1=ALU.mult),
                                 reads=[psI, wi], writes=[sc])
                        else:
                            tr = tmpr[(h + blk) % 2]
                            c.op("dve", lambda e, k0=k0, n=n, h=h, tr=tr: e.tensor_scalar(tr[:, 0:n], psI[:, 0:n], 0.0, wi[:, qt, h:h + 1], op0=ALU.max, op1=ALU.mult),
                                 reads=[psI, wi], writes=[tr])
                            c.op("pool", lambda e, k0=k0, n=n, tr=tr: e.tensor_tensor(sc[:, k0:k0 + n], sc[:, k0:k0 + n], tr[:, 0:n], ALU.add),
                                 reads=[tr, sc], writes=[sc])
                c.op("pool", lambda e: e.tensor_tensor(sc[:, q0:q0 + 128], sc[:, q0:q0 + 128], trineg[:], ALU.add), reads=[sc, trineg], writes=[sc])
                if Sk <= 256:
                    thr_ap, thr_tok = thr0[:, 0:1], thr0
                else:
                    c.op("dve", lambda e: e.max(m8[:], sc[:, 0:Sk]), reads=[sc], writes=[m8])
                    c.op("dve", lambda e: e.match_replace(work[:, 0:Sk], m8[:], sc[:, 0:Sk], NEG), reads=[sc, m8], writes=[work])
                    for r in range(1, 32):
                        c.op("dve", lambda e: e.max(m8[:], work[:, 0:Sk]), reads=[work], writes=[m8])
                        if r < 31:
                            c.op("dve", lambda e: e.match_replace(work[:, 0:Sk], m8[:], work[:, 0:Sk], NEG), reads=[work, m8], writes=[work])
                    thr_ap, thr_tok = m8[:, 7:8], m8
                c.op("dve", lambda e, thr_ap=thr_ap: e.tensor_scalar(maskf[:, 0:Sk], sc[:, 0:Sk], thr_ap, None, op0=ALU.is_ge),
                     reads=[sc, thr_tok], writes=[maskf])
            def sB(qt):
                Sk = (qt + 1) * 128
                q0 = qt * 128
                sc, work, m8, maskf, maskT = scP[qt % 2], workP[qt % 2], m8P[qt % 2], maskfP[qt % 2], maskTP[qt % 2]
                for cb in range((qt + 8) // 8):
                    c0 = cb * 8
                    nch = min(8, qt + 1 - c0)
                    for j in range(nch):
                        c.op("pe", lambda e, j=j, c0=c0: e.transpose(psDb[:, j * 128:(j + 1) * 128], maskf[:, (c0 + j) * 128:(c0 + j + 1) * 128], S.identb[:]),
                             reads=[maskf, S.identb], writes=[psD])
                    c.op("act", lambda e, c0=c0, nch=nch: e.copy(maskT[:, c0:c0 + nch, :].rearrange("p c t -> p (c t)"), psDb[:, 0:nch * 128]),
                         reads=[psD], writes=[maskT])
            def sC(qt):
                Sk = (qt + 1) * 128
                q0 = qt * 128
                sc, work, m8, maskf, maskT = scP[qt % 2], workP[qt % 2], m8P[qt % 2], maskfP[qt % 2], maskTP[qt % 2]
                for ch in range(qt + 1):
                    Ej, Pj = E[ch % 2], PT[ch % 2]
                    for hf in range(2):
                        c.op("pe", lambda e, ch=ch, hf=hf: e.matmul(psL[:, hf * 512:(hf + 1) * 512], kT[:, ch * 128:(ch + 1) * 128],
                                                                      qT[:, hf * 4:(hf + 1) * 4, q0:q0 + 128], start=True, stop=True),
                             reads=[kT, qT], writes=[psL])
                    c.op("act", lambda e, Ej=Ej: e.activation(Ej[:], psL[:], AF.Exp, scale=SCALE), reads=[psL], writes=[Ej])
                    c.op("pool", lambda e, Ej=Ej, Pj=Pj, ch=ch: e.tensor_tensor(Pj[:].rearrange("p (h t) -> p h t", t=128), Ej[:].rearrange("p (h t) -> p h t", t=128),
                                                                               maskT[:, ch, :].unsqueeze(1).to_broadcast([128, 8, 128]), ALU.mult),
                         reads=[Ej, maskT], writes=[Pj])
                    for hf in range(2):
                        c.op("pe", lambda e, ch=ch, hf=hf, Pj=Pj: e.matmul(psO[:, hf * 512:(hf + 1) * 512], vtk[:, ch, :], Pj[:, hf * 512:(hf + 1) * 512],
                                                                             start=(ch == 0), stop=(ch == qt)), reads=[vtk, Pj], writes=[psO])
                        c.op("pe", lambda e, ch=ch, hf=hf, Pj=Pj: e.matmul(psD[:, hf * 512:(hf + 1) * 512], onesb[:], Pj[:, hf * 512:(hf + 1) * 512],
                                                                             start=(ch == 0), stop=(ch == qt)), reads=[onesb, Pj], writes=[psD])
                c.op("dve", lambda e: e.reciprocal(rden[:], psD[:]), reads=[psD], writes=[rden])
                O = oTt[qt % 2]
                c.op("dve", lambda e, O=O: e.tensor_tensor(O[:], psO[:], rden[:], ALU.mult), reads=[psO, rden], writes=[O])
                c.dma("sp", oT[b * 16 + qt], O[:], reads=[O], writes=[S.oT_tok], wtok=O)
            sA(0)
            sB(0)
            for qt in range(16):
                if qt + 1 < 16:
                    sA(qt + 1)
                sC(qt)
                if qt + 1 < 16:
                    sB(qt + 1)
        c.barrier()


def even_consts():
    j = np.arange(128)[:, None]
    i_ = np.arange(128)[None, :]
    bc = ((j // 64 == i_ // 64) & (j <= i_)).astype(np.float32)
    bo = (j // 64 == i_ // 64).astype(np.float32)
    ah = np.zeros((128, 255), np.float32)
    ah[:, 127] = 1.0
    return {"bcmask": bc, "blockones": bo, "blockonesb": bo.astype(ml_dtypes.bfloat16), "ahwin": ah.astype(ml_dtypes.bfloat16)}


EV_GROUPS = ([(c0, 128, False, 0) for c0 in (0, 128, 256, 384)] + [(1536, 16, False, 0)] +
             [(1552 + p * 128, 128, True, p * 128) for p in range(4)] +
             [(2064 + p * 128, 128, True, 512 + p * 128) for p in range(4)] +
             [(2576 + p * 128, 128, True, 1024 + p * 128) for p in range(4)] +
             [(3088, 64, True, 1536), (3152, 96, True, 1600)])


def even_phaseA(c, S, A, layer, x_in, NB):
    i = layer // 2
    EF, GV, GG = S.EF_s, S.GV_s, S.GG_s
    with ExitStack() as esA:
        c.es = esA
        win = c.sb("e_win", [128, 8, 3248], BF16)
        load_w_bf16(c, S, win, A["even_w_in"][i], 8, 3248)
        mu = c.sb("e_mu", [128, 19])
        for gi, (c0, n, sh, rc) in enumerate(EV_GROUPS):
            if sh:
                c.dma(c.qsel(), mu[0:n, gi:gi + 1], A["rwkv_mu"][i:i + 1, rc:rc + n].rearrange("o n -> n o"), writes=[mu])
        xt = [c.sb("e_xt%d" % j, [128, 1024]) for j in range(2)]
        xb = [c.sb("e_xb%d" % j, [128, 1024], BF16) for j in range(2)]
        xT = c.sb("e_xT", [128, 8, 513], BF16)
        t1 = [c.sb("e_t1%d" % j, [128, 512]) for j in range(2)]
        t2 = [c.sb("e_t2%d" % j, [128, 512]) for j in range(2)]
        stg = [c.sb("e_stg%d" % j, [128, 512]) for j in range(3)]
        vst = [c.sb("e_vst%d" % j, [128, 512], BF16) for j in range(2)]
        gst = [c.sb("e_gst%d" % j, [128, 512]) for j in range(2)]
        psQ = [c.ps("e_psQ%d" % j, [128, 512]) for j in range(2)]
        psR = [c.ps("e_psR%d" % j, [128, 512]) for j in range(2)]
        psT = c.ps("e_psT", [128, 1024], BF16)
        psV = c.ps("e_psV", [128, 1024])
        cnt = 0
        for b in range(NB):
            c.op("pool", lambda e: e.memset(xT[:, :, 0:1], 0.0), writes=[xT])
            for tb in range(4):
                if tb > 0:
                    c.op("pool", lambda e: e.tensor_copy(xT[:, :, 0:1], xT[:, :, 512:513]), reads=[xT], writes=[xT])
                for tt in range(4):
                    g = b * 16 + tb * 4 + tt
                    X, XB = xt[tt % 2], xb[tt % 2]
                    c.dma(c.qsel(), X[:], x_in[g * 128:(g + 1) * 128, :], writes=[X], reads=[S.xs_tok])
                    c.op("act", lambda e, X=X, XB=XB: e.copy(XB[:], X[:]), reads=[X], writes=[XB])
                    for k in range(8):
                        c.op("pe", lambda e, k=k, XB=XB: e.transpose(psT[:, k * 128:(k + 1) * 128], XB[:, k * 128:(k + 1) * 128], S.identb[:]),
                             reads=[XB, S.identb], writes=[psT])
                    c.op("dve", lambda e, tt=tt: e.tensor_copy(xT[:, :, 1 + tt * 128:1 + (tt + 1) * 128], psT[:].rearrange("p (k t) -> p k t", t=128)),
                         reads=[psT], writes=[xT])
                for tt in range(4):
                    g = tb * 4 + tt
                    for hf, c0 in enumerate((512, 1024)):
                        for k in range(8):
                            c.op("pe", lambda e, k=k, tt=tt, hf=hf, c0=c0: e.matmul(psV[:, hf * 512:(hf + 1) * 512], xT[:, k, 1 + tt * 128:1 + (tt + 1) * 128],
                                                                                      win[:, k, c0:c0 + 512], start=(k == 0), stop=(k == 7)),
                                 reads=[xT, win], writes=[psV])
                    V, G = vst[tt % 2], gst[tt % 2]
                    c.op("dve", lambda e, V=V: e.tensor_copy(V[:], psV[:, 0:512]), reads=[psV], writes=[V])
                    c.op("act", lambda e, G=G: e.activation(G[:], psV[:, 512:1024], AF.Silu), reads=[psV], writes=[G])
                    c.dma("sp", GV[b, g], V[:], reads=[V], writes=[S.pr_tok], wtok=V)
                    c.dma("sp", GG[b, g], G[:], reads=[G], writes=[S.pr_tok], wtok=G)
                for gi, (c0, n, sh, rc) in enumerate(EV_GROUPS):
                    pq, pr = psQ[cnt % 2], psR[cnt % 2]
                    a1, a2, sg = t1[cnt % 2], t2[cnt % 2], stg[cnt % 3]
                    cnt += 1
                    for k in range(8):
                        c.op("pe", lambda e, k=k, c0=c0, n=n, pq=pq: e.matmul(pq[0:n, :], win[:, k, c0:c0 + n], xT[:, k, 1:513], start=(k == 0), stop=(k == 7)),
                             reads=[win, xT], writes=[pq])
                    if not sh:
                        c.op("act", lambda e, pq=pq, sg=sg, n=n: e.copy(sg[0:n, :], pq[0:n, :]), reads=[pq], writes=[sg])
                    else:
                        for k in range(8):
                            c.op("pe", lambda e, k=k, c0=c0, n=n, pr=pr: e.matmul(pr[0:n, :], win[:, k, c0:c0 + n], xT[:, k, 0:512], start=(k == 0), stop=(k == 7)),
                                 reads=[win, xT], writes=[pr])
                        c.op("act", lambda e, pq=pq, a1=a1, n=n: e.copy(a1[0:n, :], pq[0:n, :]), reads=[pq], writes=[a1])
                        c.op("dve", lambda e, pr=pr, a1=a1, a2=a2, n=n: e.tensor_tensor(a2[0:n, :], pr[0:n, :], a1[0:n, :], ALU.subtract), reads=[pr, a1], writes=[a2])
                        c.op("dve", lambda e, a1=a1, a2=a2, sg=sg, n=n, gi=gi: e.scalar_tensor_tensor(sg[0:n, :], a2[0:n, :], mu[0:n, gi:gi + 1], a1[0:n, :], op0=ALU.mult, op1=ALU.add),
                             reads=[a1, a2, mu], writes=[sg])
                    c.dma(c.qsel(), EF[b, gi, 0:n, tb * 512:(tb + 1) * 512], sg[0:n, :], reads=[sg], writes=[S.pr_tok], wtok=sg)
        c.barrier()


def gla_phase(c, S, A, layer, oT, NB):
    i = layer // 2
    EF, GV, GG = S.EF_s, S.GV_s, S.GG_s
    with ExitStack() as esB:
        c.es = esB
        aw2 = c.sb("g_aw2", [16, 256])
        nab = c.sb("g_nab", [128, 2])
        ngb = c.sb("g_ngb", [128, 128])
        msk = c.sb("g_msk", [128, T])
        bcm = c.sb("g_bcm", [128, 128])
        qf = c.sb("g_qf", [128, T])
        kf = c.sb("g_kf", [128, T])
        gal = c.sb("g_gal", [16, T])
        cum = c.sb("g_cum", [128, T])
        ex = c.sb("g_ex", [128, T])
        dec = c.sb("g_dec", [128, 32])
        qz = c.sb("g_qz", [128, 16, 2, 128], BF16)
        kin = c.sb("g_kin", [128, T], BF16)
        kout = c.sb("g_kout", [128, T], BF16)
        ktok = [c.sb("g_ktok%d" % j, [128, 128], BF16) for j in range(2)]
        vt = [c.sb("g_vt%d" % j, [128, 512], BF16) for j in range(2)]
        gg = [c.sb("g_gg%d" % j, [128, 512]) for j in range(2)]
        At = [c.sb("g_At%d" % j, [128, 128], BF16) for j in range(2)]
        Sf = [c.sb("g_Sf%d" % j, [128, 128]) for j in range(2)]
        Sb = [c.sb("g_Sb%d" % j, [128, 128], BF16) for j in range(3)]
        osball = c.sb("g_osb", [128, 16, 512])
        ss = c.sb("g_ss", [128, 8])
        junk = c.sb("g_junk", [128, 128])
        gob = c.sb("g_gob", [128, 512], BF16)
        oTt = [c.sb("g_oTt%d" % j, [128, 512], BF16) for j in range(2)]
        psZ = c.ps("g_psZ", [128, 512])
        psA = c.ps("g_psA", [128, 128])
        psK = c.ps("g_psK", [128, 128], BF16)
        psKV = c.ps("g_psKV", [128, 256])
        psO = [c.ps("g_psO%d" % j, [128, 128]) for j in range(2)]
        psTt = c.ps("g_psT", [128, 512], BF16)
        c.dma("sp", aw2[:], A["gla_a_w2"][i], writes=[aw2])
        for p in range(2):
            c.dma("sp", nab[:, p:p + 1], A["gla_a_b"][i:i + 1, p * 128:(p + 1) * 128].rearrange("o n -> n o"), writes=[nab])
        c.op("dve", lambda e: e.tensor_scalar(nab[:], nab[:], -1.0, None, op0=ALU.mult), reads=[nab], writes=[nab])
        bcast_row(c, ngb, A["gla_norm_g"][i:i + 1, :], 128)
        c.dma("sp", bcm[:], A["bcmask"], writes=[bcm])
        c.op("pool", lambda e: e.memset(msk[:], 1.0), writes=[msk])
        c.op("pool", lambda e: e.memset(msk[:].rearrange("p (n c) -> p n c", c=64)[:, :, 0:1], 0.0), writes=[msk])
        c.op("pool", lambda e: e.memset(qz[:].rearrange("p a b c -> p (a b c)"), 0.0), writes=[qz])
        sbi = 0
        for b in range(NB):
            c.dma(c.qsel(), gal[:], EF[b, 4, 0:16, :], writes=[gal], reads=[S.pr_tok])
            for p in range(2):
                c.dma(c.qsel(), qf[:], EF[b, p], writes=[qf], reads=[S.pr_tok])
                c.dma(c.qsel(), kf[:], EF[b, 2 + p], writes=[kf], reads=[S.pr_tok])
                for tb in range(4):
                    c.op("pe", lambda e, tb=tb, p=p: e.matmul(psZ[:], aw2[:, p * 128:(p + 1) * 128], gal[:, tb * 512:(tb + 1) * 512], start=True, stop=True),
                         reads=[aw2, gal], writes=[psZ])
                    c.op("act", lambda e, tb=tb, p=p: e.activation(ex[:, tb * 512:(tb + 1) * 512], psZ[:], AF.Exp, scale=-1.0, bias=nab[:, p:p + 1]),
                         reads=[psZ, nab], writes=[ex])
                c.op("act", lambda e: e.activation(ex[:], ex[:], AF.Ln, bias=1.0), reads=[ex], writes=[ex])
                c.op("dve", lambda e: e.tensor_scalar(ex[:], ex[:], -1.0 / 16.0, None, op0=ALU.mult), reads=[ex], writes=[ex])
                c.op("dve", lambda e: e.tensor_tensor_scan(cum[:], msk[:], ex[:], 0.0, ALU.mult, ALU.add), reads=[msk, ex], writes=[cum])
                cum3 = cum[:].rearrange("p (n c) -> p n c", c=64)
                c.op("act", lambda e: e.activation(dec[:], cum3[:, :, 63], AF.Exp), reads=[cum], writes=[dec])
                c.op("act", lambda e: e.activation(ex[:], cum[:], AF.Exp), reads=[cum], writes=[ex])
                for par in range(2):
                    src_e = ex[:].rearrange("p (t two c) -> p t two c", two=2, c=64)[:, :, par, :]
                    src_q = qf[:].rearrange("p (t two c) -> p t two c", two=2, c=64)[:, :, par, :]
                    c.op("dve", lambda e, par=par, src_e=src_e, src_q=src_q: e.scalar_tensor_tensor(
                        qz[:, :, par, par * 64:(par + 1) * 64], src_q, 0.125, src_e, op0=ALU.mult, op1=ALU.mult),
                        reads=[qf, ex], writes=[qz])
                c.op("act", lambda e: e.activation(ex[:], cum[:], AF.Exp, scale=-1.0), reads=[cum], writes=[ex])
                c.op("dve", lambda e: e.tensor_tensor(kin[:], kf[:], ex[:], ALU.mult), reads=[kf, ex], writes=[kin])
                c.op("dve", lambda e: e.tensor_tensor(ex[:].rearrange("p (n c) -> p n c", c=64), cum3[:, :, 63:64].to_broadcast([128, 32, 64]), cum3, ALU.subtract),
                     reads=[cum], writes=[ex])
                c.op("act", lambda e: e.activation(ex[:], ex[:], AF.Exp), reads=[ex], writes=[ex])
                c.op("dve", lambda e: e.tensor_tensor(kout[:], kf[:], ex[:], ALU.mult), reads=[kf, ex], writes=[kout])
                Sc = Sf[0]
                c.op("pool", lambda e, Sc=Sc: e.memset(Sc[:], 0.0), writes=[Sc])
                S0b = Sb[sbi % 3]; sbi += 1
                c.op("pool", lambda e, S0b=S0b: e.memset(S0b[:], 0.0), writes=[S0b])
                for tt in range(16):
                    V, Gg = vt[tt % 2], gg[tt % 2]
                    c.dma(c.qsel(), V[:], GV[b, tt], writes=[V], reads=[S.pr_tok])
                    if p == 1:
                        c.dma(c.qsel(), Gg[:], GG[b, tt], writes=[Gg], reads=[S.pr_tok])
                    t0 = tt * 128
                    KT = ktok[tt % 2]
                    c.op("pe", lambda e, t0=t0: e.transpose(psK[:], kout[:, t0:t0 + 128], S.identb[:]), reads=[kout, S.identb], writes=[psK])
                    c.op("act", lambda e, KT=KT: e.copy(KT[:], psK[:]), reads=[psK], writes=[KT])
                    Sbs = [S0b]
                    Scur = Sc
                    for ch in range(2):
                        n = tt * 2 + ch
                        c.op("pe", lambda e, ch=ch, KT=KT, V=V, p=p: e.matmul(psKV[:], KT[ch * 64:(ch + 1) * 64, :], V[ch * 64:(ch + 1) * 64, p * 256:(p + 1) * 256],
                                                                               start=True, stop=True), reads=[KT, V], writes=[psKV])
                        Snew = Sf[(tt * 2 + ch + 1) % 2]
                        for hh in range(2):
                            c.op("dve", lambda e, hh=hh, n=n, Scur=Scur, Snew=Snew: e.scalar_tensor_tensor(
                                Snew[hh * 64:(hh + 1) * 64, :], Scur[hh * 64:(hh + 1) * 64, :], dec[hh * 64:(hh + 1) * 64, n:n + 1],
                                psKV[hh * 64:(hh + 1) * 64, hh * 128:(hh + 1) * 128], op0=ALU.mult, op1=ALU.add),
                                reads=[Scur, dec, psKV], writes=[Snew])
                        Sn_b = Sb[sbi % 3]; sbi += 1
                        c.op("act", lambda e, Snew=Snew, Sn_b=Sn_b: e.copy(Sn_b[:], Snew[:]), reads=[Snew], writes=[Sn_b])
                        Sbs.append(Sn_b)
                        Scur = Snew
                    Sc = Scur
                    for hh in range(2):
                        h = p * 2 + hh
                        pl, ph = hh * 64, (hh + 1) * 64
                        AT = At[hh]
                        PO = psO[hh]
                        for par in range(2):
                            c.op("pe", lambda e, pl=pl, ph=ph, t0=t0, par=par, tt=tt: e.matmul(psA[:], kin[pl:ph, t0:t0 + 128], qz[pl:ph, tt, par, :],
                                                                                               start=(par == 0), stop=(par == 1)),
                                 reads=[kin, qz], writes=[psA])
                        c.op("dve", lambda e, AT=AT: e.tensor_tensor(AT[:], psA[:], bcm[:], ALU.mult), reads=[psA, bcm], writes=[AT])
                        c.op("pe", lambda e, AT=AT, V=V, h=h, PO=PO: e.matmul(PO[:], AT[:], V[:, h * 128:(h + 1) * 128], start=True, stop=False),
                             reads=[AT, V], writes=[PO])
                        for ch in range(2):
                            c.op("pe", lambda e, ch=ch, pl=pl, ph=ph, PO=PO, sbv=Sbs[ch]: e.matmul(PO[:], qz[pl:ph, tt, ch, :], sbv[pl:ph, :], start=False, stop=(ch == 1)),
                                 reads=[qz, Sbs[ch]], writes=[PO])
                        c.op("act", lambda e, PO=PO, h=h, tt=tt: e.copy(osball[:, tt, h * 128:(h + 1) * 128], PO[:]), reads=[PO], writes=[osball])
                    S0b = Sbs[2]
                    if p == 1:
                        for h in range(4):
                            c.op("act", lambda e, h=h, tt=tt: e.activation(junk[:], osball[:, tt, h * 128:(h + 1) * 128], AF.Square, accum_out=ss[:, h:h + 1]),
                                 reads=[osball], writes=[junk, ss])
                        c.op("dve", lambda e: e.tensor_scalar(ss[:, 0:4], ss[:, 0:4], 1.0 / 128.0, LN_EPS, op0=ALU.mult, op1=ALU.add), reads=[ss], writes=[ss])
                        c.op("act", lambda e: e.activation(ss[:, 0:4], ss[:, 0:4], AF.Sqrt), reads=[ss], writes=[ss])
                        c.op("dve", lambda e: e.reciprocal(ss[:, 4:8], ss[:, 0:4]), reads=[ss], writes=[ss])
                        for h in range(4):
                            c.op("dve", lambda e, h=h, tt=tt: e.scalar_tensor_tensor(osball[:, tt, h * 128:(h + 1) * 128], osball[:, tt, h * 128:(h + 1) * 128], ss[:, 4 + h:5 + h], ngb[:],
                                                                                      op0=ALU.mult, op1=ALU.mult), reads=[osball, ss, ngb], writes=[osball])
                        c.op("pool", lambda e, tt=tt, Gg=Gg: e.tensor_tensor(gob[:], osball[:, tt, :], Gg[:], ALU.mult), reads=[osball, Gg], writes=[gob])
                        for h in range(4):
                            c.op("pe", lambda e, h=h: e.transpose(psTt[:, h * 128:(h + 1) * 128], gob[:, h * 128:(h + 1) * 128], S.identb[:]),
                                 reads=[gob, S.identb], writes=[psTt])
                        O = oTt[tt % 2]
                        c.op("act", lambda e, O=O: e.copy(O[:], psTt[:]), reads=[psTt], writes=[O])
                        c.dma("sp", oT[b * 16 + tt][:, 0:512], O[:], reads=[O], writes=[S.oT_tok], wtok=O)
        c.barrier()


def even_stage(c, S, A, layer, x_in, oT, NB):
    even_phaseA(c, S, A, layer, x_in, NB)
    gla_phase(c, S, A, layer, oT, NB)
    rwkv_phase(c, S, A, layer, oT, NB)


def rwkv_phase(c, S, A, layer, oT, NB):
    i = layer // 2
    EF = S.EF_s
    NBH = NB * 4
    NF = NBH * 64
    PW = max(NF, 512)
    chunks = [(c0, min(NF, c0 + 512)) for c0 in range(0, NF, 512)]
    DEC_SCALE = -float(np.exp(-0.5))
    with ExitStack() as esB:
        c.es = esB
        HG = NBH // 2
        HW = HG * 64
        PWH = max(HW, 512)
        Zq = [[c.sb("r_Z%d_%d" % (q, j), [128, HW]) for j in range(2)] for q in range(2)]
        tA = [c.sb("r_tA%d" % q, [128, HW], BF16) for q in range(2)]
        tB = [c.sb("r_tB%d" % q, [128, HW]) for q in range(2)]
        tP = [c.sb("r_tP%d" % q, [128, HW]) for q in range(2)]
        tD = [c.sb("r_tD%d" % q, [128, HW], BF16) for q in range(2)]
        vs = [c.sb("r_vs%d" % q, [128, HW]) for q in range(2)]
        tCn = [[c.sb("r_tCn%d_%d" % (q, j), [128, HW]) for j in range(2)] for q in range(2)]
        WOP, AOP, BOP, KOP, ROP = [c.sb("r_op%d" % j, [128, 128, NBH]) for j in range(5)]
        vtok = c.sb("r_vtok", [128, NBH, 128], BF16)
        bonus = c.sb("r_bonus", [128, NBH, 128])
        gT = c.sb("r_gT", [128, NBH, 128])
        ysb = c.sb("r_ysb", [128, NBH * 2, 64])
        ysq = c.sb("r_ysq", [128, NBH * 2, 64])
        yst = c.sb("r_yst", [128, NBH * 2, 2])
        w2b = c.sb("r_w2b", [32, 512], BF16)
        a2b = c.sb("r_a2b", [64, 512], BF16)
        g2b = c.sb("r_g2b", [96, 512], BF16)
        pp = c.sb("r_pp", [128, 4, 8])
        bof = c.sb("r_bof", [128, 128])
        bob = c.sb("r_bob", [128, 128], BF16)
        ahw = c.sb("r_ahw", [128, 255], BF16)
        rT = [c.sb("r_rT%d" % j, [128, 128]) for j in range(2)]
        kTt = [c.sb("r_kT%d" % j, [128, 128]) for j in range(2)]
        vT = [c.sb("r_vT%d" % j, [128, 128]) for j in range(2)]
        wlal = c.sb("r_wlal", [64, 128])
        glt = c.sb("r_glt", [96, 128])
        twb = c.sb("r_twb", [64, 128], BF16)
        sglb = c.sb("r_sglb", [96, 128], BF16)
        e1 = c.sb("r_e1", [128, 128])
        e2 = c.sb("r_e2", [128, 128])
        e3 = c.sb("r_e3", [128, 128])
        e4 = c.sb("r_e4", [128, 128])
        asb = c.sb("r_asb", [128, 128])
        yo = [c.sb("r_yo%d" % j, [128, 128]) for j in range(2)]
        yob = [c.sb("r_yob%d" % j, [128, 128], BF16) for j in range(2)]
        psSAq = [c.ps("r_psSA%d" % q, [128, PWH]) for q in range(2)]
        psVBq = [c.ps("r_psVB%d" % q, [128, PWH]) for q in range(2)]
        psYq = [[c.ps("r_psY%d_%d" % (q, j), [128, HW]) for j in range(2)] for q in range(2)]
        psSA, psVB = psSAq[0], psVBq[0]
        for (dst, nm, rows, pofs) in ((w2b, "rwkv_w2", 32, 0), (a2b, "rwkv_a2", 32, 32), (g2b, "rwkv_g2", 96, 0)):
            st = S.wstage[0]
            c.dma("sp", st[pofs:pofs + rows, 0:512], A[nm][i], writes=[st])
            c.op("dve", lambda e, dst=dst, st=st, rows=rows, pofs=pofs: e.tensor_copy(dst[pofs:pofs + rows, :], st[pofs:pofs + rows, 0:512]), reads=[st], writes=[dst])
        for j, nm in enumerate(("rwkv_w0", "rwkv_a0", "rwkv_k_k", "rwkv_k_a", None, "rwkv_r_k", "rwkv_lnx_g", "rwkv_lnx_b")):
            if nm is None:
                continue
            src = A[nm][i:i + 1] if nm != "rwkv_r_k" else A[nm][i:i + 1].rearrange("o h d -> o (h d)")
            for hp in range(4):
                c.dma(c.qsel(), pp[:, hp, j:j + 1], src[:, hp * 128:(hp + 1) * 128].rearrange("o n -> n o"), writes=[pp])
        c.op("dve", lambda e: e.tensor_scalar(pp[:, :, 4], pp[:, :, 3], -1.0, 1.0, op0=ALU.mult, op1=ALU.add), reads=[pp], writes=[pp])
        c.dma("sp", bof[:], A["blockones"], writes=[bof])
        c.dma("sp", bob[:], A["blockonesb"], writes=[bob])
        c.dma("sp", ahw[:], A["ahwin"], writes=[ahw])
        for q in range(2):
            c.op("pool", lambda e, q=q: e.memset(Zq[q][0][:], 0.0), writes=[Zq[q][0]])
        step = 0
        for tb in range(16):
            t0 = tb * 128
            for b in range(NB):
                c.dma(c.qsel(), wlal[:], EF[b, 17, 0:64, t0:t0 + 128], writes=[wlal], reads=[S.pr_tok])
                c.dma(c.qsel(), glt[:], EF[b, 18, 0:96, t0:t0 + 128], writes=[glt], reads=[S.pr_tok])
                c.op("act", lambda e: e.activation(twb[0:32, :], wlal[0:32, :], AF.Tanh), reads=[wlal], writes=[twb])
                c.op("act", lambda e: e.copy(twb[32:64, :], wlal[32:64, :]), reads=[wlal], writes=[twb])
                c.op("act", lambda e: e.activation(sglb[:], glt[:], AF.Sigmoid), reads=[glt], writes=[sglb])
                for hp in range(4):
                    bh = b * 4 + hp
                    R_, K_, V_ = rT[bh % 2], kTt[bh % 2], vT[bh % 2]
                    c.dma(c.qsel(), R_[:], EF[b, 5 + hp, :, t0:t0 + 128], writes=[R_], reads=[S.pr_tok])
                    c.dma(c.qsel(), K_[:], EF[b, 9 + hp, :, t0:t0 + 128], writes=[K_], reads=[S.pr_tok])
                    c.dma(c.qsel(), V_[:], EF[b, 13 + hp, :, t0:t0 + 128], writes=[V_], reads=[S.pr_tok])
                    cs = slice(hp * 128, (hp + 1) * 128)
                    c.op("pe", lambda e, cs=cs: e.matmul(psSA[:, 0:128], w2b[0:32, cs], twb[0:32, :], start=True, stop=True), reads=[w2b, twb], writes=[psSA])
                    c.op("act", lambda e, hp=hp: e.activation(e1[:], psSA[:, 0:128], AF.Sigmoid, bias=pp[:, hp, 0:1]), reads=[psSA, pp], writes=[e1])
                    c.op("act", lambda e, bh=bh: e.activation(WOP[:, :, bh], e1[:], AF.Exp, scale=DEC_SCALE), reads=[e1], writes=[WOP])
                    c.op("pe", lambda e, cs=cs: e.matmul(psSA[:, 128:256], a2b[32:64, cs], twb[32:64, :], start=True, stop=True), reads=[a2b, twb], writes=[psSA])
                    c.op("act", lambda e, hp=hp: e.activation(asb[:], psSA[:, 128:256], AF.Sigmoid, bias=pp[:, hp, 1:2]), reads=[psSA, pp], writes=[asb])
                    c.op("pe", lambda e, cs=cs: e.matmul(psVB[:, 0:128], g2b[0:96, cs], sglb[0:96, :], start=True, stop=True), reads=[g2b, sglb], writes=[psVB])
                    c.op("act", lambda e, bh=bh: e.copy(gT[:, bh, :], psVB[:, 0:128]), reads=[psVB], writes=[gT])
                    c.op("dve", lambda e, K_=K_, hp=hp: e.tensor_scalar(e2[:], K_[:], pp[:, hp, 2:3], None, op0=ALU.mult), reads=[K_, pp], writes=[e2])
                    c.op("pool", lambda e: e.tensor_tensor(e3[:], e2[:], e2[:], ALU.mult), reads=[e2], writes=[e3])
                    c.op("pe", lambda e: e.matmul(psVB[:, 128:256], bof[:], e3[:], start=True, stop=True), reads=[bof, e3], writes=[psVB])
                    c.op("act", lambda e: e.activation(e3[:], psVB[:, 128:256], AF.Sqrt), reads=[psVB], writes=[e3])
                    c.op("dve", lambda e: e.tensor_scalar(e3[:], e3[:], 1e-12, None, op0=ALU.max), reads=[e3], writes=[e3])
                    c.op("dve", lambda e: e.reciprocal(e3[:], e3[:]), reads=[e3], writes=[e3])
                    c.op("dve", lambda e: e.tensor_tensor(e2[:], e2[:], e3[:], ALU.mult), reads=[e2, e3], writes=[e2])
                    c.op("dve", lambda e, bh=bh: e.tensor_scalar(AOP[:, :, bh], e2[:], -1.0, None, op0=ALU.mult), reads=[e2], writes=[AOP])
                    c.op("dve", lambda e, bh=bh: e.tensor_tensor(BOP[:, :, bh], e2[:], asb[:], ALU.mult), reads=[e2, asb], writes=[BOP])
                    c.op("dve", lambda e, hp=hp: e.tensor_scalar(e4[:], asb[:], pp[:, hp, 3:4], pp[:, hp, 4:5], op0=ALU.mult, op1=ALU.add), reads=[asb, pp], writes=[e4])
                    c.op("dve", lambda e, K_=K_: e.tensor_tensor(e4[:], e4[:], K_[:], ALU.mult), reads=[e4, K_], writes=[e4])
                    c.op("pool", lambda e, bh=bh: e.tensor_copy(KOP[:, :, bh], e4[:]), reads=[e4], writes=[KOP])
                    c.op("pool", lambda e, bh=bh, R_=R_: e.tensor_copy(ROP[:, :, bh], R_[:]), reads=[R_], writes=[ROP])
                    c.op("dve", lambda e, R_=R_, hp=hp: e.scalar_tensor_tensor(e1[:], R_[:], pp[:, hp, 5:6], e4[:], op0=ALU.mult, op1=ALU.mult), reads=[R_, pp, e4], writes=[e1])
                    c.op("pe", lambda e: e.matmul(psVB[:, 256:384], bof[:], e1[:], start=True, stop=True), reads=[bof, e1], writes=[psVB])
                    c.op("dve", lambda e, bh=bh, V_=V_: e.tensor_tensor(bonus[:, bh, :], psVB[:, 256:384], V_[:], ALU.mult), reads=[psVB, V_], writes=[bonus])
                    c.op("pe", lambda e, V_=V_: e.transpose(psSA[:, 256:384], V_[:], S.identf[:]), reads=[V_, S.identf], writes=[psSA])
                    c.op("act", lambda e, bh=bh: e.copy(vtok[:, bh, :], psSA[:, 256:384]), reads=[psSA], writes=[vtok])
            vt4 = vtok[:].rearrange("p g (h i) -> p g h i", h=2)

            def v3(tk):
                return tk[:, 0:HW].rearrange("p (g i) -> p g i", i=64)

            def bc(op_, tl, q):
                return op_[:, tl, q * HG:(q + 1) * HG].unsqueeze(2).to_broadcast([128, HG, 64])

            def lookahead_pe(tl):
                for q in range(2):
                    for hh in range(2):
                        c.op("pe", lambda e, q=q, hh=hh, tl=tl: e.matmul(psVBq[q][hh * 64:(hh + 1) * 64, 0:HW], S.identb[:, tl:tl + 1].to_broadcast([128, 64]),
                                                                         vt4[:, q * HG:(q + 1) * HG, hh, :], start=True, stop=True),
                             reads=[S.identb, vtok], writes=[psVBq[q]])
                    c.op("act", lambda e, q=q: e.copy(vs[q][:], psVBq[q][:, 0:HW]), reads=[psVBq[q]], writes=[vs[q]])

            def lookahead_pool(tl):
                for q in range(2):
                    TC = tCn[q][tl % 2]
                    c.op("pool", lambda e, tl=tl, TC=TC, q=q: e.tensor_tensor(v3(TC), v3(vs[q]), bc(KOP, tl, q), ALU.mult), reads=[vs[q], KOP], writes=[TC])

            def emit_tmpA(tl, par):
                for q in range(2):
                    Zi = Zq[q][par]
                    c.op("dve", lambda e, tl=tl, q=q, Zi=Zi: e.tensor_tensor(v3(tA[q]), v3(Zi), bc(AOP, tl, q), ALU.mult), reads=[Zi, AOP], writes=[tA[q]])

            lookahead_pe(0)
            lookahead_pool(0)
            emit_tmpA(0, step % 2)
            for tl in range(128):
                par = step % 2
                step += 1
                for q in range(2):
                    c.op("pe", lambda e, q=q: e.matmul(psSAq[q][:, 0:HW], bob[:], tA[q][:, 0:HW], start=True, stop=True), reads=[bob, tA[q]], writes=[psSAq[q]])
                if tl < 127:
                    lookahead_pe(tl + 1)
                for q in range(2):
                    Zi, TC = Zq[q][par], tCn[q][tl % 2]
                    c.op("pool", lambda e, tl=tl, q=q, Zi=Zi: e.tensor_tensor(v3(tP[q]), v3(Zi), bc(WOP, tl, q), ALU.mult), reads=[Zi, WOP], writes=[tP[q]])
                    c.op("pool", lambda e, q=q, TC=TC: e.tensor_tensor(tP[q][:], tP[q][:], TC[:], ALU.add), reads=[tP[q], TC], writes=[tP[q]])
                if tl < 127:
                    lookahead_pool(tl + 1)
                for q in range(2):
                    Zo = Zq[q][1 - par]
                    c.op("dve", lambda e, tl=tl, q=q: e.tensor_tensor(v3(tB[q]), v3(psSAq[q]), bc(BOP, tl, q), ALU.mult), reads=[psSAq[q], BOP], writes=[tB[q]])
                    c.op("dve", lambda e, q=q, Zo=Zo: e.tensor_tensor(Zo[:], tP[q][:], tB[q][:], ALU.add), reads=[tP[q], tB[q]], writes=[Zo])
                if tl < 127:
                    emit_tmpA(tl + 1, 1 - par)
                for q in range(2):
                    Zo = Zq[q][1 - par]
                    c.op("dve", lambda e, tl=tl, q=q, Zo=Zo: e.tensor_tensor(v3(tD[q]), v3(Zo), bc(ROP, tl, q), ALU.mult), reads=[Zo, ROP], writes=[tD[q]])
                    for hh in range(2):
                        c.op("pe", lambda e, q=q, hh=hh, tl=tl: e.matmul(psYq[q][hh][:, 0:HW], ahw[hh * 64:(hh + 1) * 64, 127 - tl:255 - tl],
                                                                         tD[q][hh * 64:(hh + 1) * 64, 0:HW], start=(tl == 0), stop=(tl == 127)),
                             reads=[ahw, tD[q]], writes=[psYq[q][hh]])
            ys4 = ysb[:].rearrange("p (g h) i -> p g h i", h=2)
            for q in range(2):
                for hh in range(2):
                    c.op("act", lambda e, hh=hh, q=q: e.copy(ys4[:, q * HG:(q + 1) * HG, hh, :], psYq[q][hh][:, 0:HW].rearrange("p (g i) -> p g i", i=64)),
                         reads=[psYq[q][hh]], writes=[ysb])
            G2 = NBH * 2
            c.op("dve", lambda e: e.tensor_reduce(yst[:, :, 0], ysb[:], AX.X, ALU.add), reads=[ysb], writes=[yst])
            c.op("dve", lambda e: e.tensor_scalar(yst[:, :, 0], yst[:, :, 0], 1.0 / 64.0, None, op0=ALU.mult), reads=[yst], writes=[yst])
            c.op("dve", lambda e: e.tensor_tensor(ysb[:], ysb[:], yst[:, :, 0:1].to_broadcast([128, G2, 64]), ALU.subtract), reads=[ysb, yst], writes=[ysb])
            c.op("pool", lambda e: e.tensor_tensor(ysq[:], ysb[:], ysb[:], ALU.mult), reads=[ysb], writes=[ysq])
            c.op("dve", lambda e: e.tensor_reduce(yst[:, :, 1], ysq[:], AX.X, ALU.add), reads=[ysq], writes=[yst])
            c.op("dve", lambda e: e.tensor_scalar(yst[:, :, 1], yst[:, :, 1], 1.0 / 64.0, 64e-5, op0=ALU.mult, op1=ALU.add), reads=[yst], writes=[yst])
            c.op("act", lambda e: e.activation(yst[:, :, 1], yst[:, :, 1], AF.Sqrt), reads=[yst], writes=[yst])
            c.op("dve", lambda e: e.reciprocal(yst[:, :, 1], yst[:, :, 1]), reads=[yst], writes=[yst])
            c.op("dve", lambda e: e.tensor_tensor(ysb[:], ysb[:], yst[:, :, 1:2].to_broadcast([128, G2, 64]), ALU.mult), reads=[ysb, yst], writes=[ysb])
            for b in range(NB):
                for hp in range(4):
                    bh = b * 4 + hp
                    Y, YB = yo[bh % 2], yob[bh % 2]
                    c.op("pe", lambda e, bh=bh: e.transpose(psSA[:, 0:128], ysb[:, 2 * bh:2 * bh + 2, :].rearrange("p h i -> p (h i)"), S.identf[:]),
                         reads=[ysb, S.identf], writes=[psSA])
                    c.op("dve", lambda e, Y=Y, hp=hp: e.tensor_scalar(Y[:], psSA[:, 0:128], pp[:, hp, 6:7], pp[:, hp, 7:8], op0=ALU.mult, op1=ALU.add), reads=[psSA, pp], writes=[Y])
                    c.op("pool", lambda e, Y=Y, bh=bh: e.tensor_tensor(Y[:], Y[:], bonus[:, bh, :], ALU.add), reads=[Y, bonus], writes=[Y])
                    c.op("pool", lambda e, Y=Y, YB=YB, bh=bh: e.tensor_tensor(YB[:], Y[:], gT[:, bh, :], ALU.mult), reads=[Y, gT], writes=[YB])
                    c.dma(c.qsel(), oT[b * 16 + tb][:, (4 + hp) * 128:(5 + hp) * 128], YB[:], reads=[YB], writes=[S.oT_tok], wtok=YB)
        c.barrier()


_NC_CACHE = {}


def kernel(**inputs):
    n = 8
    NB = 32 // n
    if "nc" not in _NC_CACHE:
        _NC_CACHE["nc"] = build(NB=NB, layers=(0, 1, 2, 3), mode="full")
    nc = _NC_CACHE["nc"]
    x = np.ascontiguousarray(inputs["x"], dtype=np.float32)
    cs = consts()
    in_maps = []
    for ci in range(n):
        m = {"x": x[ci * NB:(ci + 1) * NB].reshape(NB * T, D)}
        for k in WSPEC:
            m[k] = np.ascontiguousarray(inputs[k], dtype=np.float32)
        m.update(cs)
        in_maps.append(m)
    res = run_bass_kernel_spmd(nc, in_maps, core_ids=list(range(n)))
    out = np.stack([r["y"].reshape(NB, T, D) for r in res.results], 0).reshape(32, T, D)
    return out.astype(np.float32)
```

```python
import numpy as np
from contextlib import ExitStack
import concourse.bass as bass
import concourse.mybir as mybir
from concourse.bass_utils import run_bass_kernel_spmd
import ml_dtypes

F32 = mybir.dt.float32
BF16 = mybir.dt.bfloat16
U32 = mybir.dt.uint32
AF = mybir.ActivationFunctionType
ALU = mybir.AluOpType
AX = mybir.AxisListType

T = 2048
D = 1024
DEPTH = 4
ALPHA = float((2 * DEPTH) ** 0.25)
LN_EPS = 1e-5
NEG = -1e30
SEM_EPOCH = 60000


class Tok:
    __slots__ = ("w", "r", "t", "dsem", "dcnt", "name")

    def __init__(self, t=None, name=None):
        self.w = None
        self.r = []
        self.t = t
        self.dsem = None
        self.dcnt = 0
        self.name = name

    def __getitem__(self, k):
        return self.t[k]


class Ctx:
    def __init__(self, nc, es):
        self.nc = nc
        self.es = es
        self.es0 = es
        self.dpool = {}
        self.eng = {"pe": nc.tensor, "dve": nc.vector, "act": nc.scalar, "pool": nc.gpsimd, "sp": nc.sync}
        self.sem = {}
        self.cnt = {}
        for e in self.eng:
            self.sem[e] = es.enter_context(nc.semaphore("s_" + e))
            self.cnt[e] = 0
        self.waited = {e: {} for e in self.eng}
        self.pe_sems = {id(self.sem["pe"])}
        self.nsem = 0
        self.ninst = 0
        self.rr = 0
        self.uid = 0

    def sb(self, name, shape, dt=F32):
        self.uid += 1
        return Tok(self.es.enter_context(self.nc.sbuf_tensor("sb%d_%s" % (self.uid, name), list(shape), dt)), name)

    def ps(self, name, shape, dt=F32):
        self.uid += 1
        return Tok(self.es.enter_context(self.nc.psum_tensor("ps%d_%s" % (self.uid, name), list(shape), dt)), name)

    def tok(self, name=None):
        return Tok(None, name)

    def _dsem(self, tok):
        if tok.name not in self.dpool or self.dpool[tok.name][1] >= SEM_EPOCH:
            self.dpool[tok.name] = [self.es0.enter_context(self.nc.semaphore("d%d" % self.nsem)), 0]
            self.nsem += 1
        return self.dpool[tok.name]

    def barrier(self):
        for e in self.eng:
            for o in self.eng:
                if o != e and self.cnt[o] > 0:
                    self._wait(e, (self.sem[o], self.cnt[o]))
            for sem, cnt in self.dpool.values():
                if cnt > 0:
                    self._wait(e, (sem, cnt))

    def _wait(self, e, dep):
        if dep is None:
            return
        sem, v = dep
        k = id(sem)
        if False and e == "pe" and k in self.pe_sems:
            return
        if self.waited[e].get(k, 0) >= v:
            return
        self.waited[e][k] = v
        self.eng[e].wait_ge(sem, v)
        self.ninst += 1

    def _deps(self, e, reads, writes):
        for t in reads:
            self._wait(e, t.w)
        for t in writes:
            self._wait(e, t.w)
            for d in t.r:
                self._wait(e, d)

    @staticmethod
    def _compact(lst):
        best = {}
        for sem, v in lst:
            k = id(sem)
            if k not in best or best[k][1] < v:
                best[k] = (sem, v)
        return list(best.values())

    def _mark(self, me, reads, writes):
        for t in reads:
            t.r.append(me)
            if len(t.r) > 12:
                t.r = self._compact(t.r)
        for t in writes:
            t.w = me
            t.r = []

    def op(self, e, fn, reads=(), writes=()):
        self._deps(e, reads, writes)
        if self.cnt[e] >= SEM_EPOCH:
            self.sem[e] = self.es0.enter_context(self.nc.semaphore("s_%s_%d" % (e, self.nsem)))
            self.nsem += 1
            self.cnt[e] = 0
            if e == "pe":
                self.pe_sems.add(id(self.sem[e]))
        ins = fn(self.eng[e])
        self.cnt[e] += 1
        ins.then_inc(self.sem[e], 1)
        self.ninst += 1
        self._mark((self.sem[e], self.cnt[e]), reads, writes)
        return ins

    def dma(self, q, out_ap, in_ap, reads=(), writes=(), wtok=None, **kw):
        self._deps(q, reads, writes)
        tk = wtok or (writes[0] if writes else reads[0])
        ent = self._dsem(tk)
        ins = self.eng[q].dma_start(out=out_ap, in_=in_ap, **kw)
        ent[1] += 16
        ins.then_inc(ent[0], 16)
        self.ninst += 1
        self._mark((ent[0], ent[1]), reads, writes)
        return ins

    def gather(self, out_ap, in_ap, idx_ap, reads=(), writes=()):
        q = "pool"
        self._deps(q, reads, writes)
        tk = writes[0]
        ent = self._dsem(tk)
        ins = self.nc.gpsimd.indirect_dma_start(
            out=out_ap, out_offset=None, in_=in_ap,
            in_offset=bass.IndirectOffsetOnAxis(ap=idx_ap, axis=0))
        ent[1] += 16
        ins.then_inc(ent[0], 16)
        self.ninst += 1
        self._mark((ent[0], ent[1]), reads, writes)
        return ins

    def finish(self, toks, e="sp"):
        for t in toks:
            self._wait(e, t.w)
            for d in t.r:
                self._wait(e, d)

    def qsel(self):
        self.rr += 1
        return ("sp", "act")[self.rr % 2]


class Shared:
    pass


def load_w_bf16(c, S, dst, src, K, N, pofs=0, rows=128):
    for k in range(K):
        for n0 in range(0, N, 1024):
            n1 = min(N, n0 + 1024)
            S.wsi += 1
            st = S.wstage[S.wsi % 2]
            c.dma(c.qsel(), st[pofs:pofs + rows, 0:n1 - n0], src[k * rows:(k + 1) * rows, n0:n1], writes=[st])
            e = ("dve", "pool")[S.wsi % 2]
            c.op(e, lambda en, st=st, k=k, n0=n0, n1=n1: en.tensor_copy(dst[pofs:pofs + rows, k, n0:n1], st[pofs:pofs + rows, 0:n1 - n0]),
                 reads=[st], writes=[dst])


def bcast_row(c, dst, src_row, n):
    c.dma(c.qsel(), dst[:, 0:n], src_row.partition_broadcast(128), writes=[dst])


def layer_norm(c, S, out_ap, out_tok, z, g_bc, b_bc):
    st, mv, sd = S.ln_st, S.ln_mv, S.ln_sd
    c.op("dve", lambda e: e.bn_stats(st[:, 0, :], z[:, 0:512]), reads=[z], writes=[st])
    c.op("dve", lambda e: e.bn_stats(st[:, 1, :], z[:, 512:1024]), reads=[z], writes=[st])
    c.op("dve", lambda e: e.bn_aggr(mv[:], st[:]), reads=[st], writes=[mv])
    c.op("dve", lambda e: e.tensor_scalar(sd[:, 0:1], mv[:, 1:2], LN_EPS, None, op0=ALU.add), reads=[mv], writes=[sd])
    c.op("act", lambda e: e.activation(sd[:, 1:2], sd[:, 0:1], AF.Sqrt), reads=[sd], writes=[sd])
    c.op("dve", lambda e: e.reciprocal(sd[:, 2:3], sd[:, 1:2]), reads=[sd], writes=[sd])
    c.op("dve", lambda e: e.tensor_scalar(z[:], z[:], mv[:, 0:1], sd[:, 2:3], op0=ALU.subtract, op1=ALU.mult),
         reads=[z, mv, sd], writes=[z])
    c.op("pool", lambda e: e.tensor_tensor(z[:], z[:], g_bc[:], ALU.mult), reads=[z, g_bc], writes=[z])
    c.op("pool", lambda e: e.tensor_tensor(out_ap, z[:], b_bc[:], ALU.add), reads=[z, b_bc], writes=[out_tok])


NG = 12
GS = 4
PULLN = 1
PULLD = 1


def uv_prep(c, S, A, layer):
    with ExitStack() as esP:
        c.es = esP
        uf = [c.sb("p_uf%d" % j, [128, 1024]) for j in range(2)]
        vf = [c.sb("p_vf%d" % j, [128, 1024]) for j in range(2)]
        uvb = [c.sb("p_uvb%d" % j, [128, 2048], BF16) for j in range(2)]
        for r in range(128):
            U, V, B = uf[r % 2], vf[r % 2], uvb[r % 2]
            c.dma("sp", U[:], A["peer_u"][layer, r * 128:(r + 1) * 128, :], writes=[U])
            c.dma("act", V[:], A["peer_v"][layer, r * 128:(r + 1) * 128, :], writes=[V])
            c.op("dve", lambda e, U=U, B=B: e.tensor_copy(B[:, 0:1024], U[:]), reads=[U], writes=[B])
            c.op("pool", lambda e, V=V, B=B: e.tensor_copy(B[:, 1024:2048], V[:]), reads=[V], writes=[B])
            c.dma("sp", S.UV_s[r * 128:(r + 1) * 128, :], B[:], reads=[B], writes=[S.uv_tok], wtok=B)
        c.barrier()


def tail_alloc(c, S):
    S.wout = c.sb("wout", [128, 8, 1024], BF16)
    S.wq = c.sb("wq", [128, 8, 1024], BF16)
    S.keysT = c.sb("keysT", [128, 8, 128], BF16)
    S.lng = [c.sb("lng%d" % i, [128, 1024]) for i in range(4)]
    S.iota16 = c.sb("iota16", [128, 16])
    S.oT = c.sb("oT_sb", [128, 8, 128], BF16)
    S.xt = c.sb("xt", [128, 1024])
    S.z = c.sb("z", [128, 1024])
    S.xm2 = [c.sb("xm%d" % i, [128, 1024]) for i in range(2)]
    S.xmb2 = [c.sb("xmb%d" % i, [128, 1024], BF16) for i in range(2)]
    S.idxu2 = [c.sb("idxu%d" % i, [128, 128], U32) for i in range(2)]
    S.gate2 = [c.sb("gate%d" % i, [128, 8, 16]) for i in range(2)]
    S.xm, S.xmb = S.xm2[0], S.xmb2[0]
    S.xmT = c.sb("xmT", [128, 8, 128], BF16)
    S.qT = c.sb("qT", [128, 8, 128], BF16)
    S.ssc = c.sb("ssc", [128, 128])
    S.tops = c.sb("tops", [128, 16, 16])
    S.topi = c.sb("topi", [128, 16, 16], U32)
    S.topf = c.sb("topf", [128, 16, 16])
    S.cands = c.sb("cands", [128, 8, 256])
    S.candw = c.sb("candw", [128, 256])
    S.best = c.sb("best", [128, 8, 16])
    S.pos = c.sb("pos", [128, 8, 16], U32)
    S.pab = c.sb("pab", [128, 2, 128], U32)
    S.pabf = c.sb("pabf", [128, 2, 128])
    S.eq = [c.sb("eq%d" % i, [128, 128, 16]) for i in range(2)]
    S.idx2 = c.sb("idx2", [128, 2, 128])
    S.idxf = c.sb("idxf", [128, 128])
    S.idxu, S.gate = S.idxu2[0], S.gate2[0]
    S.gsum = c.sb("gsum", [128, 8])
    S.hh4 = [c.sb("hh%d" % i, [128, GS]) for i in range(4)]
    S.coef4 = [c.sb("coef%d" % i, [128, GS]) for i in range(4)]
    S.junk2 = [c.sb("junk%d" % i, [128, 1024], BF16) for i in range(2)]
    S.acc = c.sb("acc", [128, 1024])
    S.G = [c.sb("G%d" % i, [128, 2048], BF16) for i in range(NG)]
    S.dg = [c.sb("dg%d" % i, [128, 128], BF16) for i in range(4)]
    S.ot = c.sb("ot", [128, 1024])
    S.psA = c.ps("psA", [128, 1024])
    S.psT = c.ps("psT", [128, 1024])
    S.psS = c.ps("psS", [128, 8, 128])
    S.psAcc = c.ps("psAcc", [128, 1024])


def tail_weights(c, S, A, layer):
    wo = A["even_w_out"][layer // 2] if layer % 2 == 0 else A["odd_w_out"][layer // 2]
    load_w_bf16(c, S, S.wout, wo, 8, 1024)
    load_w_bf16(c, S, S.wq, A["peer_w_q"][layer], 8, 1024)
    bcast_row(c, S.lng[0], A["mix_ln_g"][layer:layer + 1, :], 1024)
    bcast_row(c, S.lng[1], A["mix_ln_b"][layer:layer + 1, :], 1024)
    bcast_row(c, S.lng[2], A["ffn_ln_g"][layer:layer + 1, :], 1024)
    bcast_row(c, S.lng[3], A["ffn_ln_b"][layer:layer + 1, :], 1024)
    c.dma("sp", S.iota16[:], A["iota16"], writes=[S.iota16])
    sk = A["peer_sub_keys"][layer]
    for h in range(8):
        st = S.wstage[h % 2]
        for cc in range(2):
            c.dma(c.qsel(), st[:, cc * 64:(cc + 1) * 64], sk[h, cc], writes=[st])
        c.op("dve", lambda e, st=st: e.tensor_copy(S.xmb[:, 0:128], st[:, 0:128]), reads=[st], writes=[S.xmb])
        c.op("pe", lambda e: e.matmul(S.psT[:, 0:128], S.xmb[:, 0:128], S.identb[:], start=True, stop=True), reads=[S.xmb, S.identb], writes=[S.psT])
        c.op("act", lambda e, h=h: e.copy(S.keysT[:, h, :], S.psT[:, 0:128]), reads=[S.psT], writes=[S.keysT])


def tail_front(c, S, A, layer, x_src, oT_src, par):
    xm, xmb, idxu, gate = S.xm2[par], S.xmb2[par], S.idxu2[par], S.gate2[par]
    c.dma("sp", S.xt[:], x_src, writes=[S.xt], reads=[S.xs_tok])
    yield
    c.dma("act", S.oT[:].rearrange("p k t -> p (k t)"), oT_src, writes=[S.oT], reads=[S.oT_tok])
    yield
    for half in range(2):
        for k in range(8):
            c.op("pe", lambda e, k=k, half=half: e.matmul(S.psA[:, half * 512:(half + 1) * 512], S.oT[:, k, :],
                                                            S.wout[:, k, half * 512:(half + 1) * 512],
                                                            start=(k == 0), stop=(k == 7)),
                 reads=[S.oT, S.wout], writes=[S.psA])
            yield
    c.op("dve", lambda e: e.scalar_tensor_tensor(S.z[:], S.xt[:], ALPHA, S.psA[:], op0=ALU.mult, op1=ALU.add),
         reads=[S.xt, S.psA], writes=[S.z])
    yield
    layer_norm(c, S, xm[:], xm, S.z, S.lng[0], S.lng[1])
    yield
    c.op("act", lambda e: e.copy(xmb[:], xm[:]), reads=[xm], writes=[xmb])
    yield
    for k in range(8):
        c.op("pe", lambda e, k=k: e.matmul(S.psT[:, k * 128:(k + 1) * 128], xmb[:, k * 128:(k + 1) * 128], S.identb[:], start=True, stop=True),
             reads=[xmb, S.identb], writes=[S.psT])
        yield
    c.op("act", lambda e: e.copy(S.xmT[:].rearrange("p k t -> p (k t)"), S.psT[:]), reads=[S.psT], writes=[S.xmT])
    yield
    for h in range(8):
        for k in range(8):
            c.op("pe", lambda e, k=k, h=h: e.matmul(S.psA[:, h * 128:(h + 1) * 128], S.wq[:, k, h * 128:(h + 1) * 128],
                                                      S.xmT[:, k, :], start=(k == 0), stop=(k == 7)),
                 reads=[S.wq, S.xmT], writes=[S.psA])
            yield
    c.op("act", lambda e: e.copy(S.qT[:].rearrange("p k t -> p (k t)"), S.psA[:]), reads=[S.psA], writes=[S.qT])
    yield
    for hf in range(2):
        for j in range(8):
            hc = hf * 8 + j
            h, cc = hc // 2, hc % 2
            c.op("pe", lambda e, h=h, cc=cc, j=j: e.matmul(S.psS[:, j, :], S.qT[cc * 64:(cc + 1) * 64, h, :],
                                                            S.keysT[cc * 64:(cc + 1) * 64, h, :], start=True, stop=True),
                 reads=[S.qT, S.keysT], writes=[S.psS])
            yield
        for j in range(8):
            hc = hf * 8 + j
            c.op("dve", lambda e, hc=hc, j=j: e.max(S.tops[:, hc, 0:8], S.psS[:, j, :]), reads=[S.psS], writes=[S.tops])
            yield
            c.op("dve", lambda e, hc=hc, j=j: e.max_index(S.topi[:, hc, 0:8], S.tops[:, hc, 0:8], S.psS[:, j, :]),
                 reads=[S.psS, S.tops], writes=[S.topi])
            yield
            c.op("dve", lambda e, hc=hc, j=j: e.match_replace(S.ssc[:], S.tops[:, hc, 0:8], S.psS[:, j, :], NEG),
                 reads=[S.psS, S.tops], writes=[S.ssc])
            yield
            c.op("dve", lambda e, hc=hc: e.max(S.tops[:, hc, 8:16], S.ssc[:]), reads=[S.ssc], writes=[S.tops])
            yield
            c.op("dve", lambda e, hc=hc: e.max_index(S.topi[:, hc, 8:16], S.tops[:, hc, 8:16], S.ssc[:]),
                 reads=[S.ssc, S.tops], writes=[S.topi])
            yield
    c.op("dve", lambda e: e.tensor_copy(S.topf[:], S.topi[:]), reads=[S.topi], writes=[S.topf])
    yield
    tops4 = S.tops[:].rearrange("p (h c) k -> p h c k", c=2)
    topf4 = S.topf[:].rearrange("p (h c) k -> p h c k", c=2)
    c.op("dve", lambda e: e.tensor_scalar(topf4[:, :, 0, :], topf4[:, :, 0, :], 128.0, None, op0=ALU.mult),
         reads=[S.topf], writes=[S.topf])
    yield
    cs4 = S.cands[:].rearrange("p h (a b) -> p h a b", b=16)
    for h in range(8):
        c.op("pool", lambda e, h=h: e.tensor_tensor(cs4[:, h], tops4[:, h, 0, :].unsqueeze(2).to_broadcast([128, 16, 16]),
                                                     tops4[:, h, 1, :].unsqueeze(1).to_broadcast([128, 16, 16]), ALU.add),
             reads=[S.tops], writes=[S.cands])
        yield
    for h in range(8):
        c.op("dve", lambda e, h=h: e.max(S.best[:, h, 0:8], S.cands[:, h, :]), reads=[S.cands], writes=[S.best])
        yield
        c.op("dve", lambda e, h=h: e.max_index(S.pos[:, h, 0:8], S.best[:, h, 0:8], S.cands[:, h, :]), reads=[S.cands, S.best], writes=[S.pos])
        yield
        c.op("dve", lambda e, h=h: e.match_replace(S.candw[:], S.best[:, h, 0:8], S.cands[:, h, :], NEG),
             reads=[S.cands, S.best], writes=[S.candw])
        yield
        c.op("dve", lambda e, h=h: e.max(S.best[:, h, 8:16], S.candw[:]), reads=[S.candw], writes=[S.best])
        yield
        c.op("dve", lambda e, h=h: e.max_index(S.pos[:, h, 8:16], S.best[:, h, 8:16], S.candw[:]), reads=[S.candw, S.best], writes=[S.pos])
        yield
    posf = S.pos[:].rearrange("p h k -> p (h k)")
    c.op("dve", lambda e: e.tensor_scalar(S.pab[:, 0, :], posf, 4, None, op0=ALU.logical_shift_right), reads=[S.pos], writes=[S.pab])
    yield
    c.op("dve", lambda e: e.tensor_scalar(S.pab[:, 1, :], posf, 15, None, op0=ALU.bitwise_and), reads=[S.pos], writes=[S.pab])
    yield
    c.op("dve", lambda e: e.tensor_copy(S.pabf[:], S.pab[:]), reads=[S.pab], writes=[S.pabf])
    yield
    for ab in range(2):
        eq = S.eq[ab]
        c.op("dve", lambda e, ab=ab, eq=eq: e.tensor_tensor(eq[:], S.pabf[:, ab, :].unsqueeze(2).to_broadcast([128, 128, 16]),
                                                         S.iota16[:].unsqueeze(1).to_broadcast([128, 128, 16]), ALU.is_equal),
             reads=[S.pabf, S.iota16], writes=[eq])
        yield
        c.op("pool", lambda e, ab=ab, eq=eq: e.tensor_tensor(eq[:].rearrange("p (h k) a -> p h k a", h=8), eq[:].rearrange("p (h k) a -> p h k a", h=8),
                                                          topf4[:, :, ab, :].unsqueeze(2).to_broadcast([128, 8, 16, 16]), ALU.mult),
             reads=[S.topf, eq], writes=[eq])
        yield
        c.op("dve", lambda e, ab=ab, eq=eq: e.tensor_reduce(S.idx2[:, ab, :], eq[:], AX.X, ALU.add), reads=[eq], writes=[S.idx2])
        yield
    c.op("dve", lambda e: e.tensor_tensor(S.idxf[:], S.idx2[:, 0, :], S.idx2[:, 1, :], ALU.add), reads=[S.idx2], writes=[S.idxf])
    yield
    c.op("dve", lambda e: e.tensor_scalar(S.idxf[:], S.idxf[:], 16383.0, 0.0, op0=ALU.min, op1=ALU.max),
         reads=[S.idxf], writes=[S.idxf])
    yield
    c.op("dve", lambda e: e.tensor_copy(idxu[:], S.idxf[:]), reads=[S.idxf], writes=[idxu])
    yield
    c.op("dve", lambda e: e.tensor_tensor(gate[:], S.best[:], S.best[:, :, 0:1].to_broadcast([128, 8, 16]), ALU.subtract),
         reads=[S.best], writes=[gate])
    yield
    c.op("act", lambda e: e.activation(gate[:], gate[:], AF.Exp), reads=[gate], writes=[gate])
    yield
    c.op("dve", lambda e: e.tensor_reduce(S.gsum[:], gate[:], AX.X, ALU.add), reads=[gate], writes=[S.gsum])
    yield
    c.op("dve", lambda e: e.reciprocal(S.gsum[:], S.gsum[:]), reads=[S.gsum], writes=[S.gsum])
    yield
    c.op("dve", lambda e: e.tensor_tensor(gate[:], gate[:], S.gsum[:].unsqueeze(2).to_broadcast([128, 8, 16]), ALU.mult),
         reads=[gate, S.gsum], writes=[gate])
    yield
def tail_issue(c, S, par, k):
    G = S.G[(S.gi + k) % NG]
    c.gather(G[:], S.UV_s, S.idxu2[par][:, k:k + 1], reads=[S.idxu2[par], S.uv_tok], writes=[G])


def tail_back(c, S, par, out_dst, out_tok, fgen, nxt_par, prefetched):
    xm, xmb, idxu, gate = S.xm2[par], S.xmb2[par], S.idxu2[par], S.gate2[par]
    gate2 = gate[:].rearrange("p h k -> p (h k)")
    LOOK = NG - GS
    if not prefetched:
        for k in range(LOOK):
            tail_issue(c, S, par, k)
    issued = LOOK

    def pull(n):
        for _ in range(n):
            if next(fgen, "done") == "done":
                return
    for g0 in range(0, 128, GS):
        HH, CF = S.hh4[(g0 // GS) % 4], S.coef4[(g0 // GS) % 4]
        for k in range(g0, g0 + GS):
            G = S.G[(S.gi + k) % NG]
            JK = S.junk2[k % 2]
            c.op("dve", lambda e, G=G, k=k, JK=JK, HH=HH: e.scalar_tensor_tensor(JK[:], G[:, 0:1024], 1.0, xmb[:], op0=ALU.mult, op1=ALU.mult,
                                                                              accum_out=HH[:, k - g0:k - g0 + 1]),
                 reads=[G, xmb], writes=[JK, HH])
            pull(PULLD)
        c.op("act", lambda e, HH=HH, CF=CF: e.activation(CF[:], HH[:], AF.Gelu), reads=[HH], writes=[CF])
        c.op("dve", lambda e, g0=g0, CF=CF: e.tensor_tensor(CF[:], CF[:], gate2[:, g0:g0 + GS], ALU.mult),
             reads=[CF, gate], writes=[CF])
        for k in range(g0, g0 + GS):
            G = S.G[(S.gi + k) % NG]
            DG = S.dg[k % 4]
            c.op("act", lambda e, DG=DG, k=k, CF=CF: e.activation(DG[:], S.identb[:], AF.Copy, scale=CF[:, k - g0:k - g0 + 1]), reads=[S.identb, CF], writes=[DG])
            for half in range(2):
                c.op("pe", lambda e, DG=DG, G=G, half=half, k=k: e.matmul(S.psAcc[:, half * 512:(half + 1) * 512], DG[:],
                                                                          G[:, 1024 + half * 512:1536 + half * 512], start=(k == 0), stop=(k == 127)),
                     reads=[DG, G], writes=[S.psAcc])
            if issued < 128:
                tail_issue(c, S, par, issued)
                issued += 1
            pull(PULLN)
    pull(100000)
    S.gi += 128
    if nxt_par is not None:
        for k in range(LOOK):
            tail_issue(c, S, nxt_par, k)
    c.op("dve", lambda e: e.scalar_tensor_tensor(S.acc[:], xm[:], ALPHA, S.psAcc[:], op0=ALU.mult, op1=ALU.add),
         reads=[xm, S.psAcc], writes=[S.acc])
    layer_norm(c, S, S.ot[:], S.ot, S.acc, S.lng[2], S.lng[3])
    c.dma("sp", out_dst, S.ot[:], reads=[S.ot], writes=[out_tok], wtok=out_tok)


WSPEC = {
    "even_w_in": [2, 1024, 3248], "gla_a_w2": [2, 16, 256], "gla_a_b": [2, 256], "gla_norm_g": [2, 128],
    "rwkv_mu": [2, 1696], "rwkv_w0": [2, 512], "rwkv_w2": [2, 32, 512], "rwkv_a0": [2, 512],
    "rwkv_a2": [2, 32, 512], "rwkv_g2": [2, 96, 512], "rwkv_k_k": [2, 512], "rwkv_k_a": [2, 512],
    "rwkv_r_k": [2, 8, 64], "rwkv_lnx_g": [2, 512], "rwkv_lnx_b": [2, 512], "even_w_out": [2, 1024, 1024],
    "odd_w_in": [2, 1024, 1864], "odd_w_out": [2, 1024, 1024], "mix_ln_g": [4, 1024], "mix_ln_b": [4, 1024],
    "peer_w_q": [4, 1024, 1024], "peer_sub_keys": [4, 8, 2, 128, 64], "peer_u": [4, 16384, 1024],
    "peer_v": [4, 16384, 1024], "ffn_ln_g": [4, 1024], "ffn_ln_b": [4, 1024],
}


def consts():
    cs = {}
    cs["identb"] = np.eye(128, dtype=np.float32).astype(ml_dtypes.bfloat16)
    cs["identf"] = np.eye(128, dtype=np.float32)
    cs["iota16"] = np.tile(np.arange(16, dtype=np.float32)[None, :], (128, 1))
    cs.update(rope_consts())
    cs.update(even_consts())
    return cs


def shared_alloc(c, S, A):
    S.wstage = [c.sb("wst%d" % i, [128, 1024]) for i in range(2)]
    S.wsi = 0
    S.identb = c.sb("identb", [128, 128], BF16)
    S.identf = c.sb("identf", [128, 128])
    S.ln_st = c.sb("ln_st", [128, 2, 6])
    S.ln_mv = c.sb("ln_mv", [128, 2])
    S.ln_sd = c.sb("ln_sd", [128, 4])
    c.dma("sp", S.identb[:], A["identb"], writes=[S.identb])
    c.dma("sp", S.identf[:], A["identf"], writes=[S.identf])
    S.gi = 0


def build(NB=4, layers=(0, 1, 2, 3), mode="full"):
    nc = bass.Bass("TRN2", target_bir_lowering=False)
    NT = NB * T
    ntiles = NT // 128
    A = {}
    A["x"] = nc.dram_tensor("x", [NT, D], F32, kind="ExternalInput").ap()
    for k, shp in WSPEC.items():
        A[k] = nc.dram_tensor(k, shp, F32, kind="ExternalInput").ap()
    for k, v in consts().items():
        A[k] = nc.dram_tensor(k, list(v.shape), BF16 if v.dtype == ml_dtypes.bfloat16 else F32, kind="ExternalInput").ap()
    y = nc.dram_tensor("y", [NT, D], F32, kind="ExternalOutput").ap()
    if mode == "mixer":
        oT = nc.dram_tensor("oT_out", [ntiles, 128, 1024], BF16, kind="ExternalOutput").ap()
    elif mode == "tail":
        oT = nc.dram_tensor("oT_in", [ntiles, 128, 1024], BF16, kind="ExternalInput").ap()
    else:
        oT = nc.dram_tensor("oT_s", [ntiles, 128, 1024], BF16, kind="Internal").ap()
    xs = nc.dram_tensor("xs_s", [NT, D], F32, kind="Internal").ap()
    PR_s = nc.dram_tensor("PR_s", [NB, 14, 128, T], BF16, kind="Internal").ap()
    V_s = nc.dram_tensor("V_s", [NB, 128, 16 * 128], BF16, kind="Internal").ap()
    WI_s = nc.dram_tensor("WI_s", [NB, 128, 16 * 8], F32, kind="Internal").ap()
    UV_s = nc.dram_tensor("UV_s", [16384, 2048], BF16, kind="Internal").ap()
    EF_s = nc.dram_tensor("EF_s", [NB, 19, 128, T], F32, kind="Internal").ap()
    GV_s = nc.dram_tensor("GV_s", [NB, 16, 128, 512], BF16, kind="Internal").ap()
    GG_s = nc.dram_tensor("GG_s", [NB, 16, 128, 512], F32, kind="Internal").ap()
    es = ExitStack()
    with es:
        c = Ctx(nc, es)
        S = Shared()
        shared_alloc(c, S, A)
        S.xs_tok = c.tok("xs_tok")
        S.oT_tok = c.tok("oT_tok")
        S.y_tok = c.tok("y_tok")
        S.pr_tok = c.tok("pr_tok")
        S.PR_s, S.V_s, S.WI_s = PR_s, V_s, WI_s
        S.EF_s, S.GV_s, S.GG_s = EF_s, GV_s, GG_s
        S.UV_s = UV_s
        S.uv_tok = c.tok("uv_tok")
        for li, layer in enumerate(layers):
            x_in = A["x"] if li == 0 else xs
            last = (li == len(layers) - 1)
            if mode != "tail":
                with ExitStack() as es2:
                    c.es = es2
                    if layer % 2 == 0:
                        even_stage(c, S, A, layer, x_in, oT, NB)
                    else:
                        odd_stage(c, S, A, layer, x_in, oT, NB)
                    c.barrier()
            if mode == "mixer":
                continue
            uv_prep(c, S, A, layer)
            with ExitStack() as es2:
                c.es = es2
                tail_alloc(c, S)
                tail_weights(c, S, A, layer)
                dst = y if last else xs

                def mkfront(g):
                    return tail_front(c, S, A, layer, x_in[g * 128:(g + 1) * 128, :], oT[g], g % 2)

                for _ in mkfront(0):
                    pass
                for g in range(ntiles):
                    fgen = mkfront(g + 1) if g + 1 < ntiles else iter(())
                    tail_back(c, S, g % 2, dst[g * 128:(g + 1) * 128, :], S.y_tok if last else S.xs_tok, fgen,
                              (g + 1) % 2 if g + 1 < ntiles else None, g > 0)
                c.barrier()
            c.es = es
        c.finish([S.y_tok, S.xs_tok, S.oT_tok], "sp")
        c.barrier()
        print("ninst", c.ninst, "nsem", c.nsem)
    return nc


def rope_consts():
    def tabs(dim, reps):
        inv = (10000.0 ** (-np.arange(0, dim, 2, dtype=np.float32) / dim)).astype(np.float32)
        ang = np.arange(T, dtype=np.float32)[None, :] * inv[:, None]
        co, si = np.cos(ang).astype(np.float32), np.sin(ang).astype(np.float32)
        co = np.concatenate([co, co], 0)
        si = np.concatenate([si, si], 0)
        return np.ascontiguousarray(np.tile(co, (reps, 1))), np.ascontiguousarray(np.tile(si, (reps, 1)))
    cA, sA = tabs(128, 1)
    cI, sI = tabs(64, 2)
    tri = np.where(np.arange(128)[None, :] <= np.arange(128)[:, None], 0.0, NEG).astype(np.float32)
    return {"ropetab": np.ascontiguousarray(np.stack([cA, sA, cI, sI], 0)), "trineg": tri,
            "onesb": np.ones((128, 128), np.float32).astype(ml_dtypes.bfloat16)}


def odd_stage(c, S, A, layer, x_in, oT, NB):
    nc = c.nc
    i = layer // 2
    PR = S.PR_s
    VS = S.V_s
    WS = S.WI_s
    with ExitStack() as esA:
        c.es = esA
        win = c.sb("o_win", [128, 8, 1864], BF16)
        wrot = c.sb("o_wrot", [128, 8, 1792], BF16)
        wki2 = c.sb("o_wki2", [128, 8, 128], BF16)
        load_w_bf16(c, S, win, A["odd_w_in"][i], 8, 1864)
        for k in range(8):
            for (s0, d0, n, half) in ((0, 0, 1152, 64), (1280, 1152, 512, 32)):
                src = win[:, k, s0:s0 + n].rearrange("p (h two j) -> p h two j", two=2, j=half)
                dst = wrot[:, k, d0:d0 + n].rearrange("p (h two j) -> p h two j", two=2, j=half)
                c.op("dve", lambda e, src=src, dst=dst: e.tensor_scalar(dst[:, :, 0, :], src[:, :, 1, :], -1.0, None, op0=ALU.mult),
                     reads=[win], writes=[wrot])
                c.op("pool", lambda e, src=src, dst=dst: e.tensor_copy(dst[:, :, 1, :], src[:, :, 0, :]), reads=[win], writes=[wrot])
            for hlf in range(2):
                c.op("pool", lambda e, k=k, hlf=hlf: e.tensor_copy(wki2[:, k, hlf * 64:(hlf + 1) * 64], win[:, k, 1792:1856]),
                     reads=[win], writes=[wki2])
                c.op("dve", lambda e, k=k, hlf=hlf: e.tensor_scalar(wrot[:, k, 1664 + hlf * 64:1696 + hlf * 64], win[:, k, 1824:1856], -1.0, None, op0=ALU.mult),
                     reads=[win], writes=[wrot])
                c.op("pool", lambda e, k=k, hlf=hlf: e.tensor_copy(wrot[:, k, 1696 + hlf * 64:1728 + hlf * 64], win[:, k, 1792:1824]),
                     reads=[win], writes=[wrot])
        groups = []
        for h in range(8):
            groups.append((win, h * 128, wrot, h * 128, 0))
        groups.append((win, 1024, wrot, 1024, 0))
        for g in range(4):
            groups.append((win, 1280 + g * 128, wrot, 1152 + g * 128, 1))
        groups.append((wki2, 0, wrot, 1664, 1))
        xt = [c.sb("o_xt%d" % j, [128, 1024]) for j in range(2)]
        xb = [c.sb("o_xb%d" % j, [128, 1024], BF16) for j in range(2)]
        xTb = c.sb("o_xTb", [128, 8, 512], BF16)
        tabs = c.sb("o_tabs", [128, 4, 512])
        t1 = [c.sb("o_t1%d" % j, [128, 512]) for j in range(2)]
        t2 = [c.sb("o_t2%d" % j, [128, 512]) for j in range(2)]
        stg = [c.sb("o_stg%d" % j, [128, 512], BF16) for j in range(3)]
        vt = [c.sb("o_vt%d" % j, [128, 128], BF16) for j in range(2)]
        wt = [c.sb("o_wt%d" % j, [128, 8]) for j in range(2)]
        psQ = [c.ps("o_psQ%d" % j, [128, 512]) for j in range(2)]
        psR = [c.ps("o_psR%d" % j, [128, 512]) for j in range(2)]
        psT = c.ps("o_psT", [128, 1024], BF16)
        psV = c.ps("o_psV", [128, 512])
        cnt = 0
        for b in range(NB):
            for tb in range(4):
                c.dma("sp", tabs[:], A["ropetab"][:, :, tb * 512:(tb + 1) * 512].rearrange("f p t -> p f t"), writes=[tabs])
                for tt in range(4):
                    g = b * 16 + tb * 4 + tt
                    X, XB = xt[tt % 2], xb[tt % 2]
                    c.dma(c.qsel(), X[:], x_in[g * 128:(g + 1) * 128, :], writes=[X], reads=[S.xs_tok])
                    c.op("act", lambda e, X=X, XB=XB: e.copy(XB[:], X[:]), reads=[X], writes=[XB])
                    for k in range(8):
                        c.op("pe", lambda e, k=k, XB=XB: e.transpose(psT[:, k * 128:(k + 1) * 128], XB[:, k * 128:(k + 1) * 128], S.identb[:]),
                             reads=[XB, S.identb], writes=[psT])
                    c.op("dve", lambda e, tt=tt: e.tensor_copy(xTb[:, :, tt * 128:(tt + 1) * 128], psT[:].rearrange("p (k t) -> p k t", t=128)),
                         reads=[psT], writes=[xTb])
                for tt in range(4):
                    g = tb * 4 + tt
                    for k in range(8):
                        c.op("pe", lambda e, k=k, tt=tt: e.matmul(psV[:, 0:128], xTb[:, k, tt * 128:(tt + 1) * 128], win[:, k, 1152:1280],
                                                                    start=(k == 0), stop=(k == 7)), reads=[xTb, win], writes=[psV])
                    for k in range(8):
                        c.op("pe", lambda e, k=k, tt=tt: e.matmul(psV[:, 128:136], xTb[:, k, tt * 128:(tt + 1) * 128], win[:, k, 1856:1864],
                                                                    start=(k == 0), stop=(k == 7)), reads=[xTb, win], writes=[psV])
                    V, W = vt[tt % 2], wt[tt % 2]
                    c.op("act", lambda e, V=V: e.copy(V[:], psV[:, 0:128]), reads=[psV], writes=[V])
                    c.op("act", lambda e, W=W: e.copy(W[:], psV[:, 128:136]), reads=[psV], writes=[W])
                    c.dma("sp", VS[b, :, g * 128:(g + 1) * 128], V[:], reads=[V], writes=[S.pr_tok], wtok=V)
                    c.dma("sp", WS[b, :, g * 8:(g + 1) * 8], W[:], reads=[W], writes=[S.pr_tok], wtok=W)
                for gi, (w0, c0, w1, c1, tab) in enumerate(groups):
                    pq, pr = psQ[cnt % 2], psR[cnt % 2]
                    a1, a2, sg = t1[cnt % 2], t2[cnt % 2], stg[cnt % 3]
                    cnt += 1
                    for k in range(8):
                        c.op("pe", lambda e, k=k, w0=w0, c0=c0, pq=pq: e.matmul(pq[:], w0[:, k, c0:c0 + 128], xTb[:, k, :], start=(k == 0), stop=(k == 7)),
                             reads=[w0, xTb], writes=[pq])
                    for k in range(8):
                        c.op("pe", lambda e, k=k, w1=w1, c1=c1, pr=pr: e.matmul(pr[:], w1[:, k, c1:c1 + 128], xTb[:, k, :], start=(k == 0), stop=(k == 7)),
                             reads=[w1, xTb], writes=[pr])
                    c.op("dve", lambda e, pq=pq, a1=a1, tab=tab: e.tensor_tensor(a1[:], pq[:], tabs[:, 2 * tab, :], ALU.mult), reads=[pq, tabs], writes=[a1])
                    c.op("dve", lambda e, pr=pr, a2=a2, tab=tab: e.tensor_tensor(a2[:], pr[:], tabs[:, 2 * tab + 1, :], ALU.mult), reads=[pr, tabs], writes=[a2])
                    c.op("pool", lambda e, a1=a1, a2=a2, sg=sg: e.tensor_tensor(sg[:], a1[:], a2[:], ALU.add), reads=[a1, a2], writes=[sg])
                    c.dma(c.qsel(), PR[b, gi, :, tb * 512:(tb + 1) * 512], sg[:], reads=[sg], writes=[S.pr_tok], wtok=sg)
        c.barrier()
    with ExitStack() as esB:
        c.es = esB
        qT = c.sb("o_qT", [128, 8, T], BF16)
        kT = c.sb("o_kT", [128, T], BF16)
        qiT = c.sb("o_qiT", [128, 4, T], BF16)
        kiT = c.sb("o_kiT", [128, T], BF16)
        vtk = c.sb("o_vtk", [128, 16, 128], BF16)
        wi = c.sb("o_wi", [128, 16, 8])
        scP = [c.sb("o_sc%d" % j, [128, T]) for j in range(2)]
        workP = [c.sb("o_work%d" % j, [128, T]) for j in range(2)]
        tmpr = [c.sb("o_tmpr%d" % j, [128, 1024]) for j in range(2)]
        m8P = [c.sb("o_m8%d" % j, [128, 8]) for j in range(2)]
        thr0 = c.sb("o_thr0", [128, 1])
        maskfP = [c.sb("o_maskf%d" % j, [128, T], BF16) for j in range(2)]
        maskTP = [c.sb("o_maskT%d" % j, [128, 16, 128], BF16) for j in range(2)]
        E = [c.sb("o_E%d" % j, [128, 1024], BF16) for j in range(2)]
        PT = [c.sb("o_PT%d" % j, [128, 1024], BF16) for j in range(2)]
        rden = c.sb("o_rden", [128, 1024])
        oTt = [c.sb("o_oTt%d" % j, [128, 1024], BF16) for j in range(2)]
        trineg = c.sb("o_tri", [128, 128])
        onesb = c.sb("o_ones", [128, 128], BF16)
        psI = c.ps("o_psI", [128, 1024])
        psL = c.ps("o_psL", [128, 1024])
        psO = c.ps("o_psO", [128, 1024])
        psD = c.ps("o_psD", [128, 1024])
        psDb = psD[:].bitcast(BF16)
        c.dma("sp", trineg[:], A["trineg"], writes=[trineg])
        c.dma("sp", onesb[:], A["onesb"], writes=[onesb])
        c.op("pool", lambda e: e.memset(thr0[:], -1e29), writes=[thr0])
        SCALE = float(128 ** -0.5)
        for b in range(NB):
            for h in range(8):
                c.dma(c.qsel(), qT[:, h, :], PR[b, h], writes=[qT], reads=[S.pr_tok])
            c.dma(c.qsel(), kT[:], PR[b, 8], writes=[kT], reads=[S.pr_tok])
            for g in range(4):
                c.dma(c.qsel(), qiT[:, g, :], PR[b, 9 + g], writes=[qiT], reads=[S.pr_tok])
            c.dma(c.qsel(), kiT[:], PR[b, 13], writes=[kiT], reads=[S.pr_tok])
            c.dma(c.qsel(), vtk[:].rearrange("p g d -> p (g d)"), VS[b], writes=[vtk], reads=[S.pr_tok])
            c.dma(c.qsel(), wi[:].rearrange("p g d -> p (g d)"), WS[b], writes=[wi], reads=[S.pr_tok])
            def sA(qt):
                Sk = (qt + 1) * 128
                q0 = qt * 128
                sc, work, m8, maskf, maskT = scP[qt % 2], workP[qt % 2], m8P[qt % 2], maskfP[qt % 2], maskTP[qt % 2]
                for h in range(8):
                    hh, g = h % 2, h // 2
                    for blk in range((Sk + 1023) // 1024):
                        k0 = blk * 1024
                        n = min(1024, Sk - k0)
                        for sub in range((n + 511) // 512):
                            s0 = k0 + sub * 512
                            m = min(512, Sk - s0)
                            c.op("pe", lambda e, hh=hh, g=g, s0=s0, m=m, sub=sub: e.matmul(
                                psI[:, sub * 512:sub * 512 + m], qiT[hh * 64:(hh + 1) * 64, g, q0:q0 + 128],
                                kiT[hh * 64:(hh + 1) * 64, s0:s0 + m], start=True, stop=True),
                                reads=[qiT, kiT], writes=[psI])
                        if h == 0:
                            c.op("dve", lambda e, k0=k0, n=n, h=h: e.tensor_scalar(sc[:, k0:k0 + n], psI[:, 0:n], 0.0, wi[:, qt, h:h + 1], op0=ALU.max, op1=ALU.mult),
                                 reads=[psI, wi], writes=[sc])
                        else:
                            tr = tmpr[(h + blk) % 2]
                            c.op("dve", lambda e, k0=k0, n=n, h=h, tr=tr: e.tensor_scalar(tr[:, 0:n], psI[:, 0:n], 0.0, wi[:, qt, h:h + 1], op0=ALU.max, op1=ALU.mult),
                                 reads=[psI, wi], writes=[tr])
                            c.op("pool", lambda e, k0=k0, n=n, tr=tr: e.tensor_tensor(sc[:, k0:k0 + n], sc[:, k0:k0 + n], tr[:, 0:n], ALU.add),
                                 reads=[tr, sc], writes=[sc])
                c.op("pool", lambda e: e.tensor_tensor(sc[:, q0:q0 + 128], sc[:, q0:q0 + 128], trineg[:], ALU.add), reads=[sc, trineg], writes=[sc])
                if Sk <= 256:
                    thr_ap, thr_tok = thr0[:, 0:1], thr0
                else:
                    c.op("dve", lambda e: e.max(m8[:], sc[:, 0:Sk]), reads=[sc], writes=[m8])
                    c.op("dve", lambda e: e.match_replace(work[:, 0:Sk], m8[:], sc[:, 0:Sk], NEG), reads=[sc, m8], writes=[work])
                    for r in range(1, 32):
                        c.op("dve", lambda e: e.max(m8[:], work[:, 0:Sk]), reads=[work], writes=[m8])
                        if r < 31:
                            c.op("dve", lambda e: e.match_replace(work[:, 0:Sk], m8[:], work[:, 0:Sk], NEG), reads=[work, m8], writes=[work])
                    thr_ap, thr_tok = m8[:, 7:8], m8
                c.op("dve", lambda e, thr_ap=thr_ap: e.tensor_scalar(maskf[:, 0:Sk], sc[:, 0:Sk], thr_ap, None, op0=ALU.is_ge),
                     reads=[sc, thr_tok], writes=[maskf])
            def sB(qt):
                Sk = (qt + 1) * 128
                q0 = qt * 128
                sc, work, m8, maskf, maskT = scP[qt % 2], workP[qt % 2], m8P[qt % 2], maskfP[qt % 2], maskTP[qt % 2]
                for cb in range((qt + 8) // 8):
                    c0 = cb * 8
                    nch = min(8, qt + 1 - c0)
                    for j in range(nch):
                        c.op("pe", lambda e, j=j, c0=c0: e.transpose(psDb[:, j * 128:(j + 1) * 128], maskf[:, (c0 + j) * 128:(c0 + j + 1) * 128], S.identb[:]),
                             reads=[maskf, S.identb], writes=[psD])
                    c.op("act", lambda e, c0=c0, nch=nch: e.copy(maskT[:, c0:c0 + nch, :].rearrange("p c t -> p (c t)"), psDb[:, 0:nch * 128]),
                         reads=[psD], writes=[maskT])
            def sC(qt):
                Sk = (qt + 1) * 128
                q0 = qt * 128
                sc, work, m8, maskf, maskT = scP[qt % 2], workP[qt % 2], m8P[qt % 2], maskfP[qt % 2], maskTP[qt % 2]
                for ch in range(qt + 1):
                    Ej, Pj = E[ch % 2], PT[ch % 2]
                    for hf in range(2):
                        c.op("pe", lambda e, ch=ch, hf=hf: e.matmul(psL[:, hf * 512:(hf + 1) * 512], kT[:, ch * 128:(ch + 1) * 128],
                                                                      qT[:, hf * 4:(hf + 1) * 4, q0:q0 + 128], start=True, stop=True),
                             reads=[kT, qT], writes=[psL])
                    c.op("act", lambda e, Ej=Ej: e.activation(Ej[:], psL[:], AF.Exp, scale=SCALE), reads=[psL], writes=[Ej])
                    c.op("pool", lambda e, Ej=Ej, Pj=Pj, ch=ch: e.tensor_tensor(Pj[:].rearrange("p (h t) -> p h t", t=128), Ej[:].rearrange("p (h t) -> p h t", t=128),
                                                                               maskT[:, ch, :].unsqueeze(1).to_broadcast([128, 8, 128]), ALU.mult),
                         reads=[Ej, maskT], writes=[Pj])
                    for hf in range(2):
                        c.op("pe", lambda e, ch=ch, hf=hf, Pj=Pj: e.matmul(psO[:, hf * 512:(hf + 1) * 512], vtk[:, ch, :], Pj[:, hf * 512:(hf + 1) * 512],
                                                                             start=(ch == 0), stop=(ch == qt)), reads=[vtk, Pj], writes=[psO])
                        c.op("pe", lambda e, ch=ch, hf=hf, Pj=Pj: e.matmul(psD[:, hf * 512:(hf + 1) * 512], onesb[:], Pj[:, hf * 512:(hf + 1) * 512],
                                                                             start=(ch == 0), stop=(ch == qt)), reads=[onesb, Pj], writes=[psD])
                c.op("dve", lambda e: e.reciprocal(rden[:], psD[:]), reads=[psD], writes=[rden])
                O = oTt[qt % 2]
                c.op("dve", lambda e, O=O: e.tensor_tensor(O[:], psO[:], rden[:], ALU.mult), reads=[psO, rden], writes=[O])
                c.dma("sp", oT[b * 16 + qt], O[:], reads=[O], writes=[S.oT_tok], wtok=O)
            sA(0)
            sB(0)
            for qt in range(16):
                if qt + 1 < 16:
                    sA(qt + 1)
                sC(qt)
                if qt + 1 < 16:
                    sB(qt + 1)
        c.barrier()


def even_consts():
    j = np.arange(128)[:, None]
    i_ = np.arange(128)[None, :]
    bc = ((j // 64 == i_ // 64) & (j <= i_)).astype(np.float32)
    bo = (j // 64 == i_ // 64).astype(np.float32)
    ah = np.zeros((128, 255), np.float32)
    ah[:, 127] = 1.0
    return {"bcmask": bc, "blockones": bo, "blockonesb": bo.astype(ml_dtypes.bfloat16), "ahwin": ah.astype(ml_dtypes.bfloat16)}


EV_GROUPS = ([(c0, 128, False, 0) for c0 in (0, 128, 256, 384)] + [(1536, 16, False, 0)] +
             [(1552 + p * 128, 128, True, p * 128) for p in range(4)] +
             [(2064 + p * 128, 128, True, 512 + p * 128) for p in range(4)] +
             [(2576 + p * 128, 128, True, 1024 + p * 128) for p in range(4)] +
             [(3088, 64, True, 1536), (3152, 96, True, 1600)])


def even_phaseA(c, S, A, layer, x_in, NB):
    i = layer // 2
    EF, GV, GG = S.EF_s, S.GV_s, S.GG_s
    with ExitStack() as esA:
        c.es = esA
        win = c.sb("e_win", [128, 8, 3248], BF16)
        load_w_bf16(c, S, win, A["even_w_in"][i], 8, 3248)
        mu = c.sb("e_mu", [128, 19])
        for gi, (c0, n, sh, rc) in enumerate(EV_GROUPS):
            if sh:
                c.dma(c.qsel(), mu[0:n, gi:gi + 1], A["rwkv_mu"][i:i + 1, rc:rc + n].rearrange("o n -> n o"), writes=[mu])
        xt = [c.sb("e_xt%d" % j, [128, 1024]) for j in range(2)]
        xb = [c.sb("e_xb%d" % j, [128, 1024], BF16) for j in range(2)]
        xT = c.sb("e_xT", [128, 8, 513], BF16)
        t1 = [c.sb("e_t1%d" % j, [128, 512]) for j in range(2)]
        t2 = [c.sb("e_t2%d" % j, [128, 512]) for j in range(2)]
        stg = [c.sb("e_stg%d" % j, [128, 512]) for j in range(3)]
        vst = [c.sb("e_vst%d" % j, [128, 512], BF16) for j in range(2)]
        gst = [c.sb("e_gst%d" % j, [128, 512]) for j in range(2)]
        psQ = [c.ps("e_psQ%d" % j, [128, 512]) for j in range(2)]
        psR = [c.ps("e_psR%d" % j, [128, 512]) for j in range(2)]
        psT = c.ps("e_psT", [128, 1024], BF16)
        psV = c.ps("e_psV", [128, 1024])
        cnt = 0
        for b in range(NB):
            c.op("pool", lambda e: e.memset(xT[:, :, 0:1], 0.0), writes=[xT])
            for tb in range(4):
                if tb > 0:
                    c.op("pool", lambda e: e.tensor_copy(xT[:, :, 0:1], xT[:, :, 512:513]), reads=[xT], writes=[xT])
                for tt in range(4):
                    g = b * 16 + tb * 4 + tt
                    X, XB = xt[tt % 2], xb[tt % 2]
                    c.dma(c.qsel(), X[:], x_in[g * 128:(g + 1) * 128, :], writes=[X], reads=[S.xs_tok])
                    c.op("act", lambda e, X=X, XB=XB: e.copy(XB[:], X[:]), reads=[X], writes=[XB])
                    for k in range(8):
                        c.op("pe", lambda e, k=k, XB=XB: e.transpose(psT[:, k * 128:(k + 1) * 128], XB[:, k * 128:(k + 1) * 128], S.identb[:]),
                             reads=[XB, S.identb], writes=[psT])
                    c.op("dve", lambda e, tt=tt: e.tensor_copy(xT[:, :, 1 + tt * 128:1 + (tt + 1) * 128], psT[:].rearrange("p (k t) -> p k t", t=128)),
                         reads=[psT], writes=[xT])
                for tt in range(4):
                    g = tb * 4 + tt
                    for hf, c0 in enumerate((512, 1024)):
                        for k in range(8):
                            c.op("pe", lambda e, k=k, tt=tt, hf=hf, c0=c0: e.matmul(psV[:, hf * 512:(hf + 1) * 512], xT[:, k, 1 + tt * 128:1 + (tt + 1) * 128],
                                                                                      win[:, k, c0:c0 + 512], start=(k == 0), stop=(k == 7)),
                                 reads=[xT, win], writes=[psV])
                    V, G = vst[tt % 2], gst[tt % 2]
                    c.op("dve", lambda e, V=V: e.tensor_copy(V[:], psV[:, 0:512]), reads=[psV], writes=[V])
                    c.op("act", lambda e, G=G: e.activation(G[:], psV[:, 512:1024], AF.Silu), reads=[psV], writes=[G])
                    c.dma("sp", GV[b, g], V[:], reads=[V], writes=[S.pr_tok], wtok=V)
                    c.dma("sp", GG[b, g], G[:], reads=[G], writes=[S.pr_tok], wtok=G)
                for gi, (c0, n, sh, rc) in enumerate(EV_GROUPS):
                    pq, pr = psQ[cnt % 2], psR[cnt % 2]
                    a1, a2, sg = t1[cnt % 2], t2[cnt % 2], stg[cnt % 3]
                    cnt += 1
                    for k in range(8):
                        c.op("pe", lambda e, k=k, c0=c0, n=n, pq=pq: e.matmul(pq[0:n, :], win[:, k, c0:c0 + n], xT[:, k, 1:513], start=(k == 0), stop=(k == 7)),
                             reads=[win, xT], writes=[pq])
                    if not sh:
                        c.op("act", lambda e, pq=pq, sg=sg, n=n: e.copy(sg[0:n, :], pq[0:n, :]), reads=[pq], writes=[sg])
                    else:
                        for k in range(8):
                            c.op("pe", lambda e, k=k, c0=c0, n=n, pr=pr: e.matmul(pr[0:n, :], win[:, k, c0:c0 + n], xT[:, k, 0:512], start=(k == 0), stop=(k == 7)),
                                 reads=[win, xT], writes=[pr])
                        c.op("act", lambda e, pq=pq, a1=a1, n=n: e.copy(a1[0:n, :], pq[0:n, :]), reads=[pq], writes=[a1])
                        c.op("dve", lambda e, pr=pr, a1=a1, a2=a2, n=n: e.tensor_tensor(a2[0:n, :], pr[0:n, :], a1[0:n, :], ALU.subtract), reads=[pr, a1], writes=[a2])
                        c.op("dve", lambda e, a1=a1, a2=a2, sg=sg, n=n, gi=gi: e.scalar_tensor_tensor(sg[0:n, :], a2[0:n, :], mu[0:n, gi:gi + 1], a1[0:n, :], op0=ALU.mult, op1=ALU.add),
                             reads=[a1, a2, mu], writes=[sg])
                    c.dma(c.qsel(), EF[b, gi, 0:n, tb * 512:(tb + 1) * 512], sg[0:n, :], reads=[sg], writes=[S.pr_tok], wtok=sg)
        c.barrier()


def gla_phase(c, S, A, layer, oT, NB):
    i = layer // 2
    EF, GV, GG = S.EF_s, S.GV_s, S.GG_s
    with ExitStack() as esB:
        c.es = esB
        aw2 = c.sb("g_aw2", [16, 256])
        nab = c.sb("g_nab", [128, 2])
        ngb = c.sb("g_ngb", [128, 128])
        msk = c.sb("g_msk", [128, T])
        bcm = c.sb("g_bcm", [128, 128])
        qf = c.sb("g_qf", [128, T])
        kf = c.sb("g_kf", [128, T])
        gal = c.sb("g_gal", [16, T])
        cum = c.sb("g_cum", [128, T])
        ex = c.sb("g_ex", [128, T])
        dec = c.sb("g_dec", [128, 32])
        qz = c.sb("g_qz", [128, 16, 2, 128], BF16)
        kin = c.sb("g_kin", [128, T], BF16)
        kout = c.sb("g_kout", [128, T], BF16)
        ktok = [c.sb("g_ktok%d" % j, [128, 128], BF16) for j in range(2)]
        vt = [c.sb("g_vt%d" % j, [128, 512], BF16) for j in range(2)]
        gg = [c.sb("g_gg%d" % j, [128, 512]) for j in range(2)]
        At = [c.sb("g_At%d" % j, [128, 128], BF16) for j in range(2)]
        Sf = [c.sb("g_Sf%d" % j, [128, 128]) for j in range(2)]
        Sb = [c.sb("g_Sb%d" % j, [128, 128], BF16) for j in range(3)]
        osball = c.sb("g_osb", [128, 16, 512])
        ss = c.sb("g_ss", [128, 8])
        junk = c.sb("g_junk", [128, 128])
        gob = c.sb("g_gob", [128, 512], BF16)
        oTt = [c.sb("g_oTt%d" % j, [128, 512], BF16) for j in range(2)]
        psZ = c.ps("g_psZ", [128, 512])
        psA = c.ps("g_psA", [128, 128])
        psK = c.ps("g_psK", [128, 128], BF16)
        psKV = c.ps("g_psKV", [128, 256])
        psO = [c.ps("g_psO%d" % j, [128, 128]) for j in range(2)]
        psTt = c.ps("g_psT", [128, 512], BF16)
        c.dma("sp", aw2[:], A["gla_a_w2"][i], writes=[aw2])
        for p in range(2):
            c.dma("sp", nab[:, p:p + 1], A["gla_a_b"][i:i + 1, p * 128:(p + 1) * 128].rearrange("o n -> n o"), writes=[nab])
        c.op("dve", lambda e: e.tensor_scalar(nab[:], nab[:], -1.0, None, op0=ALU.mult), reads=[nab], writes=[nab])
        bcast_row(c, ngb, A["gla_norm_g"][i:i + 1, :], 128)
        c.dma("sp", bcm[:], A["bcmask"], writes=[bcm])
        c.op("pool", lambda e: e.memset(msk[:], 1.0), writes=[msk])
        c.op("pool", lambda e: e.memset(msk[:].rearrange("p (n c) -> p n c", c=64)[:, :, 0:1], 0.0), writes=[msk])
        c.op("pool", lambda e: e.memset(qz[:].rearrange("p a b c -> p (a b c)"), 0.0), writes=[qz])
        sbi = 0
        for b in range(NB):
            c.dma(c.qsel(), gal[:], EF[b, 4, 0:16, :], writes=[gal], reads=[S.pr_tok])
            for p in range(2):
                c.dma(c.qsel(), qf[:], EF[b, p], writes=[qf], reads=[S.pr_tok])
                c.dma(c.qsel(), kf[:], EF[b, 2 + p], writes=[kf], reads=[S.pr_tok])
                for tb in range(4):
                    c.op("pe", lambda e, tb=tb, p=p: e.matmul(psZ[:], aw2[:, p * 128:(p + 1) * 128], gal[:, tb * 512:(tb + 1) * 512], start=True, stop=True),
                         reads=[aw2, gal], writes=[psZ])
                    c.op("act", lambda e, tb=tb, p=p: e.activation(ex[:, tb * 512:(tb + 1) * 512], psZ[:], AF.Exp, scale=-1.0, bias=nab[:, p:p + 1]),
                         reads=[psZ, nab], writes=[ex])
                c.op("act", lambda e: e.activation(ex[:], ex[:], AF.Ln, bias=1.0), reads=[ex], writes=[ex])
                c.op("dve", lambda e: e.tensor_scalar(ex[:], ex[:], -1.0 / 16.0, None, op0=ALU.mult), reads=[ex], writes=[ex])
                c.op("dve", lambda e: e.tensor_tensor_scan(cum[:], msk[:], ex[:], 0.0, ALU.mult, ALU.add), reads=[msk, ex], writes=[cum])
                cum3 = cum[:].rearrange("p (n c) -> p n c", c=64)
                c.op("act", lambda e: e.activation(dec[:], cum3[:, :, 63], AF.Exp), reads=[cum], writes=[dec])
                c.op("act", lambda e: e.activation(ex[:], cum[:], AF.Exp), reads=[cum], writes=[ex])
                for par in range(2):
                    src_e = ex[:].rearrange("p (t two c) -> p t two c", two=2, c=64)[:, :, par, :]
                    src_q = qf[:].rearrange("p (t two c) -> p t two c", two=2, c=64)[:, :, par, :]
                    c.op("dve", lambda e, par=par, src_e=src_e, src_q=src_q: e.scalar_tensor_tensor(
                        qz[:, :, par, par * 64:(par + 1) * 64], src_q, 0.125, src_e, op0=ALU.mult, op1=ALU.mult),
                        reads=[qf, ex], writes=[qz])
                c.op("act", lambda e: e.activation(ex[:], cum[:], AF.Exp, scale=-1.0), reads=[cum], writes=[ex])
                c.op("dve", lambda e: e.tensor_tensor(kin[:], kf[:], ex[:], ALU.mult), reads=[kf, ex], writes=[kin])
                c.op("dve", lambda e: e.tensor_tensor(ex[:].rearrange("p (n c) -> p n c", c=64), cum3[:, :, 63:64].to_broadcast([128, 32, 64]), cum3, ALU.subtract),
                     reads=[cum], writes=[ex])
                c.op("act", lambda e: e.activation(ex[:], ex[:], AF.Exp), reads=[ex], writes=[ex])
                c.op("dve", lambda e: e.tensor_tensor(kout[:], kf[:], ex[:], ALU.mult), reads=[kf, ex], writes=[kout])
                Sc = Sf[0]
                c.op("pool", lambda e, Sc=Sc: e.memset(Sc[:], 0.0), writes=[Sc])
                S0b = Sb[sbi % 3]; sbi += 1
                c.op("pool", lambda e, S0b=S0b: e.memset(S0b[:], 0.0), writes=[S0b])
                for tt in range(16):
                    V, Gg = vt[tt % 2], gg[tt % 2]
                    c.dma(c.qsel(), V[:], GV[b, tt], writes=[V], reads=[S.pr_tok])
                    if p == 1:
                        c.dma(c.qsel(), Gg[:], GG[b, tt], writes=[Gg], reads=[S.pr_tok])
                    t0 = tt * 128
                    KT = ktok[tt % 2]
                    c.op("pe", lambda e, t0=t0: e.transpose(psK[:], kout[:, t0:t0 + 128], S.identb[:]), reads=[kout, S.identb], writes=[psK])
                    c.op("act", lambda e, KT=KT: e.copy(KT[:], psK[:]), reads=[psK], writes=[KT])
                    Sbs = [S0b]
                    Scur = Sc
                    for ch in range(2):
                        n = tt * 2 + ch
                        c.op("pe", lambda e, ch=ch, KT=KT, V=V, p=p: e.matmul(psKV[:], KT[ch * 64:(ch + 1) * 64, :], V[ch * 64:(ch + 1) * 64, p * 256:(p + 1) * 256],
                                                                               start=True, stop=True), reads=[KT, V], writes=[psKV])
                        Snew = Sf[(tt * 2 + ch + 1) % 2]
                        for hh in range(2):
                            c.op("dve", lambda e, hh=hh, n=n, Scur=Scur, Snew=Snew: e.scalar_tensor_tensor(
                                Snew[hh * 64:(hh + 1) * 64, :], Scur[hh * 64:(hh + 1) * 64, :], dec[hh * 64:(hh + 1) * 64, n:n + 1],
                                psKV[hh * 64:(hh + 1) * 64, hh * 128:(hh + 1) * 128], op0=ALU.mult, op1=ALU.add),
                                reads=[Scur, dec, psKV], writes=[Snew])
                        Sn_b = Sb[sbi % 3]; sbi += 1
                        c.op("act", lambda e, Snew=Snew, Sn_b=Sn_b: e.copy(Sn_b[:], Snew[:]), reads=[Snew], writes=[Sn_b])
                        Sbs.append(Sn_b)
                        Scur = Snew
                    Sc = Scur
                    for hh in range(2):
                        h = p * 2 + hh
                        pl, ph = hh * 64, (hh + 1) * 64
                        AT = At[hh]
                        PO = psO[hh]
                        for par in range(2):
                            c.op("pe", lambda e, pl=pl, ph=ph, t0=t0, par=par, tt=tt: e.matmul(psA[:], kin[pl:ph, t0:t0 + 128], qz[pl:ph, tt, par, :],
                                                                                               start=(par == 0), stop=(par == 1)),
                                 reads=[kin, qz], writes=[psA])
                        c.op("dve", lambda e, AT=AT: e.tensor_tensor(AT[:], psA[:], bcm[:], ALU.mult), reads=[psA, bcm], writes=[AT])
                        c.op("pe", lambda e, AT=AT, V=V, h=h, PO=PO: e.matmul(PO[:], AT[:], V[:, h * 128:(h + 1) * 128], start=True, stop=False),
                             reads=[AT, V], writes=[PO])
                        for ch in range(2):
                            c.op("pe", lambda e, ch=ch, pl=pl, ph=ph, PO=PO, sbv=Sbs[ch]: e.matmul(PO[:], qz[pl:ph, tt, ch, :], sbv[pl:ph, :], start=False, stop=(ch == 1)),
                                 reads=[qz, Sbs[ch]], writes=[PO])
                        c.op("act", lambda e, PO=PO, h=h, tt=tt: e.copy(osball[:, tt, h * 128:(h + 1) * 128], PO[:]), reads=[PO], writes=[osball])
                    S0b = Sbs[2]
                    if p == 1:
                        for h in range(4):
                            c.op("act", lambda e, h=h, tt=tt: e.activation(junk[:], osball[:, tt, h * 128:(h + 1) * 128], AF.Square, accum_out=ss[:, h:h + 1]),
                                 reads=[osball], writes=[junk, ss])
                        c.op("dve", lambda e: e.tensor_scalar(ss[:, 0:4], ss[:, 0:4], 1.0 / 128.0, LN_EPS, op0=ALU.mult, op1=ALU.add), reads=[ss], writes=[ss])
                        c.op("act", lambda e: e.activation(ss[:, 0:4], ss[:, 0:4], AF.Sqrt), reads=[ss], writes=[ss])
                        c.op("dve", lambda e: e.reciprocal(ss[:, 4:8], ss[:, 0:4]), reads=[ss], writes=[ss])
                        for h in range(4):
                            c.op("dve", lambda e, h=h, tt=tt: e.scalar_tensor_tensor(osball[:, tt, h * 128:(h + 1) * 128], osball[:, tt, h * 128:(h + 1) * 128], ss[:, 4 + h:5 + h], ngb[:],
                                                                                      op0=ALU.mult, op1=ALU.mult), reads=[osball, ss, ngb], writes=[osball])
                        c.op("pool", lambda e, tt=tt, Gg=Gg: e.tensor_tensor(gob[:], osball[:, tt, :], Gg[:], ALU.mult), reads=[osball, Gg], writes=[gob])
                        for h in range(4):
                            c.op("pe", lambda e, h=h: e.transpose(psTt[:, h * 128:(h + 1) * 128], gob[:, h * 128:(h + 1) * 128], S.identb[:]),
                                 reads=[gob, S.identb], writes=[psTt])
                        O = oTt[tt % 2]
                        c.op("act", lambda e, O=O: e.copy(O[:], psTt[:]), reads=[psTt], writes=[O])
                        c.dma("sp", oT[b * 16 + tt][:, 0:512], O[:], reads=[O], writes=[S.oT_tok], wtok=O)
        c.barrier()


def even_stage(c, S, A, layer, x_in, oT, NB):
    even_phaseA(c, S, A, layer, x_in, NB)
    gla_phase(c, S, A, layer, oT, NB)
    rwkv_phase(c, S, A, layer, oT, NB)


def rwkv_phase(c, S, A, layer, oT, NB):
    i = layer // 2
    EF = S.EF_s
    NBH = NB * 4
    NF = NBH * 64
    PW = max(NF, 512)
    chunks = [(c0, min(NF, c0 + 512)) for c0 in range(0, NF, 512)]
    DEC_SCALE = -float(np.exp(-0.5))
    with ExitStack() as esB:
        c.es = esB
        HG = NBH // 2
        HW = HG * 64
        PWH = max(HW, 512)
        Zq = [[c.sb("r_Z%d_%d" % (q, j), [128, HW]) for j in range(2)] for q in range(2)]
        tA = [c.sb("r_tA%d" % q, [128, HW], BF16) for q in range(2)]
        tB = [c.sb("r_tB%d" % q, [128, HW]) for q in range(2)]
        tP = [c.sb("r_tP%d" % q, [128, HW]) for q in range(2)]
        tD = [c.sb("r_tD%d" % q, [128, HW], BF16) for q in range(2)]
        vs = [c.sb("r_vs%d" % q, [128, HW]) for q in range(2)]
        tCn = [[c.sb("r_tCn%d_%d" % (q, j), [128, HW]) for j in range(2)] for q in range(2)]
        WOP, AOP, BOP, KOP, ROP = [c.sb("r_op%d" % j, [128, 128, NBH]) for j in range(5)]
        vtok = c.sb("r_vtok", [128, NBH, 128], BF16)
        bonus = c.sb("r_bonus", [128, NBH, 128])
        gT = c.sb("r_gT", [128, NBH, 128])
        ysb = c.sb("r_ysb", [128, NBH * 2, 64])
        ysq = c.sb("r_ysq", [128, NBH * 2, 64])
        yst = c.sb("r_yst", [128, NBH * 2, 2])
        w2b = c.sb("r_w2b", [32, 512], BF16)
        a2b = c.sb("r_a2b", [64, 512], BF16)
        g2b = c.sb("r_g2b", [96, 512], BF16)
        pp = c.sb("r_pp", [128, 4, 8])
        bof = c.sb("r_bof", [128, 128])
        bob = c.sb("r_bob", [128, 128], BF16)
        ahw = c.sb("r_ahw", [128, 255], BF16)
        rT = [c.sb("r_rT%d" % j, [128, 128]) for j in range(2)]
        kTt = [c.sb("r_kT%d" % j, [128, 128]) for j in range(2)]
        vT = [c.sb("r_vT%d" % j, [128, 128]) for j in range(2)]
        wlal = c.sb("r_wlal", [64, 128])
        glt = c.sb("r_glt", [96, 128])
        twb = c.sb("r_twb", [64, 128], BF16)
        sglb = c.sb("r_sglb", [96, 128], BF16)
        ET = [[c.sb("r_e%d_%d" % (k, j), [128, 128]) for k in range(5)] for j in range(2)]
        yo = [c.sb("r_yo%d" % j, [128, 128]) for j in range(2)]
        yob = [c.sb("r_yob%d" % j, [128, 128], BF16) for j in range(2)]
        psSAq = [c.ps("r_psSA%d" % q, [128, PWH]) for q in range(2)]
        psVBq = [c.ps("r_psVB%d" % q, [128, PWH]) for q in range(2)]
        psYq = [[c.ps("r_psY%d_%d" % (q, j), [128, HW]) for j in range(2)] for q in range(2)]
        psSA, psVB = psSAq[0], psVBq[0]
        for (dst, nm, rows, pofs) in ((w2b, "rwkv_w2", 32, 0), (a2b, "rwkv_a2", 32, 32), (g2b, "rwkv_g2", 96, 0)):
            st = S.wstage[0]
            c.dma("sp", st[pofs:pofs + rows, 0:512], A[nm][i], writes=[st])
            c.op("dve", lambda e, dst=dst, st=st, rows=rows, pofs=pofs: e.tensor_copy(dst[pofs:pofs + rows, :], st[pofs:pofs + rows, 0:512]), reads=[st], writes=[dst])
        for j, nm in enumerate(("rwkv_w0", "rwkv_a0", "rwkv_k_k", "rwkv_k_a", None, "rwkv_r_k", "rwkv_lnx_g", "rwkv_lnx_b")):
            if nm is None:
                continue
            src = A[nm][i:i + 1] if nm != "rwkv_r_k" else A[nm][i:i + 1].rearrange("o h d -> o (h d)")
            for hp in range(4):
                c.dma(c.qsel(), pp[:, hp, j:j + 1], src[:, hp * 128:(hp + 1) * 128].rearrange("o n -> n o"), writes=[pp])
        c.op("dve", lambda e: e.tensor_scalar(pp[:, :, 4], pp[:, :, 3], -1.0, 1.0, op0=ALU.mult, op1=ALU.add), reads=[pp], writes=[pp])
        c.dma("sp", bof[:], A["blockones"], writes=[bof])
        c.dma("sp", bob[:], A["blockonesb"], writes=[bob])
        c.dma("sp", ahw[:], A["ahwin"], writes=[ahw])
        for q in range(2):
            c.op("pool", lambda e, q=q: e.memset(Zq[q][0][:], 0.0), writes=[Zq[q][0]])
        step = 0
        for tb in range(16):
            t0 = tb * 128
            for b in range(NB):
                c.dma(c.qsel(), wlal[:], EF[b, 17, 0:64, t0:t0 + 128], writes=[wlal], reads=[S.pr_tok])
                c.dma(c.qsel(), glt[:], EF[b, 18, 0:96, t0:t0 + 128], writes=[glt], reads=[S.pr_tok])
                c.op("act", lambda e: e.activation(twb[0:32, :], wlal[0:32, :], AF.Tanh), reads=[wlal], writes=[twb])
                c.op("act", lambda e: e.copy(twb[32:64, :], wlal[32:64, :]), reads=[wlal], writes=[twb])
                c.op("act", lambda e: e.activation(sglb[:], glt[:], AF.Sigmoid), reads=[glt], writes=[sglb])
                for hp in range(4):
                    bh = b * 4 + hp
                    R_, K_, V_ = rT[bh % 2], kTt[bh % 2], vT[bh % 2]
                    pSA, pVB = psSAq[bh % 2], psVBq[bh % 2]
                    e1, e2, e3, e4, asb = ET[bh % 2]
                    c.dma(c.qsel(), R_[:], EF[b, 5 + hp, :, t0:t0 + 128], writes=[R_], reads=[S.pr_tok])
                    c.dma(c.qsel(), K_[:], EF[b, 9 + hp, :, t0:t0 + 128], writes=[K_], reads=[S.pr_tok])
                    c.dma(c.qsel(), V_[:], EF[b, 13 + hp, :, t0:t0 + 128], writes=[V_], reads=[S.pr_tok])
                    cs = slice(hp * 128, (hp + 1) * 128)
                    c.op("pe", lambda e, cs=cs: e.matmul(pSA[:, 0:128], w2b[0:32, cs], twb[0:32, :], start=True, stop=True), reads=[w2b, twb], writes=[pSA])
                    c.op("act", lambda e, hp=hp: e.activation(e1[:], pSA[:, 0:128], AF.Sigmoid, bias=pp[:, hp, 0:1]), reads=[pSA, pp], writes=[e1])
                    c.op("act", lambda e, bh=bh: e.activation(WOP[:, :, bh], e1[:], AF.Exp, scale=DEC_SCALE), reads=[e1], writes=[WOP])
                    c.op("pe", lambda e, cs=cs: e.matmul(pSA[:, 128:256], a2b[32:64, cs], twb[32:64, :], start=True, stop=True), reads=[a2b, twb], writes=[pSA])
                    c.op("act", lambda e, hp=hp: e.activation(asb[:], pSA[:, 128:256], AF.Sigmoid, bias=pp[:, hp, 1:2]), reads=[pSA, pp], writes=[asb])
                    c.op("pe", lambda e, cs=cs: e.matmul(pVB[:, 0:128], g2b[0:96, cs], sglb[0:96, :], start=True, stop=True), reads=[g2b, sglb], writes=[pVB])
                    c.op("act", lambda e, bh=bh: e.copy(gT[:, bh, :], pVB[:, 0:128]), reads=[pVB], writes=[gT])
                    c.op("dve", lambda e, K_=K_, hp=hp: e.tensor_scalar(e2[:], K_[:], pp[:, hp, 2:3], None, op0=ALU.mult), reads=[K_, pp], writes=[e2])
                    c.op("pool", lambda e: e.tensor_tensor(e3[:], e2[:], e2[:], ALU.mult), reads=[e2], writes=[e3])
                    c.op("pe", lambda e: e.matmul(pVB[:, 128:256], bof[:], e3[:], start=True, stop=True), reads=[bof, e3], writes=[pVB])
                    c.op("act", lambda e: e.activation(e3[:], pVB[:, 128:256], AF.Sqrt), reads=[pVB], writes=[e3])
                    c.op("dve", lambda e: e.tensor_scalar(e3[:], e3[:], 1e-12, None, op0=ALU.max), reads=[e3], writes=[e3])
                    c.op("dve", lambda e: e.reciprocal(e3[:], e3[:]), reads=[e3], writes=[e3])
                    c.op("dve", lambda e: e.tensor_tensor(e2[:], e2[:], e3[:], ALU.mult), reads=[e2, e3], writes=[e2])
                    c.op("dve", lambda e, bh=bh: e.tensor_scalar(AOP[:, :, bh], e2[:], -1.0, None, op0=ALU.mult), reads=[e2], writes=[AOP])
                    c.op("dve", lambda e, bh=bh: e.tensor_tensor(BOP[:, :, bh], e2[:], asb[:], ALU.mult), reads=[e2, asb], writes=[BOP])
                    c.op("dve", lambda e, hp=hp: e.tensor_scalar(e4[:], asb[:], pp[:, hp, 3:4], pp[:, hp, 4:5], op0=ALU.mult, op1=ALU.add), reads=[asb, pp], writes=[e4])
                    c.op("dve", lambda e, K_=K_: e.tensor_tensor(e4[:], e4[:], K_[:], ALU.mult), reads=[e4, K_], writes=[e4])
                    c.op("pool", lambda e, bh=bh: e.tensor_copy(KOP[:, :, bh], e4[:]), reads=[e4], writes=[KOP])
                    c.op("pool", lambda e, bh=bh, R_=R_: e.tensor_copy(ROP[:, :, bh], R_[:]), reads=[R_], writes=[ROP])
                    c.op("dve", lambda e, R_=R_, hp=hp: e.scalar_tensor_tensor(e1[:], R_[:], pp[:, hp, 5:6], e4[:], op0=ALU.mult, op1=ALU.mult), reads=[R_, pp, e4], writes=[e1])
                    c.op("pe", lambda e: e.matmul(pVB[:, 256:384], bof[:], e1[:], start=True, stop=True), reads=[bof, e1], writes=[pVB])
                    c.op("dve", lambda e, bh=bh, V_=V_: e.tensor_tensor(bonus[:, bh, :], pVB[:, 256:384], V_[:], ALU.mult), reads=[pVB, V_], writes=[bonus])
                    c.op("pe", lambda e, V_=V_: e.transpose(pSA[:, 256:384], V_[:], S.identf[:]), reads=[V_, S.identf], writes=[pSA])
                    c.op("act", lambda e, bh=bh: e.copy(vtok[:, bh, :], pSA[:, 256:384]), reads=[pSA], writes=[vtok])
            vt4 = vtok[:].rearrange("p g (h i) -> p g h i", h=2)

            def v3(tk):
                return tk[:, 0:HW].rearrange("p (g i) -> p g i", i=64)

            def bc(op_, tl, q):
                return op_[:, tl, q * HG:(q + 1) * HG].unsqueeze(2).to_broadcast([128, HG, 64])

            def lookahead_pe(tl):
                for q in range(2):
                    for hh in range(2):
                        c.op("pe", lambda e, q=q, hh=hh, tl=tl: e.matmul(psVBq[q][hh * 64:(hh + 1) * 64, 0:HW], S.identb[:, tl:tl + 1].to_broadcast([128, 64]),
                                                                         vt4[:, q * HG:(q + 1) * HG, hh, :], start=True, stop=True),
                             reads=[S.identb, vtok], writes=[psVBq[q]])
                    c.op("act", lambda e, q=q: e.copy(vs[q][:], psVBq[q][:, 0:HW]), reads=[psVBq[q]], writes=[vs[q]])

            def lookahead_pool(tl):
                for q in range(2):
                    TC = tCn[q][tl % 2]
                    c.op("pool", lambda e, tl=tl, TC=TC, q=q: e.tensor_tensor(v3(TC), v3(vs[q]), bc(KOP, tl, q), ALU.mult), reads=[vs[q], KOP], writes=[TC])

            def emit_tmpA(tl, par):
                for q in range(2):
                    Zi = Zq[q][par]
                    c.op("dve", lambda e, tl=tl, q=q, Zi=Zi: e.tensor_tensor(v3(tA[q]), v3(Zi), bc(AOP, tl, q), ALU.mult), reads=[Zi, AOP], writes=[tA[q]])

            lookahead_pe(0)
            lookahead_pool(0)
            emit_tmpA(0, step % 2)
            for tl in range(128):
                par = step % 2
                step += 1
                for q in range(2):
                    c.op("pe", lambda e, q=q: e.matmul(psSAq[q][:, 0:HW], bob[:], tA[q][:, 0:HW], start=True, stop=True), reads=[bob, tA[q]], writes=[psSAq[q]])
                if tl < 127:
                    lookahead_pe(tl + 1)
                for q in range(2):
                    Zi, TC = Zq[q][par], tCn[q][tl % 2]
                    c.op("pool", lambda e, tl=tl, q=q, Zi=Zi: e.tensor_tensor(v3(tP[q]), v3(Zi), bc(WOP, tl, q), ALU.mult), reads=[Zi, WOP], writes=[tP[q]])
                    c.op("pool", lambda e, q=q, TC=TC: e.tensor_tensor(tP[q][:], tP[q][:], TC[:], ALU.add), reads=[tP[q], TC], writes=[tP[q]])
                if tl < 127:
                    lookahead_pool(tl + 1)
                for q in range(2):
                    Zo = Zq[q][1 - par]
                    c.op("dve", lambda e, tl=tl, q=q: e.tensor_tensor(v3(tB[q]), v3(psSAq[q]), bc(BOP, tl, q), ALU.mult), reads=[psSAq[q], BOP], writes=[tB[q]])
                    c.op("dve", lambda e, q=q, Zo=Zo: e.tensor_tensor(Zo[:], tP[q][:], tB[q][:], ALU.add), reads=[tP[q], tB[q]], writes=[Zo])
                if tl < 127:
                    emit_tmpA(tl + 1, 1 - par)
                for q in range(2):
                    Zo = Zq[q][1 - par]
                    c.op("dve", lambda e, tl=tl, q=q, Zo=Zo: e.tensor_tensor(v3(tD[q]), v3(Zo), bc(ROP, tl, q), ALU.mult), reads=[Zo, ROP], writes=[tD[q]])
                    for hh in range(2):
                        c.op("pe", lambda e, q=q, hh=hh, tl=tl: e.matmul(psYq[q][hh][:, 0:HW], ahw[hh * 64:(hh + 1) * 64, 127 - tl:255 - tl],
                                                                         tD[q][hh * 64:(hh + 1) * 64, 0:HW], start=(tl == 0), stop=(tl == 127)),
                             reads=[ahw, tD[q]], writes=[psYq[q][hh]])
            ys4 = ysb[:].rearrange("p (g h) i -> p g h i", h=2)
            for q in range(2):
                for hh in range(2):
                    c.op("act", lambda e, hh=hh, q=q: e.copy(ys4[:, q * HG:(q + 1) * HG, hh, :], psYq[q][hh][:, 0:HW].rearrange("p (g i) -> p g i", i=64)),
                         reads=[psYq[q][hh]], writes=[ysb])
            G2 = NBH * 2
            c.op("dve", lambda e: e.tensor_reduce(yst[:, :, 0], ysb[:], AX.X, ALU.add), reads=[ysb], writes=[yst])
            c.op("dve", lambda e: e.tensor_scalar(yst[:, :, 0], yst[:, :, 0], 1.0 / 64.0, None, op0=ALU.mult), reads=[yst], writes=[yst])
            c.op("dve", lambda e: e.tensor_tensor(ysb[:], ysb[:], yst[:, :, 0:1].to_broadcast([128, G2, 64]), ALU.subtract), reads=[ysb, yst], writes=[ysb])
            c.op("pool", lambda e: e.tensor_tensor(ysq[:], ysb[:], ysb[:], ALU.mult), reads=[ysb], writes=[ysq])
            c.op("dve", lambda e: e.tensor_reduce(yst[:, :, 1], ysq[:], AX.X, ALU.add), reads=[ysq], writes=[yst])
            c.op("dve", lambda e: e.tensor_scalar(yst[:, :, 1], yst[:, :, 1], 1.0 / 64.0, 64e-5, op0=ALU.mult, op1=ALU.add), reads=[yst], writes=[yst])
            c.op("act", lambda e: e.activation(yst[:, :, 1], yst[:, :, 1], AF.Sqrt), reads=[yst], writes=[yst])
            c.op("dve", lambda e: e.reciprocal(yst[:, :, 1], yst[:, :, 1]), reads=[yst], writes=[yst])
            c.op("dve", lambda e: e.tensor_tensor(ysb[:], ysb[:], yst[:, :, 1:2].to_broadcast([128, G2, 64]), ALU.mult), reads=[ysb, yst], writes=[ysb])
            for b in range(NB):
                for hp in range(4):
                    bh = b * 4 + hp
                    Y, YB = yo[bh % 2], yob[bh % 2]
                    pSA = psSAq[bh % 2]
                    c.op("pe", lambda e, bh=bh, pSA=pSA: e.transpose(pSA[:, 0:128], ysb[:, 2 * bh:2 * bh + 2, :].rearrange("p h i -> p (h i)"), S.identf[:]),
                         reads=[ysb, S.identf], writes=[pSA])
                    c.op("dve", lambda e, Y=Y, hp=hp, pSA=pSA: e.tensor_scalar(Y[:], pSA[:, 0:128], pp[:, hp, 6:7], pp[:, hp, 7:8], op0=ALU.mult, op1=ALU.add), reads=[pSA, pp], writes=[Y])
                    c.op("pool", lambda e, Y=Y, bh=bh: e.tensor_tensor(Y[:], Y[:], bonus[:, bh, :], ALU.add), reads=[Y, bonus], writes=[Y])
                    c.op("pool", lambda e, Y=Y, YB=YB, bh=bh: e.tensor_tensor(YB[:], Y[:], gT[:, bh, :], ALU.mult), reads=[Y, gT], writes=[YB])
                    c.dma(c.qsel(), oT[b * 16 + tb][:, (4 + hp) * 128:(5 + hp) * 128], YB[:], reads=[YB], writes=[S.oT_tok], wtok=YB)
        c.barrier()


_NC_CACHE = {}


def kernel(**inputs):
    n = 8
    NB = 32 // n
    if "nc" not in _NC_CACHE:
        _NC_CACHE["nc"] = build(NB=NB, layers=(0, 1, 2, 3), mode="full")
    nc = _NC_CACHE["nc"]
    x = np.ascontiguousarray(inputs["x"], dtype=np.float32)
    cs = consts()
    in_maps = []
    for ci in range(n):
        m = {"x": x[ci * NB:(ci + 1) * NB].reshape(NB * T, D)}
        for k in WSPEC:
            m[k] = np.ascontiguousarray(inputs[k], dtype=np.float32)
        m.update(cs)
        in_maps.append(m)
    res = run_bass_kernel_spmd(nc, in_maps, core_ids=list(range(n)))
    out = np.stack([r["y"].reshape(NB, T, D) for r in res.results], 0).reshape(32, T, D)
    return out.astype(np.float32)
```

```python
import numpy as np
from contextlib import ExitStack
import concourse.bass as bass
import concourse.mybir as mybir
from concourse.bass_utils import run_bass_kernel_spmd
import ml_dtypes

F32 = mybir.dt.float32
BF16 = mybir.dt.bfloat16
U32 = mybir.dt.uint32
AF = mybir.ActivationFunctionType
ALU = mybir.AluOpType
AX = mybir.AxisListType

T = 2048
D = 1024
DEPTH = 4
ALPHA = float((2 * DEPTH) ** 0.25)
LN_EPS = 1e-5
NEG = -1e30
SEM_EPOCH = 60000


class Tok:
    __slots__ = ("w", "r", "t", "dsem", "dcnt", "name")

    def __init__(self, t=None, name=None):
        self.w = None
        self.r = []
        self.t = t
        self.dsem = None
        self.dcnt = 0
        self.name = name

    def __getitem__(self, k):
        return self.t[k]


class Ctx:
    def __init__(self, nc, es):
        self.nc = nc
        self.es = es
        self.es0 = es
        self.dpool = {}
        self.eng = {"pe": nc.tensor, "dve": nc.vector, "act": nc.scalar, "pool": nc.gpsimd, "sp": nc.sync}
        self.sem = {}
        self.cnt = {}
        for e in self.eng:
            self.sem[e] = es.enter_context(nc.semaphore("s_" + e))
            self.cnt[e] = 0
        self.waited = {e: {} for e in self.eng}
        self.pe_sems = {id(self.sem["pe"])}
        self.nsem = 0
        self.ninst = 0
        self.rr = 0
        self.uid = 0

    def sb(self, name, shape, dt=F32):
        self.uid += 1
        return Tok(self.es.enter_context(self.nc.sbuf_tensor("sb%d_%s" % (self.uid, name), list(shape), dt)), name)

    def ps(self, name, shape, dt=F32):
        self.uid += 1
        return Tok(self.es.enter_context(self.nc.psum_tensor("ps%d_%s" % (self.uid, name), list(shape), dt)), name)

    def tok(self, name=None):
        return Tok(None, name)

    def _dsem(self, tok):
        if tok.name not in self.dpool or self.dpool[tok.name][1] >= SEM_EPOCH:
            self.dpool[tok.name] = [self.es0.enter_context(self.nc.semaphore("d%d" % self.nsem)), 0]
            self.nsem += 1
        return self.dpool[tok.name]

    def barrier(self):
        for e in self.eng:
            for o in self.eng:
                if o != e and self.cnt[o] > 0:
                    self._wait(e, (self.sem[o], self.cnt[o]))
            for sem, cnt in self.dpool.values():
                if cnt > 0:
                    self._wait(e, (sem, cnt))

    def _wait(self, e, dep):
        if dep is None:
            return
        sem, v = dep
        k = id(sem)
        if False and e == "pe" and k in self.pe_sems:
            return
        if self.waited[e].get(k, 0) >= v:
            return
        self.waited[e][k] = v
        self.eng[e].wait_ge(sem, v)
        self.ninst += 1

    def _deps(self, e, reads, writes):
        for t in reads:
            self._wait(e, t.w)
        for t in writes:
            self._wait(e, t.w)
            for d in t.r:
                self._wait(e, d)

    @staticmethod
    def _compact(lst):
        best = {}
        for sem, v in lst:
            k = id(sem)
            if k not in best or best[k][1] < v:
                best[k] = (sem, v)
        return list(best.values())

    def _mark(self, me, reads, writes):
        for t in reads:
            t.r.append(me)
            if len(t.r) > 12:
                t.r = self._compact(t.r)
        for t in writes:
            t.w = me
            t.r = []

    def op(self, e, fn, reads=(), writes=()):
        self._deps(e, reads, writes)
        if self.cnt[e] >= SEM_EPOCH:
            self.sem[e] = self.es0.enter_context(self.nc.semaphore("s_%s_%d" % (e, self.nsem)))
            self.nsem += 1
            self.cnt[e] = 0
            if e == "pe":
                self.pe_sems.add(id(self.sem[e]))
        ins = fn(self.eng[e])
        self.cnt[e] += 1
        ins.then_inc(self.sem[e], 1)
        self.ninst += 1
        self._mark((self.sem[e], self.cnt[e]), reads, writes)
        return ins

    def dma(self, q, out_ap, in_ap, reads=(), writes=(), wtok=None, **kw):
        self._deps(q, reads, writes)
        tk = wtok or (writes[0] if writes else reads[0])
        ent = self._dsem(tk)
        ins = self.eng[q].dma_start(out=out_ap, in_=in_ap, **kw)
        ent[1] += 16
        ins.then_inc(ent[0], 16)
        self.ninst += 1
        self._mark((ent[0], ent[1]), reads, writes)
        return ins

    def gather(self, out_ap, in_ap, idx_ap, reads=(), writes=()):
        q = "pool"
        self._deps(q, reads, writes)
        tk = writes[0]
        ent = self._dsem(tk)
        ins = self.nc.gpsimd.indirect_dma_start(
            out=out_ap, out_offset=None, in_=in_ap,
            in_offset=bass.IndirectOffsetOnAxis(ap=idx_ap, axis=0))
        ent[1] += 16
        ins.then_inc(ent[0], 16)
        self.ninst += 1
        self._mark((ent[0], ent[1]), reads, writes)
        return ins

    def finish(self, toks, e="sp"):
        for t in toks:
            self._wait(e, t.w)
            for d in t.r:
                self._wait(e, d)

    def qsel(self):
        self.rr += 1
        return ("sp", "act")[self.rr % 2]


class Shared:
    pass


def load_w_bf16(c, S, dst, src, K, N, pofs=0, rows=128):
    for k in range(K):
        for n0 in range(0, N, 1024):
            n1 = min(N, n0 + 1024)
            S.wsi += 1
            st = S.wstage[S.wsi % 2]
            c.dma(c.qsel(), st[pofs:pofs + rows, 0:n1 - n0], src[k * rows:(k + 1) * rows, n0:n1], writes=[st])
            if S.wsi % 2 == 0:
                c.op("dve", lambda en, st=st, k=k, n0=n0, n1=n1: en.tensor_copy(dst[pofs:pofs + rows, k, n0:n1], st[pofs:pofs + rows, 0:n1 - n0]),
                     reads=[st], writes=[dst])
            else:
                c.op("act", lambda en, st=st, k=k, n0=n0, n1=n1: en.copy(dst[pofs:pofs + rows, k, n0:n1], st[pofs:pofs + rows, 0:n1 - n0]),
                     reads=[st], writes=[dst])


def bcast_row(c, dst, src_row, n):
    c.dma(c.qsel(), dst[:, 0:n], src_row.partition_broadcast(128), writes=[dst])


def layer_norm(c, S, out_ap, out_tok, z, g_bc, b_bc):
    st, mv, sd = S.ln_st, S.ln_mv, S.ln_sd
    c.op("dve", lambda e: e.bn_stats(st[:, 0, :], z[:, 0:512]), reads=[z], writes=[st])
    c.op("dve", lambda e: e.bn_stats(st[:, 1, :], z[:, 512:1024]), reads=[z], writes=[st])
    c.op("dve", lambda e: e.bn_aggr(mv[:], st[:]), reads=[st], writes=[mv])
    c.op("dve", lambda e: e.tensor_scalar(sd[:, 0:1], mv[:, 1:2], LN_EPS, None, op0=ALU.add), reads=[mv], writes=[sd])
    c.op("act", lambda e: e.activation(sd[:, 1:2], sd[:, 0:1], AF.Sqrt), reads=[sd], writes=[sd])
    c.op("dve", lambda e: e.reciprocal(sd[:, 2:3], sd[:, 1:2]), reads=[sd], writes=[sd])
    c.op("dve", lambda e: e.tensor_scalar(z[:], z[:], mv[:, 0:1], sd[:, 2:3], op0=ALU.subtract, op1=ALU.mult),
         reads=[z, mv, sd], writes=[z])
    c.op("pool", lambda e: e.tensor_tensor(z[:], z[:], g_bc[:], ALU.mult), reads=[z, g_bc], writes=[z])
    c.op("pool", lambda e: e.tensor_tensor(out_ap, z[:], b_bc[:], ALU.add), reads=[z, b_bc], writes=[out_tok])


NG = 12
GS = 4
PULLN = 1
PULLD = 1


def uv_prep(c, S, A, layer):
    with ExitStack() as esP:
        c.es = esP
        uf = [c.sb("p_uf%d" % j, [128, 1024]) for j in range(2)]
        vf = [c.sb("p_vf%d" % j, [128, 1024]) for j in range(2)]
        uvb = [c.sb("p_uvb%d" % j, [128, 2048], BF16) for j in range(2)]
        for r in range(128):
            U, V, B = uf[r % 2], vf[r % 2], uvb[r % 2]
            c.dma("sp", U[:], A["peer_u"][layer, r * 128:(r + 1) * 128, :], writes=[U])
            c.dma("act", V[:], A["peer_v"][layer, r * 128:(r + 1) * 128, :], writes=[V])
            c.op("dve", lambda e, U=U, B=B: e.tensor_copy(B[:, 0:1024], U[:]), reads=[U], writes=[B])
            c.op("act", lambda e, V=V, B=B: e.copy(B[:, 1024:2048], V[:]), reads=[V], writes=[B])
            c.dma("sp", S.UV_s[r * 128:(r + 1) * 128, :], B[:], reads=[B], writes=[S.uv_tok], wtok=B)
        c.barrier()


def tail_alloc(c, S):
    S.wout = c.sb("wout", [128, 8, 1024], BF16)
    S.wq = c.sb("wq", [128, 8, 1024], BF16)
    S.keysT = c.sb("keysT", [128, 8, 128], BF16)
    S.lng = [c.sb("lng%d" % i, [128, 1024]) for i in range(4)]
    S.iota16 = c.sb("iota16", [128, 16])
    S.oT = c.sb("oT_sb", [128, 8, 128], BF16)
    S.xt = c.sb("xt", [128, 1024])
    S.z = c.sb("z", [128, 1024])
    S.xm2 = [c.sb("xm%d" % i, [128, 1024]) for i in range(2)]
    S.xmb2 = [c.sb("xmb%d" % i, [128, 1024], BF16) for i in range(2)]
    S.idxu2 = [c.sb("idxu%d" % i, [128, 128], U32) for i in range(2)]
    S.gate2 = [c.sb("gate%d" % i, [128, 8, 16]) for i in range(2)]
    S.xm, S.xmb = S.xm2[0], S.xmb2[0]
    S.xmT = c.sb("xmT", [128, 8, 128], BF16)
    S.qT = c.sb("qT", [128, 8, 128], BF16)
    S.ssc = c.sb("ssc", [128, 128])
    S.tops = c.sb("tops", [128, 16, 16])
    S.topi = c.sb("topi", [128, 16, 16], U32)
    S.topf = c.sb("topf", [128, 16, 16])
    S.cands = c.sb("cands", [128, 8, 256])
    S.candw = c.sb("candw", [128, 256])
    S.best = c.sb("best", [128, 8, 16])
    S.pos = c.sb("pos", [128, 8, 16], U32)
    S.pab = c.sb("pab", [128, 2, 128], U32)
    S.pabf = c.sb("pabf", [128, 2, 128])
    S.eq = [c.sb("eq%d" % i, [128, 128, 16]) for i in range(2)]
    S.idx2 = c.sb("idx2", [128, 2, 128])
    S.idxf = c.sb("idxf", [128, 128])
    S.idxu, S.gate = S.idxu2[0], S.gate2[0]
    S.gsum = c.sb("gsum", [128, 8])
    S.hh4 = [c.sb("hh%d" % i, [128, GS]) for i in range(4)]
    S.coef4 = [c.sb("coef%d" % i, [128, GS]) for i in range(4)]
    S.junk2 = [c.sb("junk%d" % i, [128, 1024], BF16) for i in range(2)]
    S.acc = c.sb("acc", [128, 1024])
    S.G = [c.sb("G%d" % i, [128, 2048], BF16) for i in range(NG)]
    S.dg = [c.sb("dg%d" % i, [128, 128], BF16) for i in range(4)]
    S.ot = c.sb("ot", [128, 1024])
    S.psA = c.ps("psA", [128, 1024])
    S.psT = c.ps("psT", [128, 1024])
    S.psS = c.ps("psS", [128, 8, 128])
    S.psAcc = c.ps("psAcc", [128, 1024])


def tail_weights(c, S, A, layer):
    wo = A["even_w_out"][layer // 2] if layer % 2 == 0 else A["odd_w_out"][layer // 2]
    load_w_bf16(c, S, S.wout, wo, 8, 1024)
    load_w_bf16(c, S, S.wq, A["peer_w_q"][layer], 8, 1024)
    bcast_row(c, S.lng[0], A["mix_ln_g"][layer:layer + 1, :], 1024)
    bcast_row(c, S.lng[1], A["mix_ln_b"][layer:layer + 1, :], 1024)
    bcast_row(c, S.lng[2], A["ffn_ln_g"][layer:layer + 1, :], 1024)
    bcast_row(c, S.lng[3], A["ffn_ln_b"][layer:layer + 1, :], 1024)
    c.dma("sp", S.iota16[:], A["iota16"], writes=[S.iota16])
    sk = A["peer_sub_keys"][layer]
    for h in range(8):
        st = S.wstage[h % 2]
        for cc in range(2):
            c.dma(c.qsel(), st[:, cc * 64:(cc + 1) * 64], sk[h, cc], writes=[st])
        c.op("dve", lambda e, st=st: e.tensor_copy(S.xmb[:, 0:128], st[:, 0:128]), reads=[st], writes=[S.xmb])
        c.op("pe", lambda e: e.matmul(S.psT[:, 0:128], S.xmb[:, 0:128], S.identb[:], start=True, stop=True), reads=[S.xmb, S.identb], writes=[S.psT])
        c.op("act", lambda e, h=h: e.copy(S.keysT[:, h, :], S.psT[:, 0:128]), reads=[S.psT], writes=[S.keysT])


def tail_front(c, S, A, layer, x_src, oT_src, par):
    xm, xmb, idxu, gate = S.xm2[par], S.xmb2[par], S.idxu2[par], S.gate2[par]
    c.dma("sp", S.xt[:], x_src, writes=[S.xt], reads=[S.xs_tok])
    yield
    c.dma("act", S.oT[:].rearrange("p k t -> p (k t)"), oT_src, writes=[S.oT], reads=[S.oT_tok])
    yield
    for half in range(2):
        for k in range(8):
            c.op("pe", lambda e, k=k, half=half: e.matmul(S.psA[:, half * 512:(half + 1) * 512], S.oT[:, k, :],
                                                            S.wout[:, k, half * 512:(half + 1) * 512],
                                                            start=(k == 0), stop=(k == 7)),
                 reads=[S.oT, S.wout], writes=[S.psA])
            yield
    c.op("dve", lambda e: e.scalar_tensor_tensor(S.z[:], S.xt[:], ALPHA, S.psA[:], op0=ALU.mult, op1=ALU.add),
         reads=[S.xt, S.psA], writes=[S.z])
    yield
    layer_norm(c, S, xm[:], xm, S.z, S.lng[0], S.lng[1])
    yield
    c.op("act", lambda e: e.copy(xmb[:], xm[:]), reads=[xm], writes=[xmb])
    yield
    for k in range(8):
        c.op("pe", lambda e, k=k: e.matmul(S.psT[:, k * 128:(k + 1) * 128], xmb[:, k * 128:(k + 1) * 128], S.identb[:], start=True, stop=True),
             reads=[xmb, S.identb], writes=[S.psT])
        yield
    c.op("act", lambda e: e.copy(S.xmT[:].rearrange("p k t -> p (k t)"), S.psT[:]), reads=[S.psT], writes=[S.xmT])
    yield
    for h in range(8):
        for k in range(8):
            c.op("pe", lambda e, k=k, h=h: e.matmul(S.psA[:, h * 128:(h + 1) * 128], S.wq[:, k, h * 128:(h + 1) * 128],
                                                      S.xmT[:, k, :], start=(k == 0), stop=(k == 7)),
                 reads=[S.wq, S.xmT], writes=[S.psA])
            yield
    c.op("act", lambda e: e.copy(S.qT[:].rearrange("p k t -> p (k t)"), S.psA[:]), reads=[S.psA], writes=[S.qT])
    yield
    for hf in range(2):
        for j in range(8):
            hc = hf * 8 + j
            h, cc = hc // 2, hc % 2
            c.op("pe", lambda e, h=h, cc=cc, j=j: e.matmul(S.psS[:, j, :], S.qT[cc * 64:(cc + 1) * 64, h, :],
                                                            S.keysT[cc * 64:(cc + 1) * 64, h, :], start=True, stop=True),
                 reads=[S.qT, S.keysT], writes=[S.psS])
            yield
        for j in range(8):
            hc = hf * 8 + j
            c.op("dve", lambda e, hc=hc, j=j: e.max(S.tops[:, hc, 0:8], S.psS[:, j, :]), reads=[S.psS], writes=[S.tops])
            yield
            c.op("dve", lambda e, hc=hc, j=j: e.max_index(S.topi[:, hc, 0:8], S.tops[:, hc, 0:8], S.psS[:, j, :]),
                 reads=[S.psS, S.tops], writes=[S.topi])
            yield
            c.op("dve", lambda e, hc=hc, j=j: e.match_replace(S.ssc[:], S.tops[:, hc, 0:8], S.psS[:, j, :], NEG),
                 reads=[S.psS, S.tops], writes=[S.ssc])
            yield
            c.op("dve", lambda e, hc=hc: e.max(S.tops[:, hc, 8:16], S.ssc[:]), reads=[S.ssc], writes=[S.tops])
            yield
            c.op("dve", lambda e, hc=hc: e.max_index(S.topi[:, hc, 8:16], S.tops[:, hc, 8:16], S.ssc[:]),
                 reads=[S.ssc, S.tops], writes=[S.topi])
            yield
    c.op("dve", lambda e: e.tensor_copy(S.topf[:], S.topi[:]), reads=[S.topi], writes=[S.topf])
    yield
    tops4 = S.tops[:].rearrange("p (h c) k -> p h c k", c=2)
    topf4 = S.topf[:].rearrange("p (h c) k -> p h c k", c=2)
    c.op("dve", lambda e: e.tensor_scalar(topf4[:, :, 0, :], topf4[:, :, 0, :], 128.0, None, op0=ALU.mult),
         reads=[S.topf], writes=[S.topf])
    yield
    cs4 = S.cands[:].rearrange("p h (a b) -> p h a b", b=16)
    for h in range(8):
        c.op("pool", lambda e, h=h: e.tensor_tensor(cs4[:, h], tops4[:, h, 0, :].unsqueeze(2).to_broadcast([128, 16, 16]),
                                                     tops4[:, h, 1, :].unsqueeze(1).to_broadcast([128, 16, 16]), ALU.add),
             reads=[S.tops], writes=[S.cands])
        yield
    for h in range(8):
        c.op("dve", lambda e, h=h: e.max(S.best[:, h, 0:8], S.cands[:, h, :]), reads=[S.cands], writes=[S.best])
        yield
        c.op("dve", lambda e, h=h: e.max_index(S.pos[:, h, 0:8], S.best[:, h, 0:8], S.cands[:, h, :]), reads=[S.cands, S.best], writes=[S.pos])
        yield
        c.op("dve", lambda e, h=h: e.match_replace(S.candw[:], S.best[:, h, 0:8], S.cands[:, h, :], NEG),
             reads=[S.cands, S.best], writes=[S.candw])
        yield
        c.op("dve", lambda e, h=h: e.max(S.best[:, h, 8:16], S.candw[:]), reads=[S.candw], writes=[S.best])
        yield
        c.op("dve", lambda e, h=h: e.max_index(S.pos[:, h, 8:16], S.best[:, h, 8:16], S.candw[:]), reads=[S.candw, S.best], writes=[S.pos])
        yield
    posf = S.pos[:].rearrange("p h k -> p (h k)")
    c.op("dve", lambda e: e.tensor_scalar(S.pab[:, 0, :], posf, 4, None, op0=ALU.logical_shift_right), reads=[S.pos], writes=[S.pab])
    yield
    c.op("dve", lambda e: e.tensor_scalar(S.pab[:, 1, :], posf, 15, None, op0=ALU.bitwise_and), reads=[S.pos], writes=[S.pab])
    yield
    c.op("dve", lambda e: e.tensor_copy(S.pabf[:], S.pab[:]), reads=[S.pab], writes=[S.pabf])
    yield
    for ab in range(2):
        eq = S.eq[ab]
        c.op("dve", lambda e, ab=ab, eq=eq: e.tensor_tensor(eq[:], S.pabf[:, ab, :].unsqueeze(2).to_broadcast([128, 128, 16]),
                                                         S.iota16[:].unsqueeze(1).to_broadcast([128, 128, 16]), ALU.is_equal),
             reads=[S.pabf, S.iota16], writes=[eq])
        yield
        c.op("pool", lambda e, ab=ab, eq=eq: e.tensor_tensor(eq[:].rearrange("p (h k) a -> p h k a", h=8), eq[:].rearrange("p (h k) a -> p h k a", h=8),
                                                          topf4[:, :, ab, :].unsqueeze(2).to_broadcast([128, 8, 16, 16]), ALU.mult),
             reads=[S.topf, eq], writes=[eq])
        yield
        c.op("dve", lambda e, ab=ab, eq=eq: e.tensor_reduce(S.idx2[:, ab, :], eq[:], AX.X, ALU.add), reads=[eq], writes=[S.idx2])
        yield
    c.op("dve", lambda e: e.tensor_tensor(S.idxf[:], S.idx2[:, 0, :], S.idx2[:, 1, :], ALU.add), reads=[S.idx2], writes=[S.idxf])
    yield
    c.op("dve", lambda e: e.tensor_scalar(S.idxf[:], S.idxf[:], 16383.0, 0.0, op0=ALU.min, op1=ALU.max),
         reads=[S.idxf], writes=[S.idxf])
    yield
    c.op("dve", lambda e: e.tensor_copy(idxu[:], S.idxf[:]), reads=[S.idxf], writes=[idxu])
    yield
    c.op("dve", lambda e: e.tensor_tensor(gate[:], S.best[:], S.best[:, :, 0:1].to_broadcast([128, 8, 16]), ALU.subtract),
         reads=[S.best], writes=[gate])
    yield
    c.op("act", lambda e: e.activation(gate[:], gate[:], AF.Exp), reads=[gate], writes=[gate])
    yield
    c.op("dve", lambda e: e.tensor_reduce(S.gsum[:], gate[:], AX.X, ALU.add), reads=[gate], writes=[S.gsum])
    yield
    c.op("dve", lambda e: e.reciprocal(S.gsum[:], S.gsum[:]), reads=[S.gsum], writes=[S.gsum])
    yield
    c.op("dve", lambda e: e.tensor_tensor(gate[:], gate[:], S.gsum[:].unsqueeze(2).to_broadcast([128, 8, 16]), ALU.mult),
         reads=[gate, S.gsum], writes=[gate])
    yield
def tail_issue(c, S, par, k):
    G = S.G[(S.gi + k) % NG]
    c.gather(G[:], S.UV_s, S.idxu2[par][:, k:k + 1], reads=[S.idxu2[par], S.uv_tok], writes=[G])


def tail_back(c, S, par, out_dst, out_tok, fgen, nxt_par, prefetched):
    xm, xmb, idxu, gate = S.xm2[par], S.xmb2[par], S.idxu2[par], S.gate2[par]
    gate2 = gate[:].rearrange("p h k -> p (h k)")
    LOOK = NG - GS
    if not prefetched:
        for k in range(LOOK):
            tail_issue(c, S, par, k)
    issued = LOOK

    def pull(n):
        for _ in range(n):
            if next(fgen, "done") == "done":
                return
    for g0 in range(0, 128, GS):
        HH, CF = S.hh4[(g0 // GS) % 4], S.coef4[(g0 // GS) % 4]
        for k in range(g0, g0 + GS):
            G = S.G[(S.gi + k) % NG]
            JK = S.junk2[k % 2]
            c.op("dve", lambda e, G=G, k=k, JK=JK, HH=HH: e.scalar_tensor_tensor(JK[:], G[:, 0:1024], 1.0, xmb[:], op0=ALU.mult, op1=ALU.mult,
                                                                              accum_out=HH[:, k - g0:k - g0 + 1]),
                 reads=[G, xmb], writes=[JK, HH])
            pull(PULLD)
        c.op("act", lambda e, HH=HH, CF=CF: e.activation(CF[:], HH[:], AF.Gelu), reads=[HH], writes=[CF])
        c.op("dve", lambda e, g0=g0, CF=CF: e.tensor_tensor(CF[:], CF[:], gate2[:, g0:g0 + GS], ALU.mult),
             reads=[CF, gate], writes=[CF])
        for k in range(g0, g0 + GS):
            G = S.G[(S.gi + k) % NG]
            DG = S.dg[k % 4]
            c.op("act", lambda e, DG=DG, k=k, CF=CF: e.activation(DG[:], S.identb[:], AF.Copy, scale=CF[:, k - g0:k - g0 + 1]), reads=[S.identb, CF], writes=[DG])
            for half in range(2):
                c.op("pe", lambda e, DG=DG, G=G, half=half, k=k: e.matmul(S.psAcc[:, half * 512:(half + 1) * 512], DG[:],
                                                                          G[:, 1024 + half * 512:1536 + half * 512], start=(k == 0), stop=(k == 127)),
                     reads=[DG, G], writes=[S.psAcc])
            if issued < 128:
                tail_issue(c, S, par, issued)
                issued += 1
            pull(PULLN)
    pull(100000)
    S.gi += 128
    if nxt_par is not None:
        for k in range(LOOK):
            tail_issue(c, S, nxt_par, k)
    c.op("dve", lambda e: e.scalar_tensor_tensor(S.acc[:], xm[:], ALPHA, S.psAcc[:], op0=ALU.mult, op1=ALU.add),
         reads=[xm, S.psAcc], writes=[S.acc])
    layer_norm(c, S, S.ot[:], S.ot, S.acc, S.lng[2], S.lng[3])
    c.dma("sp", out_dst, S.ot[:], reads=[S.ot], writes=[out_tok], wtok=out_tok)


WSPEC = {
    "even_w_in": [2, 1024, 3248], "gla_a_w2": [2, 16, 256], "gla_a_b": [2, 256], "gla_norm_g": [2, 128],
    "rwkv_mu": [2, 1696], "rwkv_w0": [2, 512], "rwkv_w2": [2, 32, 512], "rwkv_a0": [2, 512],
    "rwkv_a2": [2, 32, 512], "rwkv_g2": [2, 96, 512], "rwkv_k_k": [2, 512], "rwkv_k_a": [2, 512],
    "rwkv_r_k": [2, 8, 64], "rwkv_lnx_g": [2, 512], "rwkv_lnx_b": [2, 512], "even_w_out": [2, 1024, 1024],
    "odd_w_in": [2, 1024, 1864], "odd_w_out": [2, 1024, 1024], "mix_ln_g": [4, 1024], "mix_ln_b": [4, 1024],
    "peer_w_q": [4, 1024, 1024], "peer_sub_keys": [4, 8, 2, 128, 64], "peer_u": [4, 16384, 1024],
    "peer_v": [4, 16384, 1024], "ffn_ln_g": [4, 1024], "ffn_ln_b": [4, 1024],
}


def consts():
    cs = {}
    cs["identb"] = np.eye(128, dtype=np.float32).astype(ml_dtypes.bfloat16)
    cs["identf"] = np.eye(128, dtype=np.float32)
    cs["iota16"] = np.tile(np.arange(16, dtype=np.float32)[None, :], (128, 1))
    cs.update(rope_consts())
    cs.update(even_consts())
    return cs


def shared_alloc(c, S, A):
    S.wstage = [c.sb("wst%d" % i, [128, 1024]) for i in range(2)]
    S.wsi = 0
    S.identb = c.sb("identb", [128, 128], BF16)
    S.identf = c.sb("identf", [128, 128])
    S.ln_st = c.sb("ln_st", [128, 2, 6])
    S.ln_mv = c.sb("ln_mv", [128, 2])
    S.ln_sd = c.sb("ln_sd", [128, 4])
    c.dma("sp", S.identb[:], A["identb"], writes=[S.identb])
    c.dma("sp", S.identf[:], A["identf"], writes=[S.identf])
    S.gi = 0


def build(NB=4, layers=(0, 1, 2, 3), mode="full"):
    nc = bass.Bass("TRN2", target_bir_lowering=False)
    NT = NB * T
    ntiles = NT // 128
    A = {}
    A["x"] = nc.dram_tensor("x", [NT, D], F32, kind="ExternalInput").ap()
    for k, shp in WSPEC.items():
        A[k] = nc.dram_tensor(k, shp, F32, kind="ExternalInput").ap()
    for k, v in consts().items():
        A[k] = nc.dram_tensor(k, list(v.shape), BF16 if v.dtype == ml_dtypes.bfloat16 else F32, kind="ExternalInput").ap()
    y = nc.dram_tensor("y", [NT, D], F32, kind="ExternalOutput").ap()
    if mode == "mixer":
        oT = nc.dram_tensor("oT_out", [ntiles, 128, 1024], BF16, kind="ExternalOutput").ap()
    elif mode == "tail":
        oT = nc.dram_tensor("oT_in", [ntiles, 128, 1024], BF16, kind="ExternalInput").ap()
    else:
        oT = nc.dram_tensor("oT_s", [ntiles, 128, 1024], BF16, kind="Internal").ap()
    xs = nc.dram_tensor("xs_s", [NT, D], F32, kind="Internal").ap()
    PR_s = nc.dram_tensor("PR_s", [NB, 14, 128, T], BF16, kind="Internal").ap()
    V_s = nc.dram_tensor("V_s", [NB, 128, 16 * 128], BF16, kind="Internal").ap()
    WI_s = nc.dram_tensor("WI_s", [NB, 128, 16 * 8], F32, kind="Internal").ap()
    UV_s = nc.dram_tensor("UV_s", [16384, 2048], BF16, kind="Internal").ap()
    EF_s = nc.dram_tensor("EF_s", [NB, 19, 128, T], F32, kind="Internal").ap()
    GV_s = nc.dram_tensor("GV_s", [NB, 16, 128, 512], BF16, kind="Internal").ap()
    GG_s = nc.dram_tensor("GG_s", [NB, 16, 128, 512], F32, kind="Internal").ap()
    es = ExitStack()
    with es:
        c = Ctx(nc, es)
        S = Shared()
        shared_alloc(c, S, A)
        S.xs_tok = c.tok("xs_tok")
        S.oT_tok = c.tok("oT_tok")
        S.y_tok = c.tok("y_tok")
        S.pr_tok = c.tok("pr_tok")
        S.PR_s, S.V_s, S.WI_s = PR_s, V_s, WI_s
        S.EF_s, S.GV_s, S.GG_s = EF_s, GV_s, GG_s
        S.UV_s = UV_s
        S.uv_tok = c.tok("uv_tok")
        for li, layer in enumerate(layers):
            x_in = A["x"] if li == 0 else xs
            last = (li == len(layers) - 1)
            if mode != "tail":
                with ExitStack() as es2:
                    c.es = es2
                    if layer % 2 == 0:
                        even_stage(c, S, A, layer, x_in, oT, NB)
                    else:
                        odd_stage(c, S, A, layer, x_in, oT, NB)
                    c.barrier()
            if mode == "mixer":
                continue
            uv_prep(c, S, A, layer)
            with ExitStack() as es2:
                c.es = es2
                tail_alloc(c, S)
                tail_weights(c, S, A, layer)
                dst = y if last else xs

                def mkfront(g):
                    return tail_front(c, S, A, layer, x_in[g * 128:(g + 1) * 128, :], oT[g], g % 2)

                for _ in mkfront(0):
                    pass
                for g in range(ntiles):
                    fgen = mkfront(g + 1) if g + 1 < ntiles else iter(())
                    tail_back(c, S, g % 2, dst[g * 128:(g + 1) * 128, :], S.y_tok if last else S.xs_tok, fgen,
                              (g + 1) % 2 if g + 1 < ntiles else None, g > 0)
                c.barrier()
            c.es = es
        c.finish([S.y_tok, S.xs_tok, S.oT_tok], "sp")
        c.barrier()
        print("ninst", c.ninst, "nsem", c.nsem)
    return nc


def rope_consts():
    def tabs(dim, reps):
        inv = (10000.0 ** (-np.arange(0, dim, 2, dtype=np.float32) / dim)).astype(np.float32)
        ang = np.arange(T, dtype=np.float32)[None, :] * inv[:, None]
        co, si = np.cos(ang).astype(np.float32), np.sin(ang).astype(np.float32)
        co = np.concatenate([co, co], 0)
        si = np.concatenate([si, si], 0)
        return np.ascontiguousarray(np.tile(co, (reps, 1))), np.ascontiguousarray(np.tile(si, (reps, 1)))
    cA, sA = tabs(128, 1)
    cI, sI = tabs(64, 2)
    tri = np.where(np.arange(128)[None, :] <= np.arange(128)[:, None], 0.0, NEG).astype(np.float32)
    return {"ropetab": np.ascontiguousarray(np.stack([cA, sA, cI, sI], 0)), "trineg": tri,
            "onesb": np.ones((128, 128), np.float32).astype(ml_dtypes.bfloat16)}


def odd_stage(c, S, A, layer, x_in, oT, NB):
    nc = c.nc
    i = layer // 2
    PR = S.PR_s
    VS = S.V_s
    WS = S.WI_s
    with ExitStack() as esA:
        c.es = esA
        win = c.sb("o_win", [128, 8, 1864], BF16)
        wrot = c.sb("o_wrot", [128, 8, 1792], BF16)
        wki2 = c.sb("o_wki2", [128, 8, 128], BF16)
        load_w_bf16(c, S, win, A["odd_w_in"][i], 8, 1864)
        for k in range(8):
            for (s0, d0, n, half) in ((0, 0, 1152, 64), (1280, 1152, 512, 32)):
                src = win[:, k, s0:s0 + n].rearrange("p (h two j) -> p h two j", two=2, j=half)
                dst = wrot[:, k, d0:d0 + n].rearrange("p (h two j) -> p h two j", two=2, j=half)
                c.op("dve", lambda e, src=src, dst=dst: e.tensor_scalar(dst[:, :, 0, :], src[:, :, 1, :], -1.0, None, op0=ALU.mult),
                     reads=[win], writes=[wrot])
                c.op("pool", lambda e, src=src, dst=dst: e.tensor_copy(dst[:, :, 1, :], src[:, :, 0, :]), reads=[win], writes=[wrot])
            for hlf in range(2):
                c.op("pool", lambda e, k=k, hlf=hlf: e.tensor_copy(wki2[:, k, hlf * 64:(hlf + 1) * 64], win[:, k, 1792:1856]),
                     reads=[win], writes=[wki2])
                c.op("dve", lambda e, k=k, hlf=hlf: e.tensor_scalar(wrot[:, k, 1664 + hlf * 64:1696 + hlf * 64], win[:, k, 1824:1856], -1.0, None, op0=ALU.mult),
                     reads=[win], writes=[wrot])
                c.op("pool", lambda e, k=k, hlf=hlf: e.tensor_copy(wrot[:, k, 1696 + hlf * 64:1728 + hlf * 64], win[:, k, 1792:1824]),
                     reads=[win], writes=[wrot])
        groups = []
        for h in range(8):
            groups.append((win, h * 128, wrot, h * 128, 0))
        groups.append((win, 1024, wrot, 1024, 0))
        for g in range(4):
            groups.append((win, 1280 + g * 128, wrot, 1152 + g * 128, 1))
        groups.append((wki2, 0, wrot, 1664, 1))
        xt = [c.sb("o_xt%d" % j, [128, 1024]) for j in range(2)]
        xb = [c.sb("o_xb%d" % j, [128, 1024], BF16) for j in range(2)]
        xTb = c.sb("o_xTb", [128, 8, 512], BF16)
        tabs = c.sb("o_tabs", [128, 4, 512])
        t1 = [c.sb("o_t1%d" % j, [128, 512]) for j in range(2)]
        t2 = [c.sb("o_t2%d" % j, [128, 512]) for j in range(2)]
        stg = [c.sb("o_stg%d" % j, [128, 512], BF16) for j in range(3)]
        vt = [c.sb("o_vt%d" % j, [128, 128], BF16) for j in range(2)]
        wt = [c.sb("o_wt%d" % j, [128, 8]) for j in range(2)]
        psQ = [c.ps("o_psQ%d" % j, [128, 512]) for j in range(2)]
        psR = [c.ps("o_psR%d" % j, [128, 512]) for j in range(2)]
        psT = c.ps("o_psT", [128, 1024], BF16)
        psV = c.ps("o_psV", [128, 512])
        cnt = 0
        for b in range(NB):
            for tb in range(4):
                c.dma("sp", tabs[:], A["ropetab"][:, :, tb * 512:(tb + 1) * 512].rearrange("f p t -> p f t"), writes=[tabs])
                for tt in range(4):
                    g = b * 16 + tb * 4 + tt
                    X, XB = xt[tt % 2], xb[tt % 2]
                    c.dma(c.qsel(), X[:], x_in[g * 128:(g + 1) * 128, :], writes=[X], reads=[S.xs_tok])
                    c.op("act", lambda e, X=X, XB=XB: e.copy(XB[:], X[:]), reads=[X], writes=[XB])
                    for k in range(8):
                        c.op("pe", lambda e, k=k, XB=XB: e.transpose(psT[:, k * 128:(k + 1) * 128], XB[:, k * 128:(k + 1) * 128], S.identb[:]),
                             reads=[XB, S.identb], writes=[psT])
                    c.op("dve", lambda e, tt=tt: e.tensor_copy(xTb[:, :, tt * 128:(tt + 1) * 128], psT[:].rearrange("p (k t) -> p k t", t=128)),
                         reads=[psT], writes=[xTb])
                for tt in range(4):
                    g = tb * 4 + tt
                    for k in range(8):
                        c.op("pe", lambda e, k=k, tt=tt: e.matmul(psV[:, 0:128], xTb[:, k, tt * 128:(tt + 1) * 128], win[:, k, 1152:1280],
                                                                    start=(k == 0), stop=(k == 7)), reads=[xTb, win], writes=[psV])
                    for k in range(8):
                        c.op("pe", lambda e, k=k, tt=tt: e.matmul(psV[:, 128:136], xTb[:, k, tt * 128:(tt + 1) * 128], win[:, k, 1856:1864],
                                                                    start=(k == 0), stop=(k == 7)), reads=[xTb, win], writes=[psV])
                    V, W = vt[tt % 2], wt[tt % 2]
                    c.op("act", lambda e, V=V: e.copy(V[:], psV[:, 0:128]), reads=[psV], writes=[V])
                    c.op("act", lambda e, W=W: e.copy(W[:], psV[:, 128:136]), reads=[psV], writes=[W])
                    c.dma("sp", VS[b, :, g * 128:(g + 1) * 128], V[:], reads=[V], writes=[S.pr_tok], wtok=V)
                    c.dma("sp", WS[b, :, g * 8:(g + 1) * 8], W[:], reads=[W], writes=[S.pr_tok], wtok=W)
                for gi, (w0, c0, w1, c1, tab) in enumerate(groups):
                    pq, pr = psQ[cnt % 2], psR[cnt % 2]
                    a1, a2, sg = t1[cnt % 2], t2[cnt % 2], stg[cnt % 3]
                    cnt += 1
                    for k in range(8):
                        c.op("pe", lambda e, k=k, w0=w0, c0=c0, pq=pq: e.matmul(pq[:], w0[:, k, c0:c0 + 128], xTb[:, k, :], start=(k == 0), stop=(k == 7)),
                             reads=[w0, xTb], writes=[pq])
                    for k in range(8):
                        c.op("pe", lambda e, k=k, w1=w1, c1=c1, pr=pr: e.matmul(pr[:], w1[:, k, c1:c1 + 128], xTb[:, k, :], start=(k == 0), stop=(k == 7)),
                             reads=[w1, xTb], writes=[pr])
                    c.op("dve", lambda e, pq=pq, a1=a1, tab=tab: e.tensor_tensor(a1[:], pq[:], tabs[:, 2 * tab, :], ALU.mult), reads=[pq, tabs], writes=[a1])
                    c.op("dve", lambda e, pr=pr, a2=a2, tab=tab: e.tensor_tensor(a2[:], pr[:], tabs[:, 2 * tab + 1, :], ALU.mult), reads=[pr, tabs], writes=[a2])
                    c.op("pool", lambda e, a1=a1, a2=a2, sg=sg: e.tensor_tensor(sg[:], a1[:], a2[:], ALU.add), reads=[a1, a2], writes=[sg])
                    c.dma(c.qsel(), PR[b, gi, :, tb * 512:(tb + 1) * 512], sg[:], reads=[sg], writes=[S.pr_tok], wtok=sg)
        c.barrier()
    with ExitStack() as esB:
        c.es = esB
        qT = c.sb("o_qT", [128, 8, T], BF16)
        kT = c.sb("o_kT", [128, T], BF16)
        qiT = c.sb("o_qiT", [128, 4, T], BF16)
        kiT = c.sb("o_kiT", [128, T], BF16)
        vtk = c.sb("o_vtk", [128, 16, 128], BF16)
        wi = c.sb("o_wi", [128, 16, 8])
        scP = [c.sb("o_sc%d" % j, [128, T]) for j in range(2)]
        workP = [c.sb("o_work%d" % j, [128, T]) for j in range(2)]
        tmpr = [c.sb("o_tmpr%d" % j, [128, 1024]) for j in range(2)]
        m8P = [c.sb("o_m8%d" % j, [128, 8]) for j in range(2)]
        thr0 = c.sb("o_thr0", [128, 1])
        maskfP = [c.sb("o_maskf%d" % j, [128, T], BF16) for j in range(2)]
        maskTP = [c.sb("o_maskT%d" % j, [128, 16, 128], BF16) for j in range(2)]
        E = [c.sb("o_E%d" % j, [128, 1024], BF16) for j in range(2)]
        PT = [c.sb("o_PT%d" % j, [128, 1024], BF16) for j in range(2)]
        rden = c.sb("o_rden", [128, 1024])
        oTt = [c.sb("o_oTt%d" % j, [128, 1024], BF16) for j in range(2)]
        trineg = c.sb("o_tri", [128, 128])
        onesb = c.sb("o_ones", [128, 128], BF16)
        psI = c.ps("o_psI", [128, 1024])
        psL = c.ps("o_psL", [128, 1024])
        psO = c.ps("o_psO", [128, 1024])
        psD = c.ps("o_psD", [128, 1024])
        psDb = psD[:].bitcast(BF16)
        c.dma("sp", trineg[:], A["trineg"], writes=[trineg])
        c.dma("sp", onesb[:], A["onesb"], writes=[onesb])
        c.op("pool", lambda e: e.memset(thr0[:], -1e29), writes=[thr0])
        SCALE = float(128 ** -0.5)
        for b in range(NB):
            for h in range(8):
                c.dma(c.qsel(), qT[:, h, :], PR[b, h], writes=[qT], reads=[S.pr_tok])
            c.dma(c.qsel(), kT[:], PR[b, 8], writes=[kT], reads=[S.pr_tok])
            for g in range(4):
                c.dma(c.qsel(), qiT[:, g, :], PR[b, 9 + g], writes=[qiT], reads=[S.pr_tok])
            c.dma(c.qsel(), kiT[:], PR[b, 13], writes=[kiT], reads=[S.pr_tok])
            c.dma(c.qsel(), vtk[:].rearrange("p g d -> p (g d)"), VS[b], writes=[vtk], reads=[S.pr_tok])
            c.dma(c.qsel(), wi[:].rearrange("p g d -> p (g d)"), WS[b], writes=[wi], reads=[S.pr_tok])
            def sA(qt):
                Sk = (qt + 1) * 128
                q0 = qt * 128
                sc, work, m8, maskf, maskT = scP[qt % 2], workP[qt % 2], m8P[qt % 2], maskfP[qt % 2], maskTP[qt % 2]
                for h in range(8):
                    hh, g = h % 2, h // 2
                    for blk in range((Sk + 1023) // 1024):
                        k0 = blk * 1024
                        n = min(1024, Sk - k0)
                        for sub in range((n + 511) // 512):
                            s0 = k0 + sub * 512
                            m = min(512, Sk - s0)
                            c.op("pe", lambda e, hh=hh, g=g, s0=s0, m=m, sub=sub: e.matmul(
                                psI[:, sub * 512:sub * 512 + m], qiT[hh * 64:(hh + 1) * 64, g, q0:q0 + 128],
                                kiT[hh * 64:(hh + 1) * 64, s0:s0 + m], start=True, stop=True),
                                reads=[qiT, kiT], writes=[psI])
                        if h == 0:
                            c.op("dve", lambda e, k0=k0, n=n, h=h: e.tensor_scalar(sc[:, k0:k0 + n], psI[:, 0:n], 0.0, wi[:, qt, h:h + 1], op0=ALU.max, op1=ALU.mult),
                                 reads=[psI, wi], writes=[sc])
                        else:
                            tr = tmpr[(h + blk) % 2]
                            c.op("dve", lambda e, k0=k0, n=n, h=h, tr=tr: e.tensor_scalar(tr[:, 0:n], psI[:, 0:n], 0.0, wi[:, qt, h:h + 1], op0=ALU.max, op1=ALU.mult),
                                 reads=[psI, wi], writes=[tr])
                            c.op("pool", lambda e, k0=k0, n=n, tr=tr: e.tensor_tensor(sc[:, k0:k0 + n], sc[:, k0:k0 + n], tr[:, 0:n], ALU.add),
                                 reads=[tr, sc], writes=[sc])
                c.op("pool", lambda e: e.tensor_tensor(sc[:, q0:q0 + 128], sc[:, q0:q0 + 128], trineg[:], ALU.add), reads=[sc, trineg], writes=[sc])
                if Sk <= 256:
                    thr_ap, thr_tok = thr0[:, 0:1], thr0
                else:
                    c.op("dve", lambda e: e.max(m8[:], sc[:, 0:Sk]), reads=[sc], writes=[m8])
                    c.op("dve", lambda e: e.match_replace(work[:, 0:Sk], m8[:], sc[:, 0:Sk], NEG), reads=[sc, m8], writes=[work])
                    for r in range(1, 32):
                        c.op("dve", lambda e: e.max(m8[:], work[:, 0:Sk]), reads=[work], writes=[m8])
                        if r < 31:
                            c.op("dve", lambda e: e.match_replace(work[:, 0:Sk], m8[:], work[:, 0:Sk], NEG), reads=[work, m8], writes=[work])
                    thr_ap, thr_tok = m8[:, 7:8], m8
                c.op("dve", lambda e, thr_ap=thr_ap: e.tensor_scalar(maskf[:, 0:Sk], sc[:, 0:Sk], thr_ap, None, op0=ALU.is_ge),
                     reads=[sc, thr_tok], writes=[maskf])
            def sB(qt):
                Sk = (qt + 1) * 128
                q0 = qt * 128
                sc, work, m8, maskf, maskT = scP[qt % 2], workP[qt % 2], m8P[qt % 2], maskfP[qt % 2], maskTP[qt % 2]
                for cb in range((qt + 8) // 8):
                    c0 = cb * 8
                    nch = min(8, qt + 1 - c0)
                    for j in range(nch):
                        c.op("pe", lambda e, j=j, c0=c0: e.transpose(psDb[:, j * 128:(j + 1) * 128], maskf[:, (c0 + j) * 128:(c0 + j + 1) * 128], S.identb[:]),
                             reads=[maskf, S.identb], writes=[psD])
                    c.op("act", lambda e, c0=c0, nch=nch: e.copy(maskT[:, c0:c0 + nch, :].rearrange("p c t -> p (c t)"), psDb[:, 0:nch * 128]),
                         reads=[psD], writes=[maskT])
            def sC(qt):
                Sk = (qt + 1) * 128
                q0 = qt * 128
                sc, work, m8, maskf, maskT = scP[qt % 2], workP[qt % 2], m8P[qt % 2], maskfP[qt % 2], maskTP[qt % 2]
                for ch in range(qt + 1):
                    Ej, Pj = E[ch % 2], PT[ch % 2]
                    for hf in range(2):
                        c.op("pe", lambda e, ch=ch, hf=hf: e.matmul(psL[:, hf * 512:(hf + 1) * 512], kT[:, ch * 128:(ch + 1) * 128],
                                                                      qT[:, hf * 4:(hf + 1) * 4, q0:q0 + 128], start=True, stop=True),
                             reads=[kT, qT], writes=[psL])
                    c.op("act", lambda e, Ej=Ej: e.activation(Ej[:], psL[:], AF.Exp, scale=SCALE), reads=[psL], writes=[Ej])
                    c.op("pool", lambda e, Ej=Ej, Pj=Pj, ch=ch: e.tensor_tensor(Pj[:].rearrange("p (h t) -> p h t", t=128), Ej[:].rearrange("p (h t) -> p h t", t=128),
                                                                               maskT[:, ch, :].unsqueeze(1).to_broadcast([128, 8, 128]), ALU.mult),
                         reads=[Ej, maskT], writes=[Pj])
                    for hf in range(2):
                        c.op("pe", lambda e, ch=ch, hf=hf, Pj=Pj: e.matmul(psO[:, hf * 512:(hf + 1) * 512], vtk[:, ch, :], Pj[:, hf * 512:(hf + 1) * 512],
                                                                             start=(ch == 0), stop=(ch == qt)), reads=[vtk, Pj], writes=[psO])
                        c.op("pe", lambda e, ch=ch, hf=hf, Pj=Pj: e.matmul(psD[:, hf * 512:(hf + 1) * 512], onesb[:], Pj[:, hf * 512:(hf + 1) * 512],
                                                                             start=(ch == 0), stop=(ch == qt)), reads=[onesb, Pj], writes=[psD])
                c.op("dve", lambda e: e.reciprocal(rden[:], psD[:]), reads=[psD], writes=[rden])
                O = oTt[qt % 2]
                c.op("dve", lambda e, O=O: e.tensor_tensor(O[:], psO[:], rden[:], ALU.mult), reads=[psO, rden], writes=[O])
                c.dma("sp", oT[b * 16 + qt], O[:], reads=[O], writes=[S.oT_tok], wtok=O)
            sA(0)
            sB(0)
            for qt in range(16):
                if qt + 1 < 16:
                    sA(qt + 1)
                sC(qt)
                if qt + 1 < 16:
                    sB(qt + 1)
        c.barrier()


def even_consts():
    j = np.arange(128)[:, None]
    i_ = np.arange(128)[None, :]
    bc = ((j // 64 == i_ // 64) & (j <= i_)).astype(np.float32)
    bo = (j // 64 == i_ // 64).astype(np.float32)
    ah = np.zeros((128, 255), np.float32)
    ah[:, 127] = 1.0
    return {"bcmask": bc, "blockones": bo, "blockonesb": bo.astype(ml_dtypes.bfloat16), "ahwin": ah.astype(ml_dtypes.bfloat16)}


EV_GROUPS = ([(c0, 128, False, 0) for c0 in (0, 128, 256, 384)] + [(1536, 16, False, 0)] +
             [(1552 + p * 128, 128, True, p * 128) for p in range(4)] +
             [(2064 + p * 128, 128, True, 512 + p * 128) for p in range(4)] +
             [(2576 + p * 128, 128, True, 1024 + p * 128) for p in range(4)] +
             [(3088, 64, True, 1536), (3152, 96, True, 1600)])


def even_phaseA(c, S, A, layer, x_in, NB):
    i = layer // 2
    EF, GV, GG = S.EF_s, S.GV_s, S.GG_s
    with ExitStack() as esA:
        c.es = esA
        win = c.sb("e_win", [128, 8, 3248], BF16)
        load_w_bf16(c, S, win, A["even_w_in"][i], 8, 3248)
        mu = c.sb("e_mu", [128, 19])
        for gi, (c0, n, sh, rc) in enumerate(EV_GROUPS):
            if sh:
                c.dma(c.qsel(), mu[0:n, gi:gi + 1], A["rwkv_mu"][i:i + 1, rc:rc + n].rearrange("o n -> n o"), writes=[mu])
        xt = [c.sb("e_xt%d" % j, [128, 1024]) for j in range(2)]
        xb = [c.sb("e_xb%d" % j, [128, 1024], BF16) for j in range(2)]
        xT = c.sb("e_xT", [128, 8, 513], BF16)
        t1 = [c.sb("e_t1%d" % j, [128, 512]) for j in range(2)]
        t2 = [c.sb("e_t2%d" % j, [128, 512]) for j in range(2)]
        stg = [c.sb("e_stg%d" % j, [128, 512]) for j in range(3)]
        vst = [c.sb("e_vst%d" % j, [128, 512], BF16) for j in range(2)]
        gst = [c.sb("e_gst%d" % j, [128, 512]) for j in range(2)]
        psQ = [c.ps("e_psQ%d" % j, [128, 512]) for j in range(2)]
        psR = [c.ps("e_psR%d" % j, [128, 512]) for j in range(2)]
        psT = c.ps("e_psT", [128, 1024], BF16)
        psV = c.ps("e_psV", [128, 1024])
        cnt = 0
        for b in range(NB):
            c.op("pool", lambda e: e.memset(xT[:, :, 0:1], 0.0), writes=[xT])
            for tb in range(4):
                if tb > 0:
                    c.op("pool", lambda e: e.tensor_copy(xT[:, :, 0:1], xT[:, :, 512:513]), reads=[xT], writes=[xT])
                for tt in range(4):
                    g = b * 16 + tb * 4 + tt
                    X, XB = xt[tt % 2], xb[tt % 2]
                    c.dma(c.qsel(), X[:], x_in[g * 128:(g + 1) * 128, :], writes=[X], reads=[S.xs_tok])
                    c.op("act", lambda e, X=X, XB=XB: e.copy(XB[:], X[:]), reads=[X], writes=[XB])
                    for k in range(8):
                        c.op("pe", lambda e, k=k, XB=XB: e.transpose(psT[:, k * 128:(k + 1) * 128], XB[:, k * 128:(k + 1) * 128], S.identb[:]),
                             reads=[XB, S.identb], writes=[psT])
                    c.op("dve", lambda e, tt=tt: e.tensor_copy(xT[:, :, 1 + tt * 128:1 + (tt + 1) * 128], psT[:].rearrange("p (k t) -> p k t", t=128)),
                         reads=[psT], writes=[xT])
                for tt in range(4):
                    g = tb * 4 + tt
                    for hf, c0 in enumerate((512, 1024)):
                        for k in range(8):
                            c.op("pe", lambda e, k=k, tt=tt, hf=hf, c0=c0: e.matmul(psV[:, hf * 512:(hf + 1) * 512], xT[:, k, 1 + tt * 128:1 + (tt + 1) * 128],
                                                                                      win[:, k, c0:c0 + 512], start=(k == 0), stop=(k == 7)),
                                 reads=[xT, win], writes=[psV])
                    V, G = vst[tt % 2], gst[tt % 2]
                    c.op("dve", lambda e, V=V: e.tensor_copy(V[:], psV[:, 0:512]), reads=[psV], writes=[V])
                    c.op("act", lambda e, G=G: e.activation(G[:], psV[:, 512:1024], AF.Silu), reads=[psV], writes=[G])
                    c.dma("sp", GV[b, g], V[:], reads=[V], writes=[S.pr_tok], wtok=V)
                    c.dma("sp", GG[b, g], G[:], reads=[G], writes=[S.pr_tok], wtok=G)
                for gi, (c0, n, sh, rc) in enumerate(EV_GROUPS):
                    pq, pr = psQ[cnt % 2], psR[cnt % 2]
                    a1, a2, sg = t1[cnt % 2], t2[cnt % 2], stg[cnt % 3]
                    cnt += 1
                    for k in range(8):
                        c.op("pe", lambda e, k=k, c0=c0, n=n, pq=pq: e.matmul(pq[0:n, :], win[:, k, c0:c0 + n], xT[:, k, 1:513], start=(k == 0), stop=(k == 7)),
                             reads=[win, xT], writes=[pq])
                    if not sh:
                        c.op("act", lambda e, pq=pq, sg=sg, n=n: e.copy(sg[0:n, :], pq[0:n, :]), reads=[pq], writes=[sg])
                    else:
                        for k in range(8):
                            c.op("pe", lambda e, k=k, c0=c0, n=n, pr=pr: e.matmul(pr[0:n, :], win[:, k, c0:c0 + n], xT[:, k, 0:512], start=(k == 0), stop=(k == 7)),
                                 reads=[win, xT], writes=[pr])
                        c.op("act", lambda e, pq=pq, a1=a1, n=n: e.copy(a1[0:n, :], pq[0:n, :]), reads=[pq], writes=[a1])
                        c.op("dve", lambda e, pr=pr, a1=a1, a2=a2, n=n: e.tensor_tensor(a2[0:n, :], pr[0:n, :], a1[0:n, :], ALU.subtract), reads=[pr, a1], writes=[a2])
                        c.op("dve", lambda e, a1=a1, a2=a2, sg=sg, n=n, gi=gi: e.scalar_tensor_tensor(sg[0:n, :], a2[0:n, :], mu[0:n, gi:gi + 1], a1[0:n, :], op0=ALU.mult, op1=ALU.add),
                             reads=[a1, a2, mu], writes=[sg])
                    c.dma(c.qsel(), EF[b, gi, 0:n, tb * 512:(tb + 1) * 512], sg[0:n, :], reads=[sg], writes=[S.pr_tok], wtok=sg)
        c.barrier()


def gla_phase(c, S, A, layer, oT, NB):
    i = layer // 2
    EF, GV, GG = S.EF_s, S.GV_s, S.GG_s
    with ExitStack() as esB:
        c.es = esB
        aw2 = c.sb("g_aw2", [16, 256])
        nab = c.sb("g_nab", [128, 2])
        ngb = c.sb("g_ngb", [128, 128])
        msk = c.sb("g_msk", [128, T])
        bcm = c.sb("g_bcm", [128, 128])
        qf = c.sb("g_qf", [128, T])
        kf = c.sb("g_kf", [128, T])
        gal = c.sb("g_gal", [16, T])
        cum = c.sb("g_cum", [128, T])
        ex = c.sb("g_ex", [128, T])
        dec = c.sb("g_dec", [128, 32])
        qz = c.sb("g_qz", [128, 16, 2, 128], BF16)
        kin = c.sb("g_kin", [128, T], BF16)
        kout = c.sb("g_kout", [128, T], BF16)
        ktok = [c.sb("g_ktok%d" % j, [128, 128], BF16) for j in range(2)]
        vt = [c.sb("g_vt%d" % j, [128, 512], BF16) for j in range(2)]
        gg = [c.sb("g_gg%d" % j, [128, 512]) for j in range(2)]
        At = [c.sb("g_At%d" % j, [128, 128], BF16) for j in range(2)]
        Sf = [c.sb("g_Sf%d" % j, [128, 128]) for j in range(2)]
        Sb = [c.sb("g_Sb%d" % j, [128, 128], BF16) for j in range(3)]
        osball = c.sb("g_osb", [128, 16, 512])
        ss = c.sb("g_ss", [128, 8])
        junk = c.sb("g_junk", [128, 128])
        gob = c.sb("g_gob", [128, 512], BF16)
        oTt = [c.sb("g_oTt%d" % j, [128, 512], BF16) for j in range(2)]
        psZ = c.ps("g_psZ", [128, 512])
        psA = c.ps("g_psA", [128, 128])
        psK = c.ps("g_psK", [128, 128], BF16)
        psKV = c.ps("g_psKV", [128, 256])
        psO = [c.ps("g_psO%d" % j, [128, 128]) for j in range(2)]
        psTt = c.ps("g_psT", [128, 512], BF16)
        c.dma("sp", aw2[:], A["gla_a_w2"][i], writes=[aw2])
        for p in range(2):
            c.dma("sp", nab[:, p:p + 1], A["gla_a_b"][i:i + 1, p * 128:(p + 1) * 128].rearrange("o n -> n o"), writes=[nab])
        c.op("dve", lambda e: e.tensor_scalar(nab[:], nab[:], -1.0, None, op0=ALU.mult), reads=[nab], writes=[nab])
        bcast_row(c, ngb, A["gla_norm_g"][i:i + 1, :], 128)
        c.dma("sp", bcm[:], A["bcmask"], writes=[bcm])
        c.op("pool", lambda e: e.memset(msk[:], 1.0), writes=[msk])
        c.op("pool", lambda e: e.memset(msk[:].rearrange("p (n c) -> p n c", c=64)[:, :, 0:1], 0.0), writes=[msk])
        c.op("pool", lambda e: e.memset(qz[:].rearrange("p a b c -> p (a b c)"), 0.0), writes=[qz])
        sbi = 0
        for b in range(NB):
            c.dma(c.qsel(), gal[:], EF[b, 4, 0:16, :], writes=[gal], reads=[S.pr_tok])
            for p in range(2):
                c.dma(c.qsel(), qf[:], EF[b, p], writes=[qf], reads=[S.pr_tok])
                c.dma(c.qsel(), kf[:], EF[b, 2 + p], writes=[kf], reads=[S.pr_tok])
                for tb in range(4):
                    c.op("pe", lambda e, tb=tb, p=p: e.matmul(psZ[:], aw2[:, p * 128:(p + 1) * 128], gal[:, tb * 512:(tb + 1) * 512], start=True, stop=True),
                         reads=[aw2, gal], writes=[psZ])
                    c.op("act", lambda e, tb=tb, p=p: e.activation(ex[:, tb * 512:(tb + 1) * 512], psZ[:], AF.Exp, scale=-1.0, bias=nab[:, p:p + 1]),
                         reads=[psZ, nab], writes=[ex])
                c.op("act", lambda e: e.activation(ex[:], ex[:], AF.Ln, bias=1.0), reads=[ex], writes=[ex])
                c.op("dve", lambda e: e.tensor_scalar(ex[:], ex[:], -1.0 / 16.0, None, op0=ALU.mult), reads=[ex], writes=[ex])
                c.op("dve", lambda e: e.tensor_tensor_scan(cum[:], msk[:], ex[:], 0.0, ALU.mult, ALU.add), reads=[msk, ex], writes=[cum])
                cum3 = cum[:].rearrange("p (n c) -> p n c", c=64)
                c.op("act", lambda e: e.activation(dec[:], cum3[:, :, 63], AF.Exp), reads=[cum], writes=[dec])
                c.op("act", lambda e: e.activation(ex[:], cum[:], AF.Exp), reads=[cum], writes=[ex])
                for par in range(2):
                    src_e = ex[:].rearrange("p (t two c) -> p t two c", two=2, c=64)[:, :, par, :]
                    src_q = qf[:].rearrange("p (t two c) -> p t two c", two=2, c=64)[:, :, par, :]
                    c.op("dve", lambda e, par=par, src_e=src_e, src_q=src_q: e.scalar_tensor_tensor(
                        qz[:, :, par, par * 64:(par + 1) * 64], src_q, 0.125, src_e, op0=ALU.mult, op1=ALU.mult),
                        reads=[qf, ex], writes=[qz])
                c.op("act", lambda e: e.activation(ex[:], cum[:], AF.Exp, scale=-1.0), reads=[cum], writes=[ex])
                c.op("dve", lambda e: e.tensor_tensor(kin[:], kf[:], ex[:], ALU.mult), reads=[kf, ex], writes=[kin])
                c.op("dve", lambda e: e.tensor_tensor(ex[:].rearrange("p (n c) -> p n c", c=64), cum3[:, :, 63:64].to_broadcast([128, 32, 64]), cum3, ALU.subtract),
                     reads=[cum], writes=[ex])
                c.op("act", lambda e: e.activation(ex[:], ex[:], AF.Exp), reads=[ex], writes=[ex])
                c.op("dve", lambda e: e.tensor_tensor(kout[:], kf[:], ex[:], ALU.mult), reads=[kf, ex], writes=[kout])
                Sc = Sf[0]
                c.op("pool", lambda e, Sc=Sc: e.memset(Sc[:], 0.0), writes=[Sc])
                S0b = Sb[sbi % 3]; sbi += 1
                c.op("pool", lambda e, S0b=S0b: e.memset(S0b[:], 0.0), writes=[S0b])
                for tt in range(16):
                    V, Gg = vt[tt % 2], gg[tt % 2]
                    c.dma(c.qsel(), V[:], GV[b, tt], writes=[V], reads=[S.pr_tok])
                    if p == 1:
                        c.dma(c.qsel(), Gg[:], GG[b, tt], writes=[Gg], reads=[S.pr_tok])
                    t0 = tt * 128
                    KT = ktok[tt % 2]
                    c.op("pe", lambda e, t0=t0: e.transpose(psK[:], kout[:, t0:t0 + 128], S.identb[:]), reads=[kout, S.identb], writes=[psK])
                    c.op("act", lambda e, KT=KT: e.copy(KT[:], psK[:]), reads=[psK], writes=[KT])
                    Sbs = [S0b]
                    Scur = Sc
                    for ch in range(2):
                        n = tt * 2 + ch
                        c.op("pe", lambda e, ch=ch, KT=KT, V=V, p=p: e.matmul(psKV[:], KT[ch * 64:(ch + 1) * 64, :], V[ch * 64:(ch + 1) * 64, p * 256:(p + 1) * 256],
                                                                               start=True, stop=True), reads=[KT, V], writes=[psKV])
                        Snew = Sf[(tt * 2 + ch + 1) % 2]
                        for hh in range(2):
                            c.op("dve", lambda e, hh=hh, n=n, Scur=Scur, Snew=Snew: e.scalar_tensor_tensor(
                                Snew[hh * 64:(hh + 1) * 64, :], Scur[hh * 64:(hh + 1) * 64, :], dec[hh * 64:(hh + 1) * 64, n:n + 1],
                                psKV[hh * 64:(hh + 1) * 64, hh * 128:(hh + 1) * 128], op0=ALU.mult, op1=ALU.add),
                                reads=[Scur, dec, psKV], writes=[Snew])
                        Sn_b = Sb[sbi % 3]; sbi += 1
                        c.op("act", lambda e, Snew=Snew, Sn_b=Sn_b: e.copy(Sn_b[:], Snew[:]), reads=[Snew], writes=[Sn_b])
                        Sbs.append(Sn_b)
                        Scur = Snew
                    Sc = Scur
                    for hh in range(2):
                        h = p * 2 + hh
                        pl, ph = hh * 64, (hh + 1) * 64
                        AT = At[hh]
                        PO = psO[hh]
                        for par in range(2):
                            c.op("pe", lambda e, pl=pl, ph=ph, t0=t0, par=par, tt=tt: e.matmul(psA[:], kin[pl:ph, t0:t0 + 128], qz[pl:ph, tt, par, :],
                                                                                               start=(par == 0), stop=(par == 1)),
                                 reads=[kin, qz], writes=[psA])
                        c.op("dve", lambda e, AT=AT: e.tensor_tensor(AT[:], psA[:], bcm[:], ALU.mult), reads=[psA, bcm], writes=[AT])
                        c.op("pe", lambda e, AT=AT, V=V, h=h, PO=PO: e.matmul(PO[:], AT[:], V[:, h * 128:(h + 1) * 128], start=True, stop=False),
                             reads=[AT, V], writes=[PO])
                        for ch in range(2):
                            c.op("pe", lambda e, ch=ch, pl=pl, ph=ph, PO=PO, sbv=Sbs[ch]: e.matmul(PO[:], qz[pl:ph, tt, ch, :], sbv[pl:ph, :], start=False, stop=(ch == 1)),
                                 reads=[qz, Sbs[ch]], writes=[PO])
                        c.op("act", lambda e, PO=PO, h=h, tt=tt: e.copy(osball[:, tt, h * 128:(h + 1) * 128], PO[:]), reads=[PO], writes=[osball])
                    S0b = Sbs[2]
                    if p == 1:
                        for h in range(4):
                            c.op("act", lambda e, h=h, tt=tt: e.activation(junk[:], osball[:, tt, h * 128:(h + 1) * 128], AF.Square, accum_out=ss[:, h:h + 1]),
                                 reads=[osball], writes=[junk, ss])
                        c.op("dve", lambda e: e.tensor_scalar(ss[:, 0:4], ss[:, 0:4], 1.0 / 128.0, LN_EPS, op0=ALU.mult, op1=ALU.add), reads=[ss], writes=[ss])
                        c.op("act", lambda e: e.activation(ss[:, 0:4], ss[:, 0:4], AF.Sqrt), reads=[ss], writes=[ss])
                        c.op("dve", lambda e: e.reciprocal(ss[:, 4:8], ss[:, 0:4]), reads=[ss], writes=[ss])
                        for h in range(4):
                            c.op("dve", lambda e, h=h, tt=tt: e.scalar_tensor_tensor(osball[:, tt, h * 128:(h + 1) * 128], osball[:, tt, h * 128:(h + 1) * 128], ss[:, 4 + h:5 + h], ngb[:],
                                                                                      op0=ALU.mult, op1=ALU.mult), reads=[osball, ss, ngb], writes=[osball])
                        c.op("pool", lambda e, tt=tt, Gg=Gg: e.tensor_tensor(gob[:], osball[:, tt, :], Gg[:], ALU.mult), reads=[osball, Gg], writes=[gob])
                        for h in range(4):
                            c.op("pe", lambda e, h=h: e.transpose(psTt[:, h * 128:(h + 1) * 128], gob[:, h * 128:(h + 1) * 128], S.identb[:]),
                                 reads=[gob, S.identb], writes=[psTt])
                        O = oTt[tt % 2]
                        c.op("act", lambda e, O=O: e.copy(O[:], psTt[:]), reads=[psTt], writes=[O])
                        c.dma("sp", oT[b * 16 + tt][:, 0:512], O[:], reads=[O], writes=[S.oT_tok], wtok=O)
        c.barrier()


def even_stage(c, S, A, layer, x_in, oT, NB):
    even_phaseA(c, S, A, layer, x_in, NB)
    gla_phase(c, S, A, layer, oT, NB)
    rwkv_phase(c, S, A, layer, oT, NB)


def rwkv_phase(c, S, A, layer, oT, NB):
    i = layer // 2
    EF = S.EF_s
    NBH = NB * 4
    NF = NBH * 64
    PW = max(NF, 512)
    chunks = [(c0, min(NF, c0 + 512)) for c0 in range(0, NF, 512)]
    DEC_SCALE = -float(np.exp(-0.5))
    with ExitStack() as esB:
        c.es = esB
        HG = NBH // 2
        HW = HG * 64
        PWH = max(HW, 512)
        Zq = [[c.sb("r_Z%d_%d" % (q, j), [128, HW]) for j in range(2)] for q in range(2)]
        tA = [c.sb("r_tA%d" % q, [128, HW], BF16) for q in range(2)]
        tB = [c.sb("r_tB%d" % q, [128, HW]) for q in range(2)]
        tP = [c.sb("r_tP%d" % q, [128, HW]) for q in range(2)]
        tD = [c.sb("r_tD%d" % q, [128, HW], BF16) for q in range(2)]
        vs = [c.sb("r_vs%d" % q, [128, HW]) for q in range(2)]
        tCn = [[c.sb("r_tCn%d_%d" % (q, j), [128, HW]) for j in range(2)] for q in range(2)]
        WOP, AOP, BOP, KOP, ROP = [c.sb("r_op%d" % j, [128, 128, NBH]) for j in range(5)]
        vtok = c.sb("r_vtok", [128, NBH, 128], BF16)
        bonus = c.sb("r_bonus", [128, NBH, 128])
        gT = c.sb("r_gT", [128, NBH, 128])
        ysb = c.sb("r_ysb", [128, NBH * 2, 64])
        ysq = c.sb("r_ysq", [128, NBH * 2, 64])
        yst = c.sb("r_yst", [128, NBH * 2, 2])
        w2b = c.sb("r_w2b", [32, 512], BF16)
        a2b = c.sb("r_a2b", [64, 512], BF16)
        g2b = c.sb("r_g2b", [96, 512], BF16)
        pp = c.sb("r_pp", [128, 4, 8])
        bof = c.sb("r_bof", [128, 128])
        bob = c.sb("r_bob", [128, 128], BF16)
        ahw = c.sb("r_ahw", [128, 255], BF16)
        rT = [c.sb("r_rT%d" % j, [128, 128]) for j in range(2)]
        kTt = [c.sb("r_kT%d" % j, [128, 128]) for j in range(2)]
        vT = [c.sb("r_vT%d" % j, [128, 128]) for j in range(2)]
        wlal = c.sb("r_wlal", [64, 128])
        glt = c.sb("r_glt", [96, 128])
        twb = c.sb("r_twb", [64, 128], BF16)
        sglb = c.sb("r_sglb", [96, 128], BF16)
        ET = [[c.sb("r_e%d_%d" % (k, j), [128, 128]) for k in range(5)] for j in range(2)]
        yo = [c.sb("r_yo%d" % j, [128, 128]) for j in range(2)]
        yob = [c.sb("r_yob%d" % j, [128, 128], BF16) for j in range(2)]
        psSAq = [c.ps("r_psSA%d" % q, [128, PWH]) for q in range(2)]
        psVBq = [c.ps("r_psVB%d" % q, [128, PWH]) for q in range(2)]
        psYq = [[c.ps("r_psY%d_%d" % (q, j), [128, HW]) for j in range(2)] for q in range(2)]
        psSA, psVB = psSAq[0], psVBq[0]
        for (dst, nm, rows, pofs) in ((w2b, "rwkv_w2", 32, 0), (a2b, "rwkv_a2", 32, 32), (g2b, "rwkv_g2", 96, 0)):
            st = S.wstage[0]
            c.dma("sp", st[pofs:pofs + rows, 0:512], A[nm][i], writes=[st])
            c.op("dve", lambda e, dst=dst, st=st, rows=rows, pofs=pofs: e.tensor_copy(dst[pofs:pofs + rows, :], st[pofs:pofs + rows, 0:512]), reads=[st], writes=[dst])
        for j, nm in enumerate(("rwkv_w0", "rwkv_a0", "rwkv_k_k", "rwkv_k_a", None, "rwkv_r_k", "rwkv_lnx_g", "rwkv_lnx_b")):
            if nm is None:
                continue
            src = A[nm][i:i + 1] if nm != "rwkv_r_k" else A[nm][i:i + 1].rearrange("o h d -> o (h d)")
            for hp in range(4):
                c.dma(c.qsel(), pp[:, hp, j:j + 1], src[:, hp * 128:(hp + 1) * 128].rearrange("o n -> n o"), writes=[pp])
        c.op("dve", lambda e: e.tensor_scalar(pp[:, :, 4], pp[:, :, 3], -1.0, 1.0, op0=ALU.mult, op1=ALU.add), reads=[pp], writes=[pp])
        c.dma("sp", bof[:], A["blockones"], writes=[bof])
        c.dma("sp", bob[:], A["blockonesb"], writes=[bob])
        c.dma("sp", ahw[:], A["ahwin"], writes=[ahw])
        for q in range(2):
            c.op("pool", lambda e, q=q: e.memset(Zq[q][0][:], 0.0), writes=[Zq[q][0]])
        step = 0
        for tb in range(16):
            t0 = tb * 128
            for b in range(NB):
                c.dma(c.qsel(), wlal[:], EF[b, 17, 0:64, t0:t0 + 128], writes=[wlal], reads=[S.pr_tok])
                c.dma(c.qsel(), glt[:], EF[b, 18, 0:96, t0:t0 + 128], writes=[glt], reads=[S.pr_tok])
                c.op("act", lambda e: e.activation(twb[0:32, :], wlal[0:32, :], AF.Tanh), reads=[wlal], writes=[twb])
                c.op("act", lambda e: e.copy(twb[32:64, :], wlal[32:64, :]), reads=[wlal], writes=[twb])
                c.op("act", lambda e: e.activation(sglb[:], glt[:], AF.Sigmoid), reads=[glt], writes=[sglb])
                for hp in range(4):
                    bh = b * 4 + hp
                    R_, K_, V_ = rT[bh % 2], kTt[bh % 2], vT[bh % 2]
                    pSA, pVB = psSAq[bh % 2], psVBq[bh % 2]
                    e1, e2, e3, e4, asb = ET[bh % 2]
                    c.dma(c.qsel(), R_[:], EF[b, 5 + hp, :, t0:t0 + 128], writes=[R_], reads=[S.pr_tok])
                    c.dma(c.qsel(), K_[:], EF[b, 9 + hp, :, t0:t0 + 128], writes=[K_], reads=[S.pr_tok])
                    c.dma(c.qsel(), V_[:], EF[b, 13 + hp, :, t0:t0 + 128], writes=[V_], reads=[S.pr_tok])
                    cs = slice(hp * 128, (hp + 1) * 128)
                    c.op("pe", lambda e, cs=cs: e.matmul(pSA[:, 0:128], w2b[0:32, cs], twb[0:32, :], start=True, stop=True), reads=[w2b, twb], writes=[pSA])
                    c.op("act", lambda e, hp=hp: e.activation(e1[:], pSA[:, 0:128], AF.Sigmoid, bias=pp[:, hp, 0:1]), reads=[pSA, pp], writes=[e1])
                    c.op("act", lambda e, bh=bh: e.activation(WOP[:, :, bh], e1[:], AF.Exp, scale=DEC_SCALE), reads=[e1], writes=[WOP])
                    c.op("pe", lambda e, cs=cs: e.matmul(pSA[:, 128:256], a2b[32:64, cs], twb[32:64, :], start=True, stop=True), reads=[a2b, twb], writes=[pSA])
                    c.op("act", lambda e, hp=hp: e.activation(asb[:], pSA[:, 128:256], AF.Sigmoid, bias=pp[:, hp, 1:2]), reads=[pSA, pp], writes=[asb])
                    c.op("pe", lambda e, cs=cs: e.matmul(pVB[:, 0:128], g2b[0:96, cs], sglb[0:96, :], start=True, stop=True), reads=[g2b, sglb], writes=[pVB])
                    c.op("act", lambda e, bh=bh: e.copy(gT[:, bh, :], pVB[:, 0:128]), reads=[pVB], writes=[gT])
                    c.op("dve", lambda e, K_=K_, hp=hp: e.tensor_scalar(e2[:], K_[:], pp[:, hp, 2:3], None, op0=ALU.mult), reads=[K_, pp], writes=[e2])
                    c.op("pool", lambda e: e.tensor_tensor(e3[:], e2[:], e2[:], ALU.mult), reads=[e2], writes=[e3])
                    c.op("pe", lambda e: e.matmul(pVB[:, 128:256], bof[:], e3[:], start=True, stop=True), reads=[bof, e3], writes=[pVB])
                    c.op("act", lambda e: e.activation(e3[:], pVB[:, 128:256], AF.Sqrt), reads=[pVB], writes=[e3])
                    c.op("dve", lambda e: e.tensor_scalar(e3[:], e3[:], 1e-12, None, op0=ALU.max), reads=[e3], writes=[e3])
                    c.op("dve", lambda e: e.reciprocal(e3[:], e3[:]), reads=[e3], writes=[e3])
                    c.op("dve", lambda e: e.tensor_tensor(e2[:], e2[:], e3[:], ALU.mult), reads=[e2, e3], writes=[e2])
                    c.op("dve", lambda e, bh=bh: e.tensor_scalar(AOP[:, :, bh], e2[:], -1.0, None, op0=ALU.mult), reads=[e2], writes=[AOP])
                    c.op("dve", lambda e, bh=bh: e.tensor_tensor(BOP[:, :, bh], e2[:], asb[:], ALU.mult), reads=[e2, asb], writes=[BOP])
                    c.op("dve", lambda e, hp=hp: e.tensor_scalar(e4[:], asb[:], pp[:, hp, 3:4], pp[:, hp, 4:5], op0=ALU.mult, op1=ALU.add), reads=[asb, pp], writes=[e4])
                    c.op("dve", lambda e, K_=K_: e.tensor_tensor(e4[:], e4[:], K_[:], ALU.mult), reads=[e4, K_], writes=[e4])
                    c.op("pool", lambda e, bh=bh: e.tensor_copy(KOP[:, :, bh], e4[:]), reads=[e4], writes=[KOP])
                    c.op("pool", lambda e, bh=bh, R_=R_: e.tensor_copy(ROP[:, :, bh], R_[:]), reads=[R_], writes=[ROP])
                    c.op("dve", lambda e, R_=R_, hp=hp: e.scalar_tensor_tensor(e1[:], R_[:], pp[:, hp, 5:6], e4[:], op0=ALU.mult, op1=ALU.mult), reads=[R_, pp, e4], writes=[e1])
                    c.op("pe", lambda e: e.matmul(pVB[:, 256:384], bof[:], e1[:], start=True, stop=True), reads=[bof, e1], writes=[pVB])
                    c.op("dve", lambda e, bh=bh, V_=V_: e.tensor_tensor(bonus[:, bh, :], pVB[:, 256:384], V_[:], ALU.mult), reads=[pVB, V_], writes=[bonus])
                    c.op("pe", lambda e, V_=V_: e.transpose(pSA[:, 256:384], V_[:], S.identf[:]), reads=[V_, S.identf], writes=[pSA])
                    c.op("act", lambda e, bh=bh: e.copy(vtok[:, bh, :], pSA[:, 256:384]), reads=[pSA], writes=[vtok])
            vt4 = vtok[:].rearrange("p g (h i) -> p g h i", h=2)

            def v3(tk):
                return tk[:, 0:HW].rearrange("p (g i) -> p g i", i=64)

            def bc(op_, tl, q):
                return op_[:, tl, q * HG:(q + 1) * HG].unsqueeze(2).to_broadcast([128, HG, 64])

            def lookahead_pe(tl):
                for q in range(2):
                    for hh in range(2):
                        c.op("pe", lambda e, q=q, hh=hh, tl=tl: e.matmul(psVBq[q][hh * 64:(hh + 1) * 64, 0:HW], S.identb[:, tl:tl + 1].to_broadcast([128, 64]),
                                                                         vt4[:, q * HG:(q + 1) * HG, hh, :], start=True, stop=True),
                             reads=[S.identb, vtok], writes=[psVBq[q]])
                    c.op("act", lambda e, q=q: e.copy(vs[q][:], psVBq[q][:, 0:HW]), reads=[psVBq[q]], writes=[vs[q]])

            def lookahead_pool(tl):
                for q in range(2):
                    TC = tCn[q][tl % 2]
                    c.op("pool", lambda e, tl=tl, TC=TC, q=q: e.tensor_tensor(v3(TC), v3(vs[q]), bc(KOP, tl, q), ALU.mult), reads=[vs[q], KOP], writes=[TC])

            def emit_tmpA(tl, par):
                for q in range(2):
                    Zi = Zq[q][par]
                    c.op("dve", lambda e, tl=tl, q=q, Zi=Zi: e.tensor_tensor(v3(tA[q]), v3(Zi), bc(AOP, tl, q), ALU.mult), reads=[Zi, AOP], writes=[tA[q]])

            lookahead_pe(0)
            lookahead_pool(0)
            emit_tmpA(0, step % 2)
            for tl in range(128):
                par = step % 2
                step += 1
                for q in range(2):
                    c.op("pe", lambda e, q=q: e.matmul(psSAq[q][:, 0:HW], bob[:], tA[q][:, 0:HW], start=True, stop=True), reads=[bob, tA[q]], writes=[psSAq[q]])
                if tl < 127:
                    lookahead_pe(tl + 1)
                for q in range(2):
                    Zi, TC = Zq[q][par], tCn[q][tl % 2]
                    c.op("pool", lambda e, tl=tl, q=q, Zi=Zi: e.tensor_tensor(v3(tP[q]), v3(Zi), bc(WOP, tl, q), ALU.mult), reads=[Zi, WOP], writes=[tP[q]])
                    c.op("pool", lambda e, q=q, TC=TC: e.tensor_tensor(tP[q][:], tP[q][:], TC[:], ALU.add), reads=[tP[q], TC], writes=[tP[q]])
                if tl < 127:
                    lookahead_pool(tl + 1)
                for q in range(2):
                    Zo = Zq[q][1 - par]
                    c.op("dve", lambda e, tl=tl, q=q: e.tensor_tensor(v3(tB[q]), v3(psSAq[q]), bc(BOP, tl, q), ALU.mult), reads=[psSAq[q], BOP], writes=[tB[q]])
                    c.op("dve", lambda e, q=q, Zo=Zo: e.tensor_tensor(Zo[:], tP[q][:], tB[q][:], ALU.add), reads=[tP[q], tB[q]], writes=[Zo])
                if tl < 127:
                    emit_tmpA(tl + 1, 1 - par)
                for q in range(2):
                    Zo = Zq[q][1 - par]
                    c.op("dve", lambda e, tl=tl, q=q, Zo=Zo: e.tensor_tensor(v3(tD[q]), v3(Zo), bc(ROP, tl, q), ALU.mult), reads=[Zo, ROP], writes=[tD[q]])
                    for hh in range(2):
                        c.op("pe", lambda e, q=q, hh=hh, tl=tl: e.matmul(psYq[q][hh][:, 0:HW], ahw[hh * 64:(hh + 1) * 64, 127 - tl:255 - tl],
                                                                         tD[q][hh * 64:(hh + 1) * 64, 0:HW], start=(tl == 0), stop=(tl == 127)),
                             reads=[ahw, tD[q]], writes=[psYq[q][hh]])
            ys4 = ysb[:].rearrange("p (g h) i -> p g h i", h=2)
            for q in range(2):
                for hh in range(2):
                    c.op("act", lambda e, hh=hh, q=q: e.copy(ys4[:, q * HG:(q + 1) * HG, hh, :], psYq[q][hh][:, 0:HW].rearrange("p (g i) -> p g i", i=64)),
                         reads=[psYq[q][hh]], writes=[ysb])
            G2 = NBH * 2
            c.op("dve", lambda e: e.tensor_reduce(yst[:, :, 0], ysb[:], AX.X, ALU.add), reads=[ysb], writes=[yst])
            c.op("dve", lambda e: e.tensor_scalar(yst[:, :, 0], yst[:, :, 0], 1.0 / 64.0, None, op0=ALU.mult), reads=[yst], writes=[yst])
            c.op("dve", lambda e: e.tensor_tensor(ysb[:], ysb[:], yst[:, :, 0:1].to_broadcast([128, G2, 64]), ALU.subtract), reads=[ysb, yst], writes=[ysb])
            c.op("pool", lambda e: e.tensor_tensor(ysq[:], ysb[:], ysb[:], ALU.mult), reads=[ysb], writes=[ysq])
            c.op("dve", lambda e: e.tensor_reduce(yst[:, :, 1], ysq[:], AX.X, ALU.add), reads=[ysq], writes=[yst])
            c.op("dve", lambda e: e.tensor_scalar(yst[:, :, 1], yst[:, :, 1], 1.0 / 64.0, 64e-5, op0=ALU.mult, op1=ALU.add), reads=[yst], writes=[yst])
            c.op("act", lambda e: e.activation(yst[:, :, 1], yst[:, :, 1], AF.Sqrt), reads=[yst], writes=[yst])
            c.op("dve", lambda e: e.reciprocal(yst[:, :, 1], yst[:, :, 1]), reads=[yst], writes=[yst])
            c.op("dve", lambda e: e.tensor_tensor(ysb[:], ysb[:], yst[:, :, 1:2].to_broadcast([128, G2, 64]), ALU.mult), reads=[ysb, yst], writes=[ysb])
            for b in range(NB):
                for hp in range(4):
                    bh = b * 4 + hp
                    Y, YB = yo[bh % 2], yob[bh % 2]
                    pSA = psSAq[bh % 2]
                    c.op("pe", lambda e, bh=bh, pSA=pSA: e.transpose(pSA[:, 0:128], ysb[:, 2 * bh:2 * bh + 2, :].rearrange("p h i -> p (h i)"), S.identf[:]),
                         reads=[ysb, S.identf], writes=[pSA])
                    c.op("dve", lambda e, Y=Y, hp=hp, pSA=pSA: e.tensor_scalar(Y[:], pSA[:, 0:128], pp[:, hp, 6:7], pp[:, hp, 7:8], op0=ALU.mult, op1=ALU.add), reads=[pSA, pp], writes=[Y])
                    c.op("pool", lambda e, Y=Y, bh=bh: e.tensor_tensor(Y[:], Y[:], bonus[:, bh, :], ALU.add), reads=[Y, bonus], writes=[Y])
                    c.op("pool", lambda e, Y=Y, YB=YB, bh=bh: e.tensor_tensor(YB[:], Y[:], gT[:, bh, :], ALU.mult), reads=[Y, gT], writes=[YB])
                    c.dma(c.qsel(), oT[b * 16 + tb][:, (4 + hp) * 128:(5 + hp) * 128], YB[:], reads=[YB], writes=[S.oT_tok], wtok=YB)
        c.barrier()


_NC_CACHE = {}


def kernel(**inputs):
    n = 8
    NB = 32 // n
    if "nc" not in _NC_CACHE:
        _NC_CACHE["nc"] = build(NB=NB, layers=(0, 1, 2, 3), mode="full")
    nc = _NC_CACHE["nc"]
    x = np.ascontiguousarray(inputs["x"], dtype=np.float32)
    cs = consts()
    in_maps = []
    for ci in range(n):
        m = {"x": x[ci * NB:(ci + 1) * NB].reshape(NB * T, D)}
        for k in WSPEC:
            m[k] = np.ascontiguousarray(inputs[k], dtype=np.float32)
        m.update(cs)
        in_maps.append(m)
    res = run_bass_kernel_spmd(nc, in_maps, core_ids=list(range(n)))
    out = np.stack([r["y"].reshape(NB, T, D) for r in res.results], 0).reshape(32, T, D)
    return out.astype(np.float32)
```
